# Optimizing a Trainium2 kernel written in Bass

```python
import math
import jax, jax.numpy as jnp
from jax import lax
import numpy as np

D_MODEL = 1024
BATCH = 4
SEQ = 4096
DEPTH = 1

RET_HEADS = 4
RET_DK = 128
RET_DV = 256
RET_CHUNK = 128
ROPE_BASE = 10000.0
ATT_PATTERNS = ((128, 1), (512, 4), (2048, 16))
ATT_GROUPS = 3
ATT_HEADS_PER_GROUP = 4
ATT_HEADS = ATT_GROUPS * ATT_HEADS_PER_GROUP
ATT_DH = 128
ATT_QBLOCK = 128
REL_BUCKETS = 32
REL_MAX_EXACT = REL_BUCKETS // 2
REL_MAX_DIST = 2048
N_EXPERTS = 32
TOP_K = 4
D_EXPERT = D_MODEL
SWIGLU_LIMIT = 7.0
SWIGLU_ALPHA = 1.702
MOE_BLOCK = 128
EPS = 1e-6
GN_EPS = 1e-5

RET_QK_W = RET_HEADS * RET_DK
RET_V_W = RET_HEADS * RET_DV
ATT_W = ATT_HEADS * ATT_DH
ATT_OUT_W = ATT_HEADS_PER_GROUP * ATT_DH
IN_SPLITS = (RET_QK_W, RET_QK_W, RET_V_W, RET_V_W, ATT_W, ATT_W, ATT_W, D_MODEL, D_MODEL)
IN_W = 9728

kernel_name = "hybrid_retention_dilated_moe_block"


def _split_points(sizes):
    pts, acc = [], 0
    for s in sizes[:-1]:
        acc += s
        pts.append(acc)
    return pts


def rms_norm(x, g):
    xf = x.astype(jnp.float32)
    return xf * lax.rsqrt(jnp.mean(xf * xf, axis=-1, keepdims=True) + EPS) * g


def modulate(h, shift, scale):
    return h * (1.0 + scale[:, None, :]) + shift[:, None, :]


def rotary(t, pos):
    half = t.shape[-1] // 2
    inv = ROPE_BASE ** (-jnp.arange(half, dtype=jnp.float32) / half)
    ang = pos[:, None] * inv[None, :]
    cos, sin = jnp.cos(ang)[:, None, :], jnp.sin(ang)[:, None, :]
    t1, t2 = t[..., :half], t[..., half:]
    return jnp.concatenate([t1 * cos - t2 * sin, t1 * sin + t2 * cos], axis=-1)


def retention(q, k, v, g):
    B, S = q.shape[:2]
    H, dk, dv, C = RET_HEADS, RET_DK, RET_DV, RET_CHUNK
    nC = S // C
    pos = jnp.arange(S, dtype=jnp.float32)
    q = rotary(q.astype(jnp.float32).reshape(B, S, H, dk), pos)
    k = rotary(k.astype(jnp.float32).reshape(B, S, H, dk), pos) * (dk ** -0.5)
    v = v.astype(jnp.float32).reshape(B, S, H, dv)
    log_g = jnp.log1p(-jnp.exp2(-5.0 - jnp.arange(H, dtype=jnp.float32)))
    i = jnp.arange(C, dtype=jnp.float32)
    diff = i[:, None] - i[None, :]
    decay = jnp.where(diff[None] >= 0, jnp.exp(jnp.maximum(diff, 0.0)[None] * log_g[:, None, None]), 0.0)
    qc = q.reshape(B, nC, C, H, dk)
    kc = k.reshape(B, nC, C, H, dk)
    vc = v.reshape(B, nC, C, H, dv)
    scores = jnp.einsum('bnihd,bnjhd->bnhij', qc, kc) * decay
    inner = jnp.einsum('bnhij,bnjhe->bnihe', scores, vc)
    zeta = jnp.exp((C - 1.0 - i)[:, None] * log_g[None, :])
    kv = jnp.einsum('bnjhd,bnjhe->nbhde', kc * zeta[:, :, None], vc)
    g_chunk = jnp.exp(C * log_g)[None, :, None, None]

    def step(R, kv_n):
        return g_chunk * R + kv_n, R

    _, R_prev = lax.scan(step, jnp.zeros_like(kv[0]), kv)
    xi = jnp.exp((i + 1.0)[:, None] * log_g[None, :])
    cross = jnp.einsum('bnihd,nbhde->bnihe', qc * xi[:, :, None], R_prev)
    y = (inner + cross).reshape(B, S, H, dv)
    mu = jnp.mean(y, axis=-1, keepdims=True)
    var = jnp.mean(jnp.square(y - mu), axis=-1, keepdims=True)
    y = (y - mu) * lax.rsqrt(var + GN_EPS)
    return jax.nn.silu(g.astype(jnp.float32)) * y.reshape(B, S, H * dv)


def t5_bucket(dist):
    is_small = dist < REL_MAX_EXACT
    ratio = jnp.log(jnp.maximum(dist, 1).astype(jnp.float32) / REL_MAX_EXACT) / math.log(REL_MAX_DIST / REL_MAX_EXACT)
    large = REL_MAX_EXACT + (ratio * (REL_BUCKETS - REL_MAX_EXACT)).astype(jnp.int32)
    large = jnp.minimum(large, REL_BUCKETS - 1)
    return jnp.where(is_small, dist, large)


def dilated_attention(q, k, v, rel_bias):
    B, S = q.shape[:2]
    G, Hg, dh, QB = ATT_GROUPS, ATT_HEADS_PER_GROUP, ATT_DH, ATT_QBLOCK
    nb = S // QB
    q = q.reshape(B, S, G, Hg, dh)
    k = k.reshape(B, S, G, Hg, dh)
    v = v.reshape(B, S, G, Hg, dh)
    qb = q.reshape(B, nb, QB, G, Hg, dh).transpose(1, 0, 2, 3, 4, 5)
    ks = [k[:, :, g] for g in range(G)]
    vs = [v[:, :, g] for g in range(G)]
    biases = []
    for g, (w, r) in enumerate(ATT_PATTERNS):
        nk = w // r + 1
        bkt = t5_bucket(r * jnp.arange(nk, dtype=jnp.int32))
        biases.append(rel_bias[bkt, g * Hg:(g + 1) * Hg].T.astype(jnp.float32))
    scale = dh ** -0.5

    def block(args):
        n, qn = args
        pos = n * QB + jnp.arange(QB, dtype=jnp.int32)
        outs, lses = [], []
        for g, (w, r) in enumerate(ATT_PATTERNS):
            nk = w // r + 1
            idx = pos[:, None] - r * jnp.arange(nk, dtype=jnp.int32)[None, :]
            valid = idx >= 0
            idc = jnp.maximum(idx, 0)
            kg = jnp.take(ks[g], idc, axis=1)
            vg = jnp.take(vs[g], idc, axis=1)
            s = jnp.einsum('bqhd,bqjhd->bhqj', qn[:, :, g], kg).astype(jnp.float32) * scale
            s = s + biases[g][:, None, :]
            s = jnp.where(valid[None, None], s, -jnp.inf)
            m = jnp.max(s, axis=-1, keepdims=True)
            e = jnp.exp(s - m)
            l = jnp.sum(e, axis=-1, keepdims=True)
            o = jnp.einsum('bhqj,bqjhd->bqhd', e / l, vg.astype(jnp.float32))
            outs.append(o)
            lses.append((m + jnp.log(l))[..., 0].transpose(0, 2, 1))
        alpha = jax.nn.softmax(jnp.stack(lses, axis=0), axis=0)
        out = alpha[0][..., None] * outs[0]
        for g in range(1, G):
            out = out + alpha[g][..., None] * outs[g]
        return out

    out = lax.map(block, (jnp.arange(nb, dtype=jnp.int32), qb))
    return out.transpose(1, 0, 2, 3, 4).reshape(B, S, Hg * dh)


def moe(h, w_router, b_router, w1, b1, w2, b2):
    B, S, D = h.shape
    T = B * S
    xf = h.reshape(T, D)
    logits = (xf @ w_router + b_router).astype(jnp.float32)
    top_val, top_idx = lax.top_k(logits, TOP_K)
    gates = jax.nn.softmax(top_val, axis=-1)
    A = T * TOP_K
    flat_e = top_idx.reshape(A).astype(jnp.int32)
    flat_tok = jnp.arange(A, dtype=jnp.int32) // TOP_K
    flat_gate = gates.reshape(A)
    order = jnp.argsort(flat_e, stable=True)
    se = flat_e[order]
    counts = jnp.bincount(flat_e, length=N_EXPERTS)
    starts = jnp.cumsum(counts) - counts
    padded = (counts + MOE_BLOCK - 1) // MOE_BLOCK * MOE_BLOCK
    pend = jnp.cumsum(padded)
    pstart = pend - padded
    dest = pstart[se] + jnp.arange(A, dtype=jnp.int32) - starts[se]
    nblk = -(-A // MOE_BLOCK) + N_EXPERTS
    n_rows = nblk * MOE_BLOCK
    row_tok = jnp.full((n_rows,), T, jnp.int32).at[dest].set(flat_tok[order])
    row_gate = jnp.zeros((n_rows,), jnp.float32).at[dest].set(flat_gate[order])
    block_e = jnp.minimum(jnp.searchsorted(pend, jnp.arange(nblk, dtype=jnp.int32) * MOE_BLOCK, side='right'), N_EXPERTS - 1)
    x_pad = jnp.concatenate([xf, jnp.zeros((1, D), xf.dtype)], axis=0)
    xin = x_pad[row_tok].reshape(nblk, MOE_BLOCK, D)

    def expert_block(args):
        xb, e = args
        u = xb @ w1[e] + b1[e]
        glu = jnp.minimum(u[:, ::2], SWIGLU_LIMIT)
        lin = jnp.clip(u[:, 1::2], -SWIGLU_LIMIT, SWIGLU_LIMIT)
        a = glu * jax.nn.sigmoid(SWIGLU_ALPHA * glu) * (lin + 1.0)
        return a @ w2[e] + b2[e]

    yb = lax.map(expert_block, (xin, block_e)).reshape(n_rows, D)
    y = jnp.zeros((T + 1, D), jnp.float32).at[row_tok].add(yb.astype(jnp.float32) * row_gate[:, None])
    return y[:T].reshape(B, S, D)


def setup_inputs(seed: int = 0) -> dict:
    key = jax.random.key(seed)
    ks = jax.random.split(key, 20)
    D, L, E, F = D_MODEL, DEPTH, N_EXPERTS, D_EXPERT
    nrm = jax.random.normal
    f32 = jnp.float32
    return {
        "x": nrm(ks[0], (BATCH, SEQ, D), f32),
        "c": nrm(ks[1], (BATCH, D), f32),
        "w_ada": nrm(ks[2], (L, D, 6 * D), f32) * (0.5 * D ** -0.5),
        "b_ada": nrm(ks[3], (L, 6 * D), f32) * 0.02,
        "g_mix": 1.0 + 0.02 * nrm(ks[4], (L, D), f32),
        "w_in": nrm(ks[5], (L, D, IN_W), f32) * D ** -0.5,
        "rel_bias": nrm(ks[6], (REL_BUCKETS, ATT_HEADS), f32) * 0.2,
        "w_ret_o": nrm(ks[7], (L, RET_V_W, D), f32) * RET_V_W ** -0.5,
        "w_att_o": nrm(ks[8], (L, ATT_OUT_W, D), f32) * ATT_OUT_W ** -0.5,
        "w_o": nrm(ks[9], (L, D, D), f32) * D ** -0.5,
        "g_ffn": 1.0 + 0.02 * nrm(ks[10], (L, D), f32),
        "w_router": nrm(ks[11], (L, D, E), f32) * D ** -0.5,
        "b_router": nrm(ks[12], (L, E), f32) * 0.01,
        "w_mlp1": nrm(ks[13], (L, E, D, 2 * F), f32) * D ** -0.5,
        "b_mlp1": nrm(ks[14], (L, E, 2 * F), f32) * 0.01,
        "w_mlp2": nrm(ks[15], (L, E, F, D), f32) * F ** -0.5,
        "b_mlp2": nrm(ks[16], (L, E, D), f32) * 0.01,
        "g_final": 1.0 + 0.02 * nrm(ks[17], (D,), f32),
    }


def reference(x, c, w_ada, b_ada, g_mix, w_in, rel_bias, w_ret_o, w_att_o, w_o, g_ffn,
              w_router, b_router, w_mlp1, b_mlp1, w_mlp2, b_mlp2, g_final):
    cs = jax.nn.silu(c.astype(jnp.float32))
    pts = _split_points(IN_SPLITS)
    for l in range(DEPTH):
        mod = cs @ w_ada[l] + b_ada[l]
        sh_a, sc_a, gt_a, sh_f, sc_f, gt_f = jnp.split(mod, 6, axis=-1)
        h = modulate(rms_norm(x, g_mix[l]), sh_a, sc_a)
        u = h @ w_in[l]
        rq, rk, rv, rg, aq, ak, av, ga, gb = jnp.split(u, pts, axis=-1)
        ret = retention(rq, rk, rv, rg)
        att = dilated_attention(aq, ak, av, rel_bias)
        merged = jax.nn.sigmoid(ga) * (ret @ w_ret_o[l]) + jax.nn.sigmoid(gb) * (att @ w_att_o[l])
        x = x + gt_a[:, None, :] * (merged @ w_o[l])
        h = modulate(rms_norm(x, g_ffn[l]), sh_f, sc_f)
        x = x + gt_f[:, None, :] * moe(h, w_router[l], b_router[l], w_mlp1[l], b_mlp1[l], w_mlp2[l], b_mlp2[l])
    return rms_norm(x, g_final)
```

```python
import contextlib
import math
import numpy as np
import concourse.bass as bass
import concourse.mybir as mybir
from concourse.bass_utils import run_bass_kernel_spmd

F32 = mybir.dt.float32
BF16 = mybir.dt.bfloat16
AF = mybir.ActivationFunctionType
ALU = mybir.AluOpType
AX = mybir.AxisListType

D = 1024
SEQ = 4096
HALF = 2048
NT = 16
EPS = 1e-6
GN_EPS = 1e-5
N_EXP = 32
ATT_PATTERNS = ((128, 1), (512, 4), (2048, 16))
NSLOT = 4

C_RQ, C_RK, C_RV, C_RG = 0, 512, 1024, 2048
C_AQ, C_AK, C_AV = 3072, 4608, 6144
C_GA, C_GB = 7680, 8704


class Sched:
    ENG = ['sync', 'scalar', 'vector', 'gpsimd', 'tensor']

    def __init__(self, nc):
        self.nc = nc
        self.ops = {e: [] for e in self.ENG}
        self.cnt = {}
        self.waited = {e: {} for e in self.ENG}
        self.lastw = {}
        self.readers = {}

    def op(self, eng, fn, reads=(), writes=(), sem=None, inc=1):
        deps = {}
        own = ('E', eng)

        def add(d, raw):
            if d is None:
                return
            k, v = d
            if k == own and not raw:
                return
            if deps.get(k, 0) < v:
                deps[k] = v
        for b in reads:
            add(self.lastw.get(b), True)
        waw_own = eng in ('vector', 'gpsimd', 'scalar')
        for b in writes:
            add(self.lastw.get(b), waw_own)
            for r in self.readers.get(b, ()):
                add(r, False)
        waits = []
        for k, v in deps.items():
            if self.waited[eng].get(k, 0) < v:
                self.waited[eng][k] = v
                waits.append((k, v))
        semkey = sem if sem is not None else own
        self.cnt[semkey] = self.cnt.get(semkey, 0) + inc
        val = self.cnt[semkey]
        self.ops[eng].append((waits, fn, semkey, inc))
        for b in reads:
            self.readers.setdefault(b, []).append((semkey, val))
        for b in writes:
            self.lastw[b] = (semkey, val)
            self.readers[b] = []
        return val

    def barrier(self):
        for e in self.ENG:
            waits = []
            for k, v in self.cnt.items():
                if self.waited[e].get(k, 0) < v:
                    self.waited[e][k] = v
                    waits.append((k, v))
            if waits:
                self.ops[e].append((waits, None, None, 0))
        self.lastw = {}
        self.readers = {}

    def emit(self, final_waits=()):
        nc = self.nc
        keys = list(self.cnt.keys())
        with contextlib.ExitStack() as st:
            sems = {}
            for i, k in enumerate(keys):
                sems[k] = st.enter_context(nc.semaphore("s%d" % i))
            block = st.enter_context(nc.Block())

            def body(engname):
                def f(e):
                    for waits, fn, semkey, inc in self.ops[engname]:
                        for k, v in waits:
                            e.wait_ge(sems[k], v)
                        if fn is None:
                            continue
                        ins = fn(e)
                        ins.then_inc(sems[semkey], inc)
                    if engname == 'sync':
                        for k in final_waits:
                            e.wait_ge(sems[k], self.cnt[k])
                return f
            block.sync(body('sync'))
            block.scalar(body('scalar'))
            block.vector(body('vector'))
            block.gpsimd(body('gpsimd'))
            block.tensor(body('tensor'))


def build_program(stage=99, dbg_cols=0):
    nc = bass.Bass("TRN2", target_bir_lowering=False)
    S = Sched(nc)

    def din(name, shape, dt=F32):
        return nc.dram_tensor(name, list(shape), dt, kind="ExternalInput").ap()

    x_own = din("x_own", [HALF, D])
    x_prev = din("x_prev", [HALF, D])
    flag_d = din("flag", [128, 1])
    ccol_d = din("c_col", [128, 8])
    w_ada_d = din("w_ada", [D, 6 * D])
    badacol_d = din("bada_col", [128, 32])
    badabc_d = din("bada_bc", [128, 2, D])
    gmix_d = din("gmix_col", [128, 8])
    gffn_d = din("gffn_col", [128, 8])
    gfin_d = din("gfin_bc", [128, D])
    win1_d = din("w_in_r1", [68, 128, 8, 128])
    win2_d = din("w_in_r2", [8, 128, 8, 256])
    ropec_d = din("rope_cos", [128, SEQ])
    ropes_d = din("rope_sin", [128, SEQ])
    dm_d = din("dmask", [128, 4, 128])
    rvec_d = din("rvec", [128, 16])
    zeta2_d = din("zeta2", [128, 4, 16])
    relb_d = din("rel_bias", [32, 12])
    ohf_d = din("ohf", [32, 3, 384])
    identf_d = din("identf", [128, 128])
    jf_d = din("jf", [128, 128])
    wreto_d = din("w_ret_o", [D, D])
    watto_d = din("w_att_o", [512, D])
    wo_d = din("w_o", [D, D])
    wr_d = din("w_router", [D, N_EXP])
    brbc_d = din("brouter_bc", [128, N_EXP])
    if stage > 5:
        w1r_d = din("w1r", [N_EXP, 8, 128, 8, 256])
        b1r_d = din("b1r", [128, N_EXP, 8, 2])
        w2_d = din("w_mlp2", [N_EXP, D, D])
        b2_d = din("b_mlp2", [N_EXP, D])
    out_d = nc.dram_tensor("out", [HALF, D], F32, kind="ExternalOutput").ap()
    fd_t = nc.dram_tensor("fd_scratch", [12, 384], F32)
    fd_d = fd_t.ap()
    dbg_d = None
    if dbg_cols:
        dbg_d = nc.dram_tensor("dbg", [128, dbg_cols], F32, kind="ExternalOutput").ap()

    root = contextlib.ExitStack()
    root.__enter__()

    def sb(st, name, shape, dt, side=None):
        name = "sb_" + name
        if side is None:
            return st.enter_context(nc.sbuf_tensor(name, list(shape), dt))
        return st.enter_context(nc.sbuf_tensor(name, list(shape), dt, side=side))

    PS = root.enter_context(nc.psum_tensor("psall", [128, 8, 512], F32))

    def bank(i):
        return PS[:, i, :]

    def bank_bf(i):
        return PS[:, i, :].bitcast(BF16)

    dma_n = [0]

    def dma(eng, out, in_, reads=(), writes=(), semname=None):
        if semname is None:
            semname = "u%d" % dma_n[0]
            dma_n[0] += 1
        return S.op(eng, lambda e: e.dma_start(out=out, in_=in_), reads=reads, writes=writes,
                    sem=('D', semname), inc=16)

    def V(fn, reads=(), writes=()):
        return S.op('vector', fn, reads, writes)

    def A(fn, reads=(), writes=()):
        return S.op('scalar', fn, reads, writes)

    def G(fn, reads=(), writes=()):
        return S.op('gpsimd', fn, reads, writes)

    def T(fn, reads=(), writes=()):
        return S.op('tensor', fn, reads, writes)

    flag = sb(root, "flag", [128, 1], F32)
    identb = sb(root, "identb", [128, 128], BF16)
    identf = sb(root, "identf", [128, 128], F32)
    onesb = sb(root, "onesb", [128, 128], BF16)
    onesf = sb(root, "onesf", [128, 128], F32)
    modcol = sb(root, "modcol", [128, 32], F32)
    scA = sb(root, "scA", [128, 8], F32)
    scAp = sb(root, "scAp", [128, 8], F32)
    shAp = sb(root, "shAp", [128, 8], F32)
    scF = sb(root, "scF", [128, 8], F32)
    gtA_bc = sb(root, "gtA_bc", [128, D], F32)
    gtF_bc = sb(root, "gtF_bc", [128, D], F32)
    ss = sb(root, "ss", [128, 64], F32)
    rs = sb(root, "rs", [128, 64], F32)
    shA = modcol[:, 0:8]
    shF = modcol[:, 16:24]

    dma('sync', flag[:], flag_d, writes=['flag'])
    dma('sync', identf[:], identf_d, writes=['identf'])
    dma('gpsimd', identb[:], identf_d, writes=['identb'])
    V(lambda e: e.memset(onesb[:], 1.0), writes=['onesb'])
    V(lambda e: e.memset(onesf[:], 1.0), writes=['onesf'])

    with contextlib.ExitStack() as p0:
        ccol = sb(p0, "ccol", [128, 8], F32)
        cs2 = sb(p0, "cs2", [128, 8, 2], F32)
        csb = sb(p0, "csb", [128, 8, 128], F32)
        badacol = sb(p0, "badacol", [128, 32], F32)
        badabc = sb(p0, "badabc", [128, 2, D], F32)
        gmix = sb(p0, "gmix", [128, 8], F32)
        gffn = sb(p0, "gffn", [128, 8], F32)
        wast = [sb(p0, "wast%d" % i, [128, 8, 512], F32) for i in range(3)]
        dma('sync', ccol[:], ccol_d, writes=['ccol'])
        dma('sync', badacol[:], badacol_d, writes=['badacol'])
        dma('sync', badabc[:], badabc_d, writes=['badabc'])
        dma('sync', gmix[:], gmix_d, writes=['gmix'])
        dma('sync', gffn[:], gffn_d, writes=['gffn'])
        for j in range(2):
            A(lambda e, j=j: e.activation(out=cs2[:, :, j], in_=ccol[:], func=AF.Silu),
              reads=['ccol'], writes=[('cs2', j)])
        for kc in range(8):
            V(lambda e, kc=kc: e.tensor_scalar(out=csb[:, kc, :], in0=onesf[:], scalar1=cs2[:, kc, 0:1],
                                               scalar2=None, op0=ALU.mult),
              reads=['onesf', ('cs2', 0)], writes=['csb'])
        w_ada_v = w_ada_d.rearrange("(kc p) n -> p kc n", p=128)
        colsec = {0: 0, 1: 1, 3: 2, 4: 3}
        for j in range(12):
            sec, half = j // 2, j % 2
            wt = wast[j % 3]
            dma('sync', wt[:], w_ada_v[:, :, j * 512:(j + 1) * 512], writes=[('wast', j % 3)],
                semname="wast%d" % (j % 3))
            if sec in colsec:
                si = colsec[sec]

                def mmcol(e, wt=wt, si=si, half=half):
                    ins = None
                    for cc in range(4):
                        c = half * 4 + cc
                        o = (si * 8 + c) * 2
                        for kc in range(8):
                            ins = e.matmul(bank(0)[:, o:o + 2], lhsT=wt[:, kc, cc * 128:(cc + 1) * 128],
                                           rhs=cs2[:, kc, :], start=(kc == 0), stop=(kc == 7))
                    return ins
                T(mmcol, reads=[('wast', j % 3), ('cs2', 0), ('cs2', 1)], writes=[('ps', 0)])
            else:
                bk = 1 + half
                which = 0 if sec == 2 else 1
                dst = gtA_bc if sec == 2 else gtF_bc

                def mmrow(e, wt=wt, bk=bk):
                    ins = None
                    for kc in range(8):
                        ins = e.matmul(bank(bk)[:, 0:512], lhsT=csb[:, kc, :], rhs=wt[:, kc, :],
                                       start=(kc == 0), stop=(kc == 7))
                    return ins
                T(mmrow, reads=[('wast', j % 3), 'csb'], writes=[('ps', bk)])
                V(lambda e, bk=bk, dst=dst, which=which, half=half: e.tensor_tensor(
                    out=dst[:, half * 512:(half + 1) * 512], in0=bank(bk)[:, 0:512],
                    in1=badabc[:, which, half * 512:(half + 1) * 512], op=ALU.add),
                  reads=[('ps', bk), 'badabc'], writes=[('gt', sec, half)])
        V(lambda e: e.tensor_tensor(out=modcol[:], in0=bank(0)[:, 0:64:2], in1=badacol[:], op=ALU.add),
          reads=[('ps', 0), 'badacol'], writes=['modcol'])
        V(lambda e: e.scalar_tensor_tensor(out=scA[:], in0=modcol[:, 8:16], scalar=1.0, in1=gmix[:],
                                           op0=ALU.add, op1=ALU.mult),
          reads=['modcol', 'gmix'], writes=['scA'])
        V(lambda e: e.scalar_tensor_tensor(out=scF[:], in0=modcol[:, 24:32], scalar=1.0, in1=gffn[:],
                                           op0=ALU.add, op1=ALU.mult),
          reads=['modcol', 'gffn'], writes=['scF'])
        V(lambda e: e.tensor_scalar(out=scAp[:], in0=scA[:], scalar1=flag[:, 0:1], scalar2=None, op0=ALU.mult),
          reads=['scA', 'flag'], writes=['scAp'])
        V(lambda e: e.tensor_scalar(out=shAp[:], in0=modcol[:, 0:8], scalar1=flag[:, 0:1], scalar2=None,
                                    op0=ALU.mult),
          reads=['modcol', 'flag'], writes=['shAp'])
        S.barrier()

    if stage == 0:
        dma('sync', dbg_d[:, 0:32], modcol[:], semname="out")
        dma('sync', dbg_d[:, 32:40], scA[:], semname="out")
        dma('sync', dbg_d[:, 40:48], scAp[:], semname="out")
        dma('sync', dbg_d[:, 48:56], scF[:], semname="out")
        dma('sync', dbg_d[:, 64:64 + 1024], gtA_bc[:], semname="out")
        dma('sync', dbg_d[:, 1088:1088 + 1024], gtF_bc[:], semname="out")
        S.emit(final_waits=[k for k in (('D', 'out'), ('D', 'outg')) if k in S.cnt])
        root.close()
        return nc

    mix = contextlib.ExitStack()
    mix.__enter__()
    HT = sb(mix, "HT", [128, 8, SEQ], BF16)
    RetT = sb(mix, "RetT", [128, 8, HALF], BF16)
    WB = [sb(mix, "WB%d" % i, [128, 8, 256], BF16) for i in range(NSLOT)]

    with contextlib.ExitStack() as p1:
        NXI, NXN = 6, 3
        xin = [sb(p1, "xin%d" % i, [128, D], F32) for i in range(NXI)]
        xn = [sb(p1, "xn%d" % i, [128, D], BF16) for i in range(NXN)]
        junk = sb(p1, "junk", [128, D], BF16)

        def st_dma(i):
            src = x_prev if i < 16 else x_own
            t = i % 16
            dma('sync', xin[i % NXI][:], src[t * 128:(t + 1) * 128, :], writes=[('xin', i % NXI)],
                semname="xin%d" % (i % NXI))

        def st_sq(i):
            A(lambda e: e.activation(out=junk[:], in_=xin[i % NXI][:], func=AF.Square, accum_out=ss[:, i:i + 1]),
              reads=[('xin', i % NXI)], writes=['junk', ('ss', i)])

        def st_ts(i):
            V(lambda e: e.tensor_scalar(out=rs[:, i:i + 1], in0=ss[:, i:i + 1], scalar1=1.0 / D, scalar2=EPS,
                                        op0=ALU.mult, op1=ALU.add),
              reads=[('ss', i)], writes=[('rs', i)])

        def st_sqrt(i):
            A(lambda e: e.activation(out=rs[:, i:i + 1], in_=rs[:, i:i + 1], func=AF.Sqrt),
              reads=[('rs', i)], writes=[('rs', i)])

        def st_xn(i):
            V(lambda e: e.reciprocal(out=rs[:, i:i + 1], in_=rs[:, i:i + 1]),
              reads=[('rs', i)], writes=[('rs', i)])
            V(lambda e: e.tensor_scalar(out=xn[i % NXN][:], in0=xin[i % NXI][:], scalar1=rs[:, i:i + 1],
                                        scalar2=None, op0=ALU.mult),
              reads=[('xin', i % NXI), ('rs', i)], writes=[('xn', i % NXN)])

        def st_tr(i):
            pT = bank_bf(i % 4)

            def tr(e):
                ins = None
                for kc in range(8):
                    ins = e.transpose(out=pT[:, kc * 128:(kc + 1) * 128],
                                      in_=xn[i % NXN][:, kc * 128:(kc + 1) * 128], identity=identb[:])
                return ins
            T(tr, reads=[('xn', i % NXN)], writes=[('ps', i % 4)])

        def st_ev(i):
            pT = bank_bf(i % 4)
            sc_, sh_ = (scAp, shAp) if i < 16 else (scA, shA)
            for kc in range(8):
                dst = HT[:, kc, i * 128:(i + 1) * 128]
                srcp = pT[:, kc * 128:(kc + 1) * 128]
                if i % 2 == 0:
                    A(lambda e, dst=dst, srcp=srcp, kc=kc: e.activation(
                        out=dst, in_=srcp, func=AF.Identity, bias=sh_[:, kc:kc + 1], scale=sc_[:, kc:kc + 1]),
                      reads=[('ps', i % 4)], writes=[('HTa', i)])
                else:
                    V(lambda e, dst=dst, srcp=srcp, kc=kc: e.tensor_scalar(
                        out=dst, in0=srcp, scalar1=sc_[:, kc:kc + 1], scalar2=sh_[:, kc:kc + 1],
                        op0=ALU.mult, op1=ALU.add),
                      reads=[('ps', i % 4)], writes=[('HTv', i)])
        st_dma(0)
        st_dma(1)
        stages = [(st_sq, 0), (st_ts, 1), (st_sqrt, 2), (st_xn, 3), (st_tr, 4), (st_ev, 5)]
        for j in range(32 + 5):
            if j + 2 < 32:
                st_dma(j + 2)
            for fn, lag in stages:
                i = j - lag
                if 0 <= i < 32:
                    fn(i)
        S.barrier()

    if dbg_cols:
        dma('gpsimd', dbg_d[:, 0:4096], HT[:, 0, :], semname="outg")
        dma('gpsimd', dbg_d[:, 4096:8192], HT[:, 5, :], semname="outg")
    if stage == 1:
        S.emit(final_waits=[k for k in (('D', 'out'), ('D', 'outg')) if k in S.cnt])
        mix.close()
        root.close()
        return nc

    loads = []
    for h in range(4):
        for k in range(4):
            loads.append((win1_d[h * 4 + k], 1))
        loads.append((win2_d[h * 2 + 0], 2))
        loads.append((win2_d[h * 2 + 1], 2))
    for hg in range(4):
        for g in range(3):
            for k in range(3):
                loads.append((win1_d[16 + (hg * 3 + g) * 3 + k], 1))
    wreto_v = wreto_d.rearrange("(kc p) n -> p kc n", p=128)
    watto_v = watto_d.rearrange("(kc p) n -> p kc n", p=128)
    for m in range(8):
        loads.append((win1_d[52 + m], 1))
        loads.append((win1_d[60 + m], 1))
        loads.append((wreto_v[:, :, m * 128:(m + 1) * 128], 1))
        loads.append((watto_v[:, :, m * 128:(m + 1) * 128], 3))
    ring = {'issued': 0, 'consumed': 0}
    N_MAIN_LOADS = 60

    def ring_issue():
        n = ring['issued']
        s = n % NSLOT
        src, kind = loads[n]
        if kind == 1:
            dst = WB[s][:, :, 0:128]
        elif kind == 2:
            dst = WB[s][:, :, :]
        else:
            dst = WB[s][:, 0:4, 0:128]
        dma('gpsimd', dst, src, writes=[('WB', s)], semname="WB%d" % s)
        ring['issued'] += 1

    def ring_get(hold=1):
        while ring['issued'] < min(N_MAIN_LOADS, ring['consumed'] + NSLOT - hold + 1):
            ring_issue()
        s = ring['consumed'] % NSLOT
        ring['consumed'] += 1
        return s

    pbank = [0]

    def next_pbank():
        b = pbank[0]
        pbank[0] = (pbank[0] + 1) % 4
        return b

    def fm_mm(bk, s, col0, ncols=512, nk=8, src=None, wcols=(0, 128)):
        src = HT if src is None else src

        def f(e):
            ins = None
            for kc in range(nk):
                ins = e.matmul(bank(bk)[:, 0:ncols], lhsT=WB[s][:, kc, wcols[0]:wcols[1]],
                               rhs=src[:, kc, col0:col0 + ncols], start=(kc == 0), stop=(kc == nk - 1))
            return ins
        return f

    def tm_mm(bk, s, tok_sl, ncols):
        def f(e):
            ins = None
            for kc in range(8):
                ins = e.matmul(bank(bk)[:, 0:ncols], lhsT=HT[:, kc, tok_sl], rhs=WB[s][:, kc, 0:ncols],
                               start=(kc == 0), stop=(kc == 7))
            return ins
        return f

    with contextlib.ExitStack() as rsc:
        ropeC = sb(rsc, "ropeC", [128, SEQ], BF16)
        ropeS = sb(rsc, "ropeS", [128, SEQ], BF16)
        Dm = sb(rsc, "Dm", [128, 4, 128], F32)
        rvec = sb(rsc, "rvec", [128, 16], F32)
        zeta2 = sb(rsc, "zeta2", [128, 4, 16], F32)
        qT = sb(rsc, "qT", [128, HALF], BF16)
        kT = sb(rsc, "kT", [128, SEQ], BF16)
        Vt = sb(rsc, "Vt", [128, 32, 256], BF16)
        SG = sb(rsc, "SG", [128, 16, 256], BF16)
        t1 = [sb(rsc, "t1_%d" % i, [128, 512], F32) for i in range(2)]
        t2 = [sb(rsc, "t2_%d" % i, [128, 512], F32) for i in range(2)]
        Kz = [sb(rsc, "Kz%d" % i, [128, 128], BF16) for i in range(2)]
        PT = [sb(rsc, "PT%d" % i, [128, 128], BF16) for i in range(2)]
        yI = [sb(rsc, "yI%d" % i, [128, 256], F32) for i in range(2)]
        yy = [sb(rsc, "yy%d" % i, [128, 256], F32) for i in range(2)]
        yn = [sb(rsc, "yn%d" % i, [128, 256], BF16) for i in range(2)]
        yg = [sb(rsc, "yg%d" % i, [128, 256], BF16) for i in range(2)]
        st6 = [sb(rsc, "st6_%d" % i, [128, 6], F32) for i in range(2)]
        mv = [sb(rsc, "mv%d" % i, [128, 2], F32) for i in range(2)]
        rstd = [sb(rsc, "rstd%d" % i, [128, 1], F32) for i in range(2)]
        nmr = [sb(rsc, "nmr%d" % i, [128, 1], F32) for i in range(2)]
        Rf = sb(rsc, "Rf", [128, 256], F32)
        Rb = sb(rsc, "Rb", [128, 256], BF16)
        dma('gpsimd', ropeC[:], ropec_d, writes=['ropeC'])
        dma('gpsimd', ropeS[:], ropes_d, writes=['ropeS'])
        dma('sync', Dm[:], dm_d, writes=['Dm'])
        dma('sync', rvec[:], rvec_d, writes=['rvec'])
        dma('sync', zeta2[:], zeta2_d, writes=['zeta2'])
        ecnt = [0]

        for h in range(4):
            sq, sqs, sk, sks = None, None, None, None
            for which in ('q', 'k'):
                sa = ring_get(1)
                sbw = ring_get(2)
                ntg = 4 if which == 'q' else 8
                for tg in range(ntg):
                    col0 = (HALF + tg * 512) if which == 'q' else tg * 512
                    bA, bB = next_pbank(), next_pbank()
                    T(fm_mm(bA, sa, col0), reads=[('WB', sa)], writes=[('ps', bA)])
                    T(fm_mm(bB, sbw, col0), reads=[('WB', sbw)], writes=[('ps', bB)])
                    par = ecnt[0] % 2
                    ecnt[0] += 1
                    V(lambda e, bA=bA, col0=col0, par=par: e.tensor_tensor(
                        out=t1[par][:], in0=bank(bA)[:, 0:512], in1=ropeC[:, col0:col0 + 512], op=ALU.mult),
                      reads=[('ps', bA), 'ropeC'], writes=[('t1', par)])
                    V(lambda e, bB=bB, col0=col0, par=par: e.tensor_tensor(
                        out=t2[par][:], in0=bank(bB)[:, 0:512], in1=ropeS[:, col0:col0 + 512], op=ALU.mult),
                      reads=[('ps', bB), 'ropeS'], writes=[('t2', par)])
                    if which == 'q':
                        dst = qT[:, tg * 512:(tg + 1) * 512]
                        key = ('qT', tg)
                    else:
                        dst = kT[:, tg * 512:(tg + 1) * 512]
                        key = ('kT', tg)
                    G(lambda e, dst=dst, par=par: e.tensor_tensor(out=dst, in0=t1[par][:], in1=t2[par][:], op=ALU.add),
                      reads=[('t1', par), ('t2', par)], writes=[key])
            sv = ring_get()
            for t in range(32):
                bk = next_pbank()
                T(tm_mm(bk, sv, slice(t * 128, (t + 1) * 128), 256), reads=[('WB', sv)], writes=[('ps', bk)])
                A(lambda e, bk=bk, t=t: e.activation(out=Vt[:, t, :], in_=bank(bk)[:, 0:256], func=AF.Copy),
                  reads=[('ps', bk)], writes=[('V', t)])
            sg_ = ring_get()
            for t in range(16):
                bk = next_pbank()
                T(tm_mm(bk, sg_, slice(HALF + t * 128, HALF + (t + 1) * 128), 256), reads=[('WB', sg_)],
                  writes=[('ps', bk)])
                A(lambda e, bk=bk, t=t: e.activation(out=SG[:, t, :], in_=bank(bk)[:, 0:256], func=AF.Silu),
                  reads=[('ps', bk)], writes=[('SG', t)])
            for n in range(16):
                p = n % 2
                ksl = slice(n * 128, (n + 1) * 128)
                T(lambda e, ksl=ksl: e.transpose(out=bank_bf(4)[:, 0:128], in_=kT[:, ksl], identity=identb[:]),
                  reads=[('kT', n // 4)], writes=[('ps', 4)])
                V(lambda e, p=p, h=h, n=n: e.tensor_scalar(out=Kz[p][:], in0=bank_bf(4)[:, 0:128],
                                                           scalar1=zeta2[:, h, n:n + 1], scalar2=None, op0=ALU.mult),
                  reads=[('ps', 4), 'zeta2'], writes=[('Kz', p)])
                T(lambda e, p=p, n=n: e.matmul(bank(2)[:, 0:256], lhsT=Kz[p][:], rhs=Vt[:, n, :],
                                               start=(n == 0), stop=(n == 15)),
                  reads=[('Kz', p), ('V', n)], writes=[('ps', 2)])
            V(lambda e: e.tensor_copy(out=Rb[:], in_=bank(2)[:, 0:256]), reads=[('ps', 2)], writes=['Rb'])
            V(lambda e: e.tensor_copy(out=Rf[:], in_=bank(2)[:, 0:256]), reads=[('ps', 2)], writes=['Rf'])
            for n in range(16, 32):
                own = True
                p = n % 2
                ksl = slice(n * 128, (n + 1) * 128)
                T(lambda e, ksl=ksl: e.transpose(out=bank_bf(4)[:, 0:128], in_=kT[:, ksl], identity=identb[:]),
                  reads=[('kT', n // 4)], writes=[('ps', 4)])
                V(lambda e, p=p, h=h: e.tensor_scalar(out=Kz[p][:], in0=bank_bf(4)[:, 0:128],
                                                      scalar1=rvec[:, h:h + 1], scalar2=None, op0=ALU.mult),
                  reads=[('ps', 4), 'rvec'], writes=[('Kz', p)])
                if own:
                    j = n - 16
                    qsl = slice(j * 128, (j + 1) * 128)
                    T(lambda e, ksl=ksl, qsl=qsl: e.matmul(bank(5)[:, 0:128], lhsT=kT[:, ksl], rhs=qT[:, qsl],
                                                           start=True, stop=True),
                      reads=[('kT', n // 4), ('qT', j // 4)], writes=[('ps', 5)])
                    V(lambda e, p=p, h=h: e.tensor_tensor(out=PT[p][:], in0=bank(5)[:, 0:128], in1=Dm[:, h, :],
                                                          op=ALU.mult),
                      reads=[('ps', 5), 'Dm'], writes=[('PT', p)])
                    bI = 6 + p
                    T(lambda e, p=p, n=n, bI=bI: e.matmul(bank(bI)[:, 0:256], lhsT=PT[p][:], rhs=Vt[:, n, :],
                                                          start=True, stop=True),
                      reads=[('PT', p), ('V', n)], writes=[('ps', bI)])
                    bC = 0 + p
                    T(lambda e, qsl=qsl, bC=bC: e.matmul(bank(bC)[:, 0:256], lhsT=qT[:, qsl], rhs=Rb[:],
                                                         start=True, stop=True),
                      reads=[('qT', j // 4), 'Rb'], writes=[('ps', bC)])
                    A(lambda e, p=p, bI=bI: e.activation(out=yI[p][:], in_=bank(bI)[:, 0:256], func=AF.Copy),
                      reads=[('ps', bI)], writes=[('yI', p)])
                    V(lambda e, p=p, bC=bC, h=h: e.scalar_tensor_tensor(
                        out=yy[p][:], in0=bank(bC)[:, 0:256], scalar=rvec[:, 4 + h:5 + h], in1=yI[p][:],
                        op0=ALU.mult, op1=ALU.add),
                      reads=[('ps', bC), ('yI', p), 'rvec'], writes=[('yy', p)])
                    V(lambda e, p=p: e.bn_stats(out=st6[p][:], in_=yy[p][:]), reads=[('yy', p)], writes=[('st6', p)])
                    V(lambda e, p=p: e.bn_aggr(out=mv[p][:], in_=st6[p][:]), reads=[('st6', p)], writes=[('mv', p)])
                    V(lambda e, p=p: e.tensor_scalar(out=rstd[p][:], in0=mv[p][:, 1:2], scalar1=GN_EPS, scalar2=None,
                                                     op0=ALU.add),
                      reads=[('mv', p)], writes=[('rstd', p)])
                    A(lambda e, p=p: e.activation(out=rstd[p][:], in_=rstd[p][:], func=AF.Sqrt),
                      reads=[('rstd', p)], writes=[('rstd', p)])
                    V(lambda e, p=p: e.reciprocal(out=rstd[p][:], in_=rstd[p][:]),
                      reads=[('rstd', p)], writes=[('rstd', p)])
                    V(lambda e, p=p: e.scalar_tensor_tensor(out=nmr[p][:], in0=mv[p][:, 0:1], scalar=-1.0,
                                                            in1=rstd[p][:], op0=ALU.mult, op1=ALU.mult),
                      reads=[('mv', p), ('rstd', p)], writes=[('nmr', p)])
                    A(lambda e, p=p: e.activation(out=yn[p][:], in_=yy[p][:], func=AF.Identity,
                                                  bias=nmr[p][:, 0:1], scale=rstd[p][:, 0:1]),
                      reads=[('yy', p), ('rstd', p), ('nmr', p)], writes=[('yn', p)])
                    G(lambda e, p=p, j=j: e.tensor_tensor(out=yg[p][:], in0=yn[p][:], in1=SG[:, j, :], op=ALU.mult),
                      reads=[('yn', p), ('SG', j)], writes=[('yg', p)])

                    def trY(e, p=p):
                        ins = None
                        for dc in range(2):
                            ins = e.transpose(out=bank_bf(3)[:, dc * 128:(dc + 1) * 128],
                                              in_=yg[p][:, dc * 128:(dc + 1) * 128], identity=identb[:])
                        return ins
                    T(trY, reads=[('yg', p)], writes=[('ps', 3)])
                    for dc in range(2):
                        A(lambda e, h=h, dc=dc, qsl=qsl: e.activation(
                            out=RetT[:, h * 2 + dc, qsl], in_=bank_bf(3)[:, dc * 128:(dc + 1) * 128], func=AF.Copy),
                          reads=[('ps', 3)], writes=[('RetT', j)])
                if n < 31:
                    T(lambda e, p=p, n=n: e.matmul(bank(2)[:, 0:256], lhsT=Kz[p][:], rhs=Vt[:, n, :],
                                                   start=True, stop=True),
                      reads=[('Kz', p), ('V', n)], writes=[('ps', 2)])
                    V(lambda e, h=h: e.scalar_tensor_tensor(out=Rf[:], in0=Rf[:], scalar=rvec[:, 8 + h:9 + h],
                                                            in1=bank(2)[:, 0:256], op0=ALU.mult, op1=ALU.add),
                      reads=['Rf', ('ps', 2), 'rvec'], writes=['Rf'])
                    A(lambda e: e.activation(out=Rb[:], in_=Rf[:], func=AF.Copy), reads=['Rf'], writes=['Rb'])
        S.barrier()

    if dbg_cols:
        for kc in range(8):
            dma('gpsimd', dbg_d[:, 8192 + kc * HALF:8192 + (kc + 1) * HALF], RetT[:, kc, :], semname="outg")
    if stage == 2:
        S.emit(final_waits=[k for k in (('D', 'out'), ('D', 'outg')) if k in S.cnt])
        mix.close()
        root.close()
        return nc

    AttT = sb(mix, "AttT", [128, 4, HALF], BF16)
    with contextlib.ExitStack() as asc:
        EB = sb(asc, "EB", [128, 12, 256], BF16)
        EBm = sb(asc, "EBm", [128, 12, 256], BF16)
        with contextlib.ExitStack() as ebs:
            relb = sb(ebs, "relb", [32, 12], F32)
            ohf = sb(ebs, "ohf", [32, 3, 384], F32)
            jf = sb(ebs, "jf", [128, 128], F32)
            Fsb = sb(ebs, "Fsb", [4, 3, 384], F32)
            Gh = [sb(ebs, "Gh%d" % i, [128, 256], F32) for i in range(2)]
            dma('sync', relb[:], relb_d, writes=['relb'])
            dma('sync', ohf[:], ohf_d, writes=['ohf'])
            dma('sync', jf[:], jf_d, writes=['jf'])
            A(lambda e: e.activation(out=relb[:], in_=relb[:], func=AF.Exp), reads=['relb'], writes=['relb'])
            for g in range(3):
                T(lambda e, g=g: e.matmul(bank(0)[0:4, 0:384], lhsT=relb[:, g * 4:(g + 1) * 4], rhs=ohf[:, g, :],
                                          start=True, stop=True),
                  reads=['relb', 'ohf'], writes=[('ps', 0)])
                V(lambda e, g=g: e.tensor_copy(out=Fsb[:, g, :], in_=bank(0)[0:4, 0:384]),
                  reads=[('ps', 0)], writes=['Fsb'])
            dma('sync', fd_d.rearrange("(g h) m -> h g m", g=3), Fsb[:], reads=['Fsb'], writes=['fd'])
            for idx in range(12):
                hk = bass.AP(tensor=fd_t, offset=idx * 384, ap=[[1, 128], [1, 256]])
                dma('sync', Gh[idx % 2][:], hk, reads=['fd'], writes=[('Gh', idx % 2)], semname="Gh%d" % (idx % 2))
                bk = 1 + idx % 2
                T(lambda e, idx=idx, bk=bk: e.matmul(bank(bk)[:, 0:256], lhsT=jf[:], rhs=Gh[idx % 2][:],
                                                     start=True, stop=True),
                  reads=['jf', ('Gh', idx % 2)], writes=[('ps', bk)])
                V(lambda e, idx=idx, bk=bk: e.tensor_copy(out=EB[:, idx, :], in_=bank(bk)[:, 0:256]),
                  reads=[('ps', bk)], writes=[('EBa', idx)])
                V(lambda e, idx=idx, bk=bk: e.tensor_copy(out=EBm[:, idx, 0:128], in_=bank(bk)[:, 0:128]),
                  reads=[('ps', bk)], writes=[('EBb', idx)])
                V(lambda e, idx=idx, bk=bk: e.tensor_scalar(out=EBm[:, idx, 128:256], in0=bank(bk)[:, 128:256],
                                                            scalar1=flag[:, 0:1], scalar2=None, op0=ALU.mult),
                  reads=[('ps', bk)], writes=[('EBv', idx)])
            S.barrier()

        if dbg_cols:
            for idx in range(12):
                dma('gpsimd', dbg_d[:, 24576 + idx * 256:24576 + (idx + 1) * 256], EB[:, idx, :], semname="outg")
        if stage == 25:
            S.emit(final_waits=[k for k in (('D', 'out'), ('D', 'outg')) if k in S.cnt])
            asc.close()
            mix.close()
            root.close()
            return nc

        qa = [sb(asc, "qa%d" % i, [128, HALF], BF16) for i in range(2)]
        ka = [sb(asc, "ka%d" % i, [128, SEQ], BF16) for i in range(2)]
        VA = sb(asc, "VA", [128, 32, 128], BF16)
        ND = sb(asc, "ND", [128, 2, HALF], F32)
        Et = [sb(asc, "Et%d" % i, [128, 256], F32) for i in range(2)]
        Pt = [sb(asc, "Pt%d" % i, [128, 256], BF16) for i in range(2)]
        blkc = [0]
        items = [(hg, g) for hg in range(4) for g in range(3)]

        def qk_ops(ii):
            hg, g = items[ii]
            par = ii % 2
            qT_, kT_ = qa[par], ka[par]
            ops = []
            st = {}

            def q_op(tg):
                def f():
                    if 'sq' not in st:
                        st['sq'] = ring_get(1)
                    s_q = st['sq']
                    bk = next_pbank()
                    T(fm_mm(bk, s_q, HALF + tg * 512), reads=[('WB', s_q)], writes=[('ps', bk)])
                    A(lambda e: e.activation(out=qT_[:, tg * 512:(tg + 1) * 512], in_=bank(bk)[:, 0:512],
                                             func=AF.Copy, scale=float(128 ** -0.5)),
                      reads=[('ps', bk)], writes=[('qa', par)])
                return f

            def k_op(tg):
                def f():
                    if 'sk' not in st:
                        st['sk'] = ring_get(1)
                    s_k = st['sk']
                    bk = next_pbank()
                    T(fm_mm(bk, s_k, tg * 512), reads=[('WB', s_k)], writes=[('ps', bk)])
                    V(lambda e: e.tensor_copy(out=kT_[:, tg * 512:(tg + 1) * 512], in_=bank(bk)[:, 0:512]),
                      reads=[('ps', bk)], writes=[('ka', par)])
                return f
            for tg in range(4):
                ops.append(q_op(tg))
            for tg in (list(range(8)) if g == 2 else list(range(3, 8))):
                ops.append(k_op(tg))
            return ops

        def v_proj(ii):
            hg, g = items[ii]
            w, r = ATT_PATTERNS[g]
            nbb = 16 // r
            s_v = ring_get(1)
            vblocks = []
            for bb in range(nbb):
                for c in range(r):
                    vblocks.append((bb * r + c, HALF + 128 * r * bb + c))
            for c in range(r):
                vblocks.append((16 + c, 128 * r * (nbb - 1) + c))
            for vi, (idx, st0) in enumerate(vblocks):
                bk = next_pbank()
                tsl = slice(st0, st0 + 127 * r + 1, r)
                T(tm_mm(bk, s_v, tsl, 128), reads=[('WB', s_v)], writes=[('ps', bk)])
                if vi % 2 == 0:
                    A(lambda e, bk=bk, idx=idx: e.activation(out=VA[:, idx, :], in_=bank(bk)[:, 0:128],
                                                             func=AF.Copy),
                      reads=[('ps', bk)], writes=[('VAa', idx)])
                else:
                    V(lambda e, bk=bk, idx=idx: e.tensor_copy(out=VA[:, idx, :], in_=bank(bk)[:, 0:128]),
                      reads=[('ps', bk)], writes=[('VAv', idx)])

        def block_ops(ii):
            hg, g = items[ii]
            w, r = ATT_PATTERNS[g]
            nbb = 16 // r
            par = ii % 2
            qT_, kT_ = qa[par], ka[par]
            ebi = g * 4 + hg
            ops = []
            for bb in range(nbb):
                for c in range(r):
                    def f(bb=bb, c=c):
                        bp = blkc[0] % 2
                        blkc[0] += 1
                        q0 = 128 * r * bb + c
                        qsl = slice(q0, q0 + 127 * r + 1, r)
                        o0 = HALF + q0
                        osl = slice(o0, o0 + 127 * r + 1, r)
                        if bb > 0:
                            p0 = o0 - 128 * r
                            pidx = (bb - 1) * r + c
                        else:
                            p0 = 128 * r * (nbb - 1) + c
                            pidx = 16 + c
                        psl = slice(p0, p0 + 127 * r + 1, r)
                        oidx = bb * r + c
                        bS = 4 + bp
                        bN = 6 + bp

                        def mmS(e):
                            e.matmul(bank(bS)[:, 0:128], lhsT=kT_[:, osl], rhs=qT_[:, qsl], start=True, stop=True)
                            return e.matmul(bank(bS)[:, 128:256], lhsT=kT_[:, psl], rhs=qT_[:, qsl],
                                            start=True, stop=True)
                        T(mmS, reads=[('ka', par), ('qa', par)], writes=[('ps', bS)])
                        A(lambda e: e.activation(out=Et[bp][:], in_=bank(bS)[:, 0:256], func=AF.Exp),
                          reads=[('ps', bS)], writes=[('Et', bp)])
                        EBt = EBm if bb == 0 else EB
                        V(lambda e: e.tensor_tensor(out=Pt[bp][:], in0=Et[bp][:], in1=EBt[:, ebi, :], op=ALU.mult),
                          reads=[('Et', bp)], writes=[('Pt', bp)])

                        def mmN(e):
                            e.matmul(bank(bN)[:, 0:128], lhsT=VA[:, oidx, :], rhs=Pt[bp][:, 0:128],
                                     start=True, stop=False)
                            e.matmul(bank(bN)[:, 0:128], lhsT=VA[:, pidx, :], rhs=Pt[bp][:, 128:256],
                                     start=False, stop=True)
                            e.matmul(bank(bN)[:, 128:256], lhsT=onesb[:], rhs=Pt[bp][:, 0:128],
                                     start=True, stop=False)
                            return e.matmul(bank(bN)[:, 128:256], lhsT=onesb[:], rhs=Pt[bp][:, 128:256],
                                            start=False, stop=True)
                        T(mmN, reads=[('Pt', bp), ('VAa', oidx), ('VAv', oidx), ('VAa', pidx), ('VAv', pidx)],
                          writes=[('ps', bN)])
                        src3 = bank(bN)[:, 0:256].rearrange("p (a q) -> p a q", a=2)
                        if g == 0:
                            V(lambda e: e.tensor_copy(out=ND[:, :, qsl], in_=src3),
                              reads=[('ps', bN)], writes=['ND'])
                        else:
                            V(lambda e: e.tensor_tensor(out=ND[:, :, qsl], in0=src3, in1=ND[:, :, qsl], op=ALU.add),
                              reads=[('ps', bN), 'ND'], writes=['ND'])
                    ops.append(f)
            return ops

        for f in qk_ops(0):
            f()
        v_proj(0)
        for ii in range(len(items)):
            hg, g = items[ii]
            blks = block_ops(ii)
            nxt = qk_ops(ii + 1) if ii + 1 < len(items) else []
            ni = 0
            for bi, f in enumerate(blks):
                f()
                want = (len(nxt) * (bi + 1)) // len(blks)
                while ni < want:
                    nxt[ni]()
                    ni += 1
            while ni < len(nxt):
                nxt[ni]()
                ni += 1
            if g == 2:
                V(lambda e: e.reciprocal(out=ND[:, 1, :], in_=ND[:, 1, :]), reads=['ND'], writes=['ND'])
                V(lambda e, hg=hg: e.tensor_tensor(out=AttT[:, hg, :], in0=ND[:, 0, :], in1=ND[:, 1, :], op=ALU.mult),
                  reads=['ND'], writes=['AttT'])
            if ii + 1 < len(items):
                v_proj(ii + 1)
        S.barrier()

    if dbg_cols:
        for kc in range(4):
            dma('gpsimd', dbg_d[:, 27648 + kc * HALF:27648 + (kc + 1) * HALF], AttT[:, kc, :], semname="outg")
    if stage == 3:
        S.emit(final_waits=[k for k in (('D', 'out'), ('D', 'outg')) if k in S.cnt])
        mix.close()
        root.close()
        return nc

    mrg = contextlib.ExitStack()
    mrg.__enter__()
    MergedT = sb(mrg, "MergedT", [128, 8, HALF], BF16, side="right")
    with contextlib.ExitStack() as msc:
        sgA = [sb(msc, "sgA%d" % i, [128, 512], F32) for i in range(2)]
        sgB = [sb(msc, "sgB%d" % i, [128, 512], F32) for i in range(2)]
        m1 = [sb(msc, "m1_%d" % i, [128, 512], F32) for i in range(2)]
        m2 = [sb(msc, "m2_%d" % i, [128, 512], F32) for i in range(2)]
        WBm = WB + [sb(msc, "WBm%d" % i, [128, 8, 256], BF16) for i in range(4)]
        mloads = loads[N_MAIN_LOADS:]
        assert len(mloads) == 32 and ring['issued'] == ring['consumed'] == N_MAIN_LOADS
        mr = {'issued': 0, 'consumed': 0}

        def mring_get(hold):
            while mr['issued'] < min(len(mloads), mr['consumed'] + 8 - hold + 1):
                n = mr['issued']
                s = n % 8
                src_, kind = mloads[n]
                dst = WBm[s][:, :, 0:128] if kind == 1 else WBm[s][:, 0:4, 0:128]
                dma('gpsimd', dst, src_, writes=[('WBm', s)], semname="WBm%d" % s)
                mr['issued'] += 1
            s = mr['consumed'] % 8
            mr['consumed'] += 1
            return s
        WB_save = WB

        def fm_mm_m(bk, s, col0, nk=8, src=None):
            src = HT if src is None else src

            def f(e):
                ins = None
                for kc in range(nk):
                    ins = e.matmul(bank(bk)[:, 0:512], lhsT=WBm[s][:, kc, 0:128],
                                   rhs=src[:, kc, col0:col0 + 512], start=(kc == 0), stop=(kc == nk - 1))
                return ins
            return f
        it = 0
        for m in range(8):
            s_ga = mring_get(1)
            s_gb = mring_get(2)
            s_wr = mring_get(3)
            s_wa = mring_get(4)
            for tg in range(4):
                p = it % 2
                it += 1
                bs = 0 if p == 0 else 4
                T(fm_mm_m(bs + 0, s_ga, HALF + tg * 512), reads=[('WBm', s_ga)], writes=[('ps', bs + 0)])
                T(fm_mm_m(bs + 1, s_gb, HALF + tg * 512), reads=[('WBm', s_gb)], writes=[('ps', bs + 1)])
                T(fm_mm_m(bs + 2, s_wr, tg * 512, src=RetT), reads=[('WBm', s_wr)], writes=[('ps', bs + 2)])
                T(fm_mm_m(bs + 3, s_wa, tg * 512, nk=4, src=AttT), reads=[('WBm', s_wa)], writes=[('ps', bs + 3)])
                A(lambda e, p=p, bs=bs: e.activation(out=sgA[p][:], in_=bank(bs)[:, 0:512], func=AF.Sigmoid),
                  reads=[('ps', bs)], writes=[('sgA', p)])
                A(lambda e, p=p, bs=bs: e.activation(out=sgB[p][:], in_=bank(bs + 1)[:, 0:512], func=AF.Sigmoid),
                  reads=[('ps', bs + 1)], writes=[('sgB', p)])
                V(lambda e, p=p, bs=bs: e.tensor_tensor(out=m1[p][:], in0=bank(bs + 2)[:, 0:512], in1=sgA[p][:],
                                                        op=ALU.mult),
                  reads=[('ps', bs + 2), ('sgA', p)], writes=[('m1', p)])
                V(lambda e, p=p, bs=bs: e.tensor_tensor(out=m2[p][:], in0=bank(bs + 3)[:, 0:512], in1=sgB[p][:],
                                                        op=ALU.mult),
                  reads=[('ps', bs + 3), ('sgB', p)], writes=[('m2', p)])
                G(lambda e, p=p, m=m, tg=tg: e.tensor_tensor(out=MergedT[:, m, tg * 512:(tg + 1) * 512],
                                                             in0=m1[p][:], in1=m2[p][:], op=ALU.add),
                  reads=[('m1', p), ('m2', p)], writes=[('MergedT', m, tg)])
        S.barrier()
    mix.close()

    X = sb(root, "X", [128, NT, D], F32)
    with contextlib.ExitStack() as p3:
        WO = sb(p3, "WO", [128, 8, D], BF16)
        wst = [sb(p3, "wst%d" % i, [128, D], F32) for i in range(2)]
        for t in range(NT):
            dma('sync', X[:, t, :], x_own[t * 128:(t + 1) * 128, :], writes=[('X', t, 0), ('X', t, 1)])
        for kc in range(8):
            dma('sync', wst[kc % 2][:], wo_d[kc * 128:(kc + 1) * 128, :], writes=[('wst', kc % 2)],
                semname="wst%d" % (kc % 2))
            G(lambda e, kc=kc: e.tensor_tensor(out=WO[:, kc, :], in0=wst[kc % 2][:], in1=gtA_bc[:], op=ALU.mult),
              reads=[('wst', kc % 2)], writes=['WO'])
        for t in range(NT):
            for nh in range(2):
                bk = next_pbank()

                def mmo(e, bk=bk, t=t, nh=nh):
                    ins = None
                    for kc in range(8):
                        ins = e.matmul(bank(bk)[:, 0:512], lhsT=MergedT[:, kc, t * 128:(t + 1) * 128],
                                       rhs=WO[:, kc, nh * 512:(nh + 1) * 512], start=(kc == 0), stop=(kc == 7))
                    return ins
                T(mmo, reads=['WO'], writes=[('ps', bk)])
                V(lambda e, bk=bk, t=t, nh=nh: e.tensor_tensor(out=X[:, t, nh * 512:(nh + 1) * 512],
                                                               in0=bank(bk)[:, 0:512],
                                                               in1=X[:, t, nh * 512:(nh + 1) * 512], op=ALU.add),
                  reads=[('ps', bk), ('X', t, nh)], writes=[('X', t, nh)])
        S.barrier()

    mrg.close()

    if dbg_cols:
        for t in range(NT):
            dma('sync', dbg_d[:, 35840 + t * D:35840 + (t + 1) * D], X[:, t, :], semname="out")
    if stage == 4:
        S.emit(final_waits=[k for k in (('D', 'out'), ('D', 'outg')) if k in S.cnt])
        root.close()
        return nc

    H2T = sb(root, "H2T", [128, 8, HALF], BF16)
    Gt = sb(root, "Gt", [128, NT, N_EXP], F32)
    with contextlib.ExitStack() as p4:
        xnf = [sb(p4, "xnf%d" % i, [128, D], F32) for i in range(3)]
        h2f = [sb(p4, "h2f%d" % i, [128, 8, 128], F32) for i in range(3)]
        junk4 = sb(p4, "junk4", [128, D], BF16)
        WR = sb(p4, "WR", [128, 8, N_EXP], F32)
        brbc = sb(p4, "brbc", [128, N_EXP], F32)
        Lg = [sb(p4, "Lg%d" % i, [128, N_EXP], F32) for i in range(2)]
        m8 = [sb(p4, "m8_%d" % i, [128, 8], F32) for i in range(2)]
        ngm = [sb(p4, "ngm%d" % i, [128, 1], F32) for i in range(2)]
        msk = [sb(p4, "msk%d" % i, [128, N_EXP], F32) for i in range(2)]
        Ex = [sb(p4, "Ex%d" % i, [128, N_EXP], F32) for i in range(2)]
        sm = [sb(p4, "sm%d" % i, [128, 1], F32) for i in range(2)]
        dma('sync', WR[:], wr_d.rearrange("(kc p) n -> p kc n", p=128), writes=['WR'])
        dma('sync', brbc[:], brbc_d, writes=['brbc'])
        NP4 = 3

        def s4_sq(t):
            i = 32 + t
            A(lambda e: e.activation(out=junk4[:], in_=X[:, t, :], func=AF.Square, accum_out=ss[:, i:i + 1]),
              reads=[('X', t, 0), ('X', t, 1)], writes=['junk4', ('ss', i)])

        def s4_ts(t):
            i = 32 + t
            V(lambda e: e.tensor_scalar(out=rs[:, i:i + 1], in0=ss[:, i:i + 1], scalar1=1.0 / D, scalar2=EPS,
                                        op0=ALU.mult, op1=ALU.add),
              reads=[('ss', i)], writes=[('rs', i)])

        def s4_sqrt(t):
            i = 32 + t
            A(lambda e: e.activation(out=rs[:, i:i + 1], in_=rs[:, i:i + 1], func=AF.Sqrt),
              reads=[('rs', i)], writes=[('rs', i)])

        def s4_xn(t):
            i = 32 + t
            p = t % NP4
            V(lambda e: e.reciprocal(out=rs[:, i:i + 1], in_=rs[:, i:i + 1]),
              reads=[('rs', i)], writes=[('rs', i)])
            V(lambda e: e.tensor_scalar(out=xnf[p][:], in0=X[:, t, :], scalar1=rs[:, i:i + 1],
                                        scalar2=None, op0=ALU.mult),
              reads=[('X', t, 0), ('X', t, 1), ('rs', i)], writes=[('xnf', p)])

        def s4_b0(t):
            return (0, 2, 6)[t % NP4]

        def s4_tr(t):
            p = t % NP4
            b0 = s4_b0(t)

            def tr2(e):
                ins = None
                for kc in range(8):
                    ins = e.transpose(out=PS[:, b0 + kc // 4, (kc % 4) * 128:(kc % 4 + 1) * 128],
                                      in_=xnf[p][:, kc * 128:(kc + 1) * 128], identity=identf[:])
                return ins
            T(tr2, reads=[('xnf', p)], writes=[('ps', b0), ('ps', b0 + 1)])

        def s4_ev(t):
            p = t % NP4
            b0 = s4_b0(t)
            for kc in range(8):
                srcp = PS[:, b0 + kc // 4, (kc % 4) * 128:(kc % 4 + 1) * 128]
                if t % 2 == 0:
                    A(lambda e, srcp=srcp, kc=kc: e.activation(out=h2f[p][:, kc, :], in_=srcp, func=AF.Identity,
                                                               bias=shF[:, kc:kc + 1], scale=scF[:, kc:kc + 1]),
                      reads=[('ps', b0), ('ps', b0 + 1)], writes=[('h2fa', p)])
                else:
                    V(lambda e, srcp=srcp, kc=kc: e.tensor_scalar(out=h2f[p][:, kc, :], in0=srcp,
                                                                  scalar1=scF[:, kc:kc + 1],
                                                                  scalar2=shF[:, kc:kc + 1],
                                                                  op0=ALU.mult, op1=ALU.add),
                      reads=[('ps', b0), ('ps', b0 + 1)], writes=[('h2fv', p)])

        def s4_rt(t):
            p = t % NP4
            G(lambda e: e.tensor_copy(out=H2T[:, :, t * 128:(t + 1) * 128], in_=h2f[p][:]),
              reads=[('h2fa', p), ('h2fv', p)], writes=[('H2T', t)])
            bl = 4 + t % 2

            def mmr(e):
                ins = None
                for kc in range(8):
                    ins = e.matmul(bank(bl)[:, 0:N_EXP], lhsT=h2f[p][:, kc, :], rhs=WR[:, kc, :],
                                   start=(kc == 0), stop=(kc == 7))
                return ins
            T(mmr, reads=[('h2fa', p), ('h2fv', p), 'WR'], writes=[('ps', bl)])

        def s4_gate(t):
            q = t % 2
            bl = 4 + t % 2
            V(lambda e: e.tensor_tensor(out=Lg[q][:], in0=bank(bl)[:, 0:N_EXP], in1=brbc[:], op=ALU.add),
              reads=[('ps', bl), 'brbc'], writes=[('Lg', q)])
            V(lambda e: e.max(out=m8[q][:], in_=Lg[q][:]), reads=[('Lg', q)], writes=[('m8', q)])
            V(lambda e: e.tensor_scalar(out=ngm[q][:], in0=m8[q][:, 0:1], scalar1=-1.0, scalar2=None, op0=ALU.mult),
              reads=[('m8', q)], writes=[('ngm', q)])
            V(lambda e: e.tensor_scalar(out=msk[q][:], in0=Lg[q][:], scalar1=m8[q][:, 3:4], scalar2=None,
                                        op0=ALU.is_ge),
              reads=[('Lg', q), ('m8', q)], writes=[('msk', q)])
            A(lambda e: e.activation(out=Ex[q][:], in_=Lg[q][:], func=AF.Exp, bias=ngm[q][:, 0:1], scale=1.0),
              reads=[('Lg', q), ('ngm', q)], writes=[('Ex', q)])

        def s4_gate2(t):
            q = t % 2
            V(lambda e: e.tensor_tensor(out=Ex[q][:], in0=Ex[q][:], in1=msk[q][:], op=ALU.mult),
              reads=[('Ex', q), ('msk', q)], writes=[('Ex', q)])
            V(lambda e: e.tensor_reduce(out=sm[q][:], in_=Ex[q][:], axis=AX.X, op=ALU.add),
              reads=[('Ex', q)], writes=[('sm', q)])
            V(lambda e: e.reciprocal(out=sm[q][:], in_=sm[q][:]), reads=[('sm', q)], writes=[('sm', q)])
            V(lambda e: e.tensor_scalar(out=Gt[:, t, :], in0=Ex[q][:], scalar1=sm[q][:, 0:1], scalar2=None,
                                        op0=ALU.mult),
              reads=[('Ex', q), ('sm', q)], writes=[('Gt', t)])
        stages4 = [(s4_sq, 0), (s4_ts, 1), (s4_sqrt, 2), (s4_xn, 3), (s4_tr, 4), (s4_ev, 5), (s4_rt, 6),
                   (s4_gate, 7), (s4_gate2, 8)]
        for j in range(NT + 8):
            for fn, lag in stages4:
                t = j - lag
                if 0 <= t < NT:
                    fn(t)
        S.barrier()

    if dbg_cols:
        dma('sync', dbg_d[:, 52224:52224 + NT * N_EXP], Gt[:].rearrange("p t e -> p (t e)"), semname="out")
        dma('gpsimd', dbg_d[:, 52736:52736 + HALF], H2T[:, 0, :], semname="outg")
    if stage == 5:
        S.emit(final_waits=[k for k in (('D', 'out'), ('D', 'outg')) if k in S.cnt])
        root.close()
        return nc

    with contextlib.ExitStack() as p5:
        aT = sb(p5, "aT", [128, 8, HALF], BF16)
        W2 = sb(p5, "W2", [128, 8, D], BF16)
        W1 = [sb(p5, "W1_%d" % i, [128, 8, 256], BF16) for i in range(3)]
        w2st = [sb(p5, "w2st%d" % i, [128, D], F32) for i in range(2)]
        b1 = sb(p5, "b1", [128, N_EXP, 8, 2], F32)
        tA = [sb(p5, "tA%d" % i, [128, 512], F32) for i in range(2)]
        tB = [sb(p5, "tB%d" % i, [128, 512], F32) for i in range(2)]
        tC = [sb(p5, "tC%d" % i, [128, 512], F32) for i in range(2)]
        tD = [sb(p5, "tD%d" % i, [128, 512], F32) for i in range(2)]
        dma('sync', b1[:], b1r_d, writes=['b1'])
        n_exp_run = N_EXP
        w1n = [0]
        total_w1 = n_exp_run * 8

        def w1_issue():
            n = w1n[0]
            e_, fc_ = n // 8, n % 8
            s = n % 3
            dma('gpsimd', W1[s][:], w1r_d[e_, fc_], writes=[('W1', s)], semname="W1_%d" % s)
            w1n[0] += 1
        w1c = [0]

        def w1_get():
            while w1n[0] < min(total_w1, w1c[0] + 3):
                w1_issue()
            s = w1c[0] % 3
            w1c[0] += 1
            return s
        ac = [0]
        sc_ = [0]
        for ex in range(n_exp_run):
            def w2_dma(fc2, ex=ex):
                q = (ex * 8 + fc2) % 2
                dma('sync', w2st[q][:], w2_d[ex, fc2 * 128:(fc2 + 1) * 128, :], writes=[('w2st', q)],
                    semname="w2st%d" % q)

            def w2_chunk(fc2, ex=ex):
                q = (ex * 8 + fc2) % 2
                if fc2 + 1 < 8:
                    w2_dma(fc2 + 1)
                V(lambda e: e.tensor_tensor(out=W2[:, fc2, :], in0=w2st[q][:], in1=gtF_bc[:], op=ALU.mult),
                  reads=[('w2st', q)], writes=['W2'])
            w2_dma(0)
            for fc in range(8):
                s = w1_get()
                for tg in range(4):
                    p = ac[0] % 2
                    ac[0] += 1
                    bg, bl_ = (0, 1) if p == 0 else (2, 3)

                    def mm1(e, s=s, tg=tg, bg=bg, bl_=bl_):
                        ins = None
                        for kc in range(8):
                            e.matmul(bank(bg)[:, 0:512], lhsT=W1[s][:, kc, 0:128],
                                     rhs=H2T[:, kc, tg * 512:(tg + 1) * 512], start=(kc == 0), stop=(kc == 7))
                        for kc in range(8):
                            ins = e.matmul(bank(bl_)[:, 0:512], lhsT=W1[s][:, kc, 128:256],
                                           rhs=H2T[:, kc, tg * 512:(tg + 1) * 512], start=(kc == 0), stop=(kc == 7))
                        return ins
                    T(mm1, reads=[('W1', s)], writes=[('ps', bg), ('ps', bl_)])
                    V(lambda e, p=p, bg=bg, ex=ex, fc=fc: e.tensor_scalar(
                        out=tA[p][:], in0=bank(bg)[:, 0:512], scalar1=b1[:, ex, fc, 0:1], scalar2=7.0,
                        op0=ALU.add, op1=ALU.min),
                      reads=[('ps', bg), 'b1'], writes=[('tA', p)])
                    A(lambda e, p=p: e.activation(out=tB[p][:], in_=tA[p][:], func=AF.Sigmoid, scale=1.702),
                      reads=[('tA', p)], writes=[('tB', p)])
                    V(lambda e, p=p, bl_=bl_, ex=ex, fc=fc: e.tensor_scalar(
                        out=tC[p][:], in0=bank(bl_)[:, 0:512], scalar1=b1[:, ex, fc, 1:2], scalar2=7.0,
                        op0=ALU.add, op1=ALU.min),
                      reads=[('ps', bl_), 'b1'], writes=[('tC', p)])
                    V(lambda e, p=p: e.tensor_scalar(out=tC[p][:], in0=tC[p][:], scalar1=-7.0, scalar2=1.0,
                                                     op0=ALU.max, op1=ALU.add),
                      reads=[('tC', p)], writes=[('tC', p)])
                    G(lambda e, p=p: e.tensor_tensor(out=tD[p][:], in0=tA[p][:], in1=tB[p][:], op=ALU.mult),
                      reads=[('tA', p), ('tB', p)], writes=[('tD', p)])
                    G(lambda e, p=p, fc=fc, tg=tg: e.tensor_tensor(out=aT[:, fc, tg * 512:(tg + 1) * 512],
                                                                   in0=tD[p][:], in1=tC[p][:], op=ALU.mult),
                      reads=[('tD', p), ('tC', p)], writes=[('aT', tg)])
                w2_chunk(fc)
            for t in range(NT):
                for nh in range(2):
                    bk = 4 + sc_[0] % 2
                    sc_[0] += 1

                    def mm2(e, bk=bk, t=t, nh=nh):
                        ins = None
                        for fc in range(8):
                            ins = e.matmul(bank(bk)[:, 0:512], lhsT=aT[:, fc, t * 128:(t + 1) * 128],
                                           rhs=W2[:, fc, nh * 512:(nh + 1) * 512], start=(fc == 0), stop=(fc == 7))
                        return ins
                    T(mm2, reads=[('aT', t // 4), 'W2'], writes=[('ps', bk)])
                    V(lambda e, bk=bk, t=t, nh=nh, ex=ex: e.scalar_tensor_tensor(
                        out=X[:, t, nh * 512:(nh + 1) * 512], in0=bank(bk)[:, 0:512], scalar=Gt[:, t, ex:ex + 1],
                        in1=X[:, t, nh * 512:(nh + 1) * 512], op0=ALU.mult, op1=ALU.add),
                      reads=[('ps', bk), ('X', t, nh)], writes=[('X', t, nh)])
        S.barrier()

    with contextlib.ExitStack() as p5b:
        B2s = sb(p5b, "B2s", [N_EXP, D], F32)
        GT = sb(p5b, "GT", [N_EXP, NT, 128], F32)
        dma('sync', B2s[:], b2_d, writes=['B2s'])
        V(lambda e: e.tensor_tensor(out=B2s[:], in0=B2s[:], in1=gtF_bc[0:N_EXP, :], op=ALU.mult),
          reads=['B2s'], writes=['B2s'])
        for t in range(NT):
            bk = 6 + t % 2
            T(lambda e, t=t, bk=bk: e.transpose(out=bank(bk)[0:N_EXP, 0:128], in_=Gt[:, t, :], identity=identf[:]),
              writes=[('ps', bk)])
            V(lambda e, t=t, bk=bk: e.tensor_copy(out=GT[:, t, :], in_=bank(bk)[0:N_EXP, 0:128]),
              reads=[('ps', bk)], writes=[('GT', t)])
        for t in range(NT):
            for nh in range(2):
                bk = next_pbank()
                T(lambda e, t=t, nh=nh, bk=bk: e.matmul(bank(bk)[:, 0:512], lhsT=GT[:, t, :],
                                                        rhs=B2s[:, nh * 512:(nh + 1) * 512], start=True, stop=True),
                  reads=[('GT', t), 'B2s'], writes=[('ps', bk)])
                V(lambda e, t=t, nh=nh, bk=bk: e.tensor_tensor(out=X[:, t, nh * 512:(nh + 1) * 512],
                                                               in0=bank(bk)[:, 0:512],
                                                               in1=X[:, t, nh * 512:(nh + 1) * 512], op=ALU.add),
                  reads=[('ps', bk), ('X', t, nh)], writes=[('X', t, nh)])
        S.barrier()

    with contextlib.ExitStack() as p6:
        gfin = sb(p6, "gfin", [128, D], F32)
        ot = [sb(p6, "ot%d" % i, [128, D], F32) for i in range(3)]
        junk6 = sb(p6, "junk6", [128, D], BF16)
        dma('sync', gfin[:], gfin_d, writes=['gfin'])
        NO6 = 3

        def s6_sq(t):
            i = 48 + t
            A(lambda e: e.activation(out=junk6[:], in_=X[:, t, :], func=AF.Square, accum_out=ss[:, i:i + 1]),
              reads=[('X', t, 0), ('X', t, 1)], writes=['junk6', ('ss', i)])

        def s6_ts(t):
            i = 48 + t
            V(lambda e: e.tensor_scalar(out=rs[:, i:i + 1], in0=ss[:, i:i + 1], scalar1=1.0 / D, scalar2=EPS,
                                        op0=ALU.mult, op1=ALU.add),
              reads=[('ss', i)], writes=[('rs', i)])

        def s6_sqrt(t):
            i = 48 + t
            A(lambda e: e.activation(out=rs[:, i:i + 1], in_=rs[:, i:i + 1], func=AF.Sqrt),
              reads=[('rs', i)], writes=[('rs', i)])

        def s6_out(t):
            i = 48 + t
            p = t % NO6
            V(lambda e: e.reciprocal(out=rs[:, i:i + 1], in_=rs[:, i:i + 1]),
              reads=[('rs', i)], writes=[('rs', i)])
            V(lambda e: e.scalar_tensor_tensor(out=ot[p][:], in0=X[:, t, :], scalar=rs[:, i:i + 1],
                                               in1=gfin[:], op0=ALU.mult, op1=ALU.mult),
              reads=[('X', t, 0), ('X', t, 1), ('rs', i), 'gfin'], writes=[('ot', p)])
            dma('sync', out_d[t * 128:(t + 1) * 128, :], ot[p][:], reads=[('ot', p)], semname="out%d" % p)
        stages6 = [(s6_sq, 0), (s6_ts, 1), (s6_sqrt, 2), (s6_out, 3)]
        for j in range(NT + 3):
            for fn, lag in stages6:
                t = j - lag
                if 0 <= t < NT:
                    fn(t)
    S.emit(final_waits=[k for k in (('D', 'out'), ('D', 'outg'), ('D', 'out0'), ('D', 'out1'), ('D', 'out2')) if k in S.cnt])
    root.close()
    return nc


def _t5_bucket(dist):
    dist = np.asarray(dist, dtype=np.int64)
    small = dist < 16
    ratio = np.log(np.maximum(dist, 1).astype(np.float32) / np.float32(16)) / np.float32(math.log(2048 / 16))
    large = 16 + (ratio.astype(np.float32) * np.float32(16)).astype(np.int32)
    large = np.minimum(large, 31)
    return np.where(small, dist, large)


def _constants():
    cst = {}
    half = 64
    inv = (10000.0 ** (-np.arange(half, dtype=np.float32) / half)).astype(np.float32)
    pos = np.arange(SEQ, dtype=np.float32)
    ang = pos[None, :] * inv[:, None]
    cos = np.cos(ang).astype(np.float32)
    sin = np.sin(ang).astype(np.float32)
    cst['cos_full'] = np.concatenate([cos, cos], axis=0)
    cst['sin_full'] = np.concatenate([-sin, sin], axis=0)
    Hh = 4
    log_g = np.log1p(-np.exp2(-5.0 - np.arange(Hh, dtype=np.float64)))
    i = np.arange(128, dtype=np.float64)
    scale = 128 ** -0.5
    dm = np.zeros((128, 4, 128), np.float32)
    for h in range(Hh):
        diff = i[None, :] - i[:, None]
        dm[:, h, :] = np.where(diff >= 0, np.exp(np.maximum(diff, 0) * log_g[h]) * scale, 0.0)
    rvec = np.zeros((128, 16), np.float32)
    for h in range(Hh):
        rvec[:, h] = np.exp((127.0 - i) * log_g[h]) * scale
        rvec[:, 4 + h] = np.exp((i + 1.0) * log_g[h])
        rvec[:, 8 + h] = np.exp(128.0 * log_g[h])
    cst['dmask'] = dm
    cst['rvec'] = rvec
    z2 = np.zeros((128, 4, 16), np.float32)
    for h in range(Hh):
        for n in range(16):
            z2[:, h, n] = np.exp((127.0 - i) * log_g[h] + 128.0 * (15 - n) * log_g[h]) * scale
    cst['zeta2'] = z2
    ohf = np.zeros((32, 3, 384), np.float32)
    for g, (w, r) in enumerate(ATT_PATTERNS):
        j = np.arange(129)
        bk = _t5_bucket(r * j)
        for jj in range(129):
            ohf[bk[jj], g, 127 + jj] = 1.0
    cst['ohf'] = ohf
    cst['identf'] = np.eye(128, dtype=np.float32)
    cst['jf'] = np.ascontiguousarray(np.eye(128, dtype=np.float32)[::-1])
    return cst


def _prep_shared(inp):
    sh = {}
    w_in = inp['w_in'][0]

    def chunk(cols):
        return w_in[:, cols].reshape(8, 128, len(cols)).transpose(1, 0, 2)

    def rng(a, n=128):
        return np.arange(a, a + n)
    r1 = []
    r2 = []
    for h in range(4):
        q0 = C_RQ + h * 128
        k0 = C_RK + h * 128
        r1.append(chunk(rng(q0)))
        r1.append(chunk(np.concatenate([rng(q0 + 64, 64), rng(q0, 64)])))
        r1.append(chunk(rng(k0)))
        r1.append(chunk(np.concatenate([rng(k0 + 64, 64), rng(k0, 64)])))
        r2.append(chunk(rng(C_RV + h * 256, 256)))
        r2.append(chunk(rng(C_RG + h * 256, 256)))
    for hg in range(4):
        for g in range(3):
            hd = g * 4 + hg
            r1.append(chunk(rng(C_AQ + hd * 128)))
            r1.append(chunk(rng(C_AK + hd * 128)))
            r1.append(chunk(rng(C_AV + hd * 128)))
    for m in range(8):
        r1.append(chunk(rng(C_GA + m * 128)))
    for m in range(8):
        r1.append(chunk(rng(C_GB + m * 128)))
    sh['w_in_r1'] = np.ascontiguousarray(np.stack(r1, axis=0))
    sh['w_in_r2'] = np.ascontiguousarray(np.stack(r2, axis=0))
    b_ada = inp['b_ada'][0]
    secs = [0, 1, 3, 4]
    bc = np.zeros((128, 32), np.float32)
    for si, s in enumerate(secs):
        bc[:, si * 8:(si + 1) * 8] = b_ada[s * 1024:(s + 1) * 1024].reshape(8, 128).T
    sh['bada_col'] = bc
    bb = np.stack([b_ada[2048:3072], b_ada[5120:6144]], axis=0)
    sh['bada_bc'] = np.ascontiguousarray(np.broadcast_to(bb[None], (128, 2, 1024)))
    sh['gmix_col'] = np.ascontiguousarray(inp['g_mix'][0].reshape(8, 128).T)
    sh['gffn_col'] = np.ascontiguousarray(inp['g_ffn'][0].reshape(8, 128).T)
    sh['gfin_bc'] = np.ascontiguousarray(np.broadcast_to(inp['g_final'][None, :], (128, 1024)))
    sh['w_ada'] = np.ascontiguousarray(inp['w_ada'][0])
    sh['rel_bias'] = np.ascontiguousarray(inp['rel_bias'])
    sh['w_ret_o'] = np.ascontiguousarray(inp['w_ret_o'][0])
    sh['w_att_o'] = np.ascontiguousarray(inp['w_att_o'][0])
    sh['w_o'] = np.ascontiguousarray(inp['w_o'][0])
    sh['w_router'] = np.ascontiguousarray(inp['w_router'][0])
    sh['brouter_bc'] = np.ascontiguousarray(np.broadcast_to(inp['b_router'][0][None, :], (128, 32)))
    w1 = inp['w_mlp1'][0].reshape(32, 8, 128, 8, 128, 2)
    sh['w1r'] = np.ascontiguousarray(w1.transpose(0, 3, 2, 1, 5, 4)).reshape(32, 8, 128, 8, 256)
    sh['b1r'] = np.ascontiguousarray(inp['b_mlp1'][0].reshape(32, 8, 128, 2).transpose(2, 0, 1, 3))
    sh['w_mlp2'] = np.ascontiguousarray(inp['w_mlp2'][0])
    sh['b_mlp2'] = np.ascontiguousarray(inp['b_mlp2'][0])
    return sh


def make_in_maps(inp):
    inp = {k: np.asarray(v, dtype=np.float32) for k, v in inp.items()}
    cst = _constants()
    sh = _prep_shared(inp)
    x = inp['x']
    c = inp['c']
    zeros_prev = np.zeros((HALF, D), np.float32)
    in_maps = []
    for core in range(8):
        b, hf = core // 2, core % 2
        m = dict(sh)
        m['x_own'] = np.ascontiguousarray(x[b, hf * HALF:(hf + 1) * HALF])
        m['x_prev'] = np.ascontiguousarray(x[b, 0:HALF]) if hf == 1 else zeros_prev
        m['flag'] = np.full((128, 1), float(hf), np.float32)
        m['c_col'] = np.ascontiguousarray(c[b].reshape(8, 128).T)
        if hf == 1:
            m['rope_cos'] = cst['cos_full']
            m['rope_sin'] = cst['sin_full']
        else:
            m['rope_cos'] = np.ascontiguousarray(np.concatenate([cst['cos_full'][:, :HALF]] * 2, axis=1))
            m['rope_sin'] = np.ascontiguousarray(np.concatenate([cst['sin_full'][:, :HALF]] * 2, axis=1))
        for k in ('dmask', 'rvec', 'zeta2', 'ohf', 'identf', 'jf'):
            m[k] = cst[k]
        in_maps.append(m)
    return in_maps


def kernel(**inputs):
    in_maps = make_in_maps(inputs)
    nc = build_program()
    res = run_bass_kernel_spmd(nc, in_maps, core_ids=list(range(8)))
    out = np.zeros((4, SEQ, D), np.float32)
    for core in range(8):
        b, hf = core // 2, core % 2
        out[b, hf * HALF:(hf + 1) * HALF] = res.results[core]["out"]
    return out
```

```python
import contextlib
import math
import numpy as np
import concourse.bass as bass
import concourse.mybir as mybir
from concourse.bass_utils import run_bass_kernel_spmd

F32 = mybir.dt.float32
BF16 = mybir.dt.bfloat16
AF = mybir.ActivationFunctionType
ALU = mybir.AluOpType
AX = mybir.AxisListType

D = 1024
SEQ = 4096
HALF = 2048
NT = 16
EPS = 1e-6
GN_EPS = 1e-5
N_EXP = 32
ATT_PATTERNS = ((128, 1), (512, 4), (2048, 16))
NSLOT = 4

C_RQ, C_RK, C_RV, C_RG = 0, 512, 1024, 2048
C_AQ, C_AK, C_AV = 3072, 4608, 6144
C_GA, C_GB = 7680, 8704


class Sched:
    ENG = ['sync', 'scalar', 'vector', 'gpsimd', 'tensor']

    def __init__(self, nc):
        self.nc = nc
        self.ops = {e: [] for e in self.ENG}
        self.cnt = {}
        self.waited = {e: {} for e in self.ENG}
        self.lastw = {}
        self.readers = {}

    def op(self, eng, fn, reads=(), writes=(), sem=None, inc=1):
        deps = {}
        own = ('E', eng)

        def add(d, raw):
            if d is None:
                return
            k, v = d
            if k == own and not raw:
                return
            if deps.get(k, 0) < v:
                deps[k] = v
        for b in reads:
            add(self.lastw.get(b), True)
        waw_own = eng in ('vector', 'gpsimd', 'scalar')
        for b in writes:
            add(self.lastw.get(b), waw_own)
            for r in self.readers.get(b, ()):
                add(r, False)
        waits = []
        for k, v in deps.items():
            if self.waited[eng].get(k, 0) < v:
                self.waited[eng][k] = v
                waits.append((k, v))
        semkey = sem if sem is not None else own
        self.cnt[semkey] = self.cnt.get(semkey, 0) + inc
        val = self.cnt[semkey]
        self.ops[eng].append((waits, fn, semkey, inc))
        for b in reads:
            self.readers.setdefault(b, []).append((semkey, val))
        for b in writes:
            self.lastw[b] = (semkey, val)
            self.readers[b] = []
        return val

    def barrier(self):
        for e in self.ENG:
            waits = []
            for k, v in self.cnt.items():
                if self.waited[e].get(k, 0) < v:
                    self.waited[e][k] = v
                    waits.append((k, v))
            if waits:
                self.ops[e].append((waits, None, None, 0))
        self.lastw = {}
        self.readers = {}

    def emit(self, final_waits=()):
        nc = self.nc
        keys = list(self.cnt.keys())
        with contextlib.ExitStack() as st:
            sems = {}
            for i, k in enumerate(keys):
                sems[k] = st.enter_context(nc.semaphore("s%d" % i))
            block = st.enter_context(nc.Block())

            def body(engname):
                def f(e):
                    for waits, fn, semkey, inc in self.ops[engname]:
                        for k, v in waits:
                            e.wait_ge(sems[k], v)
                        if fn is None:
                            continue
                        ins = fn(e)
                        ins.then_inc(sems[semkey], inc)
                    if engname == 'sync':
                        for k in final_waits:
                            e.wait_ge(sems[k], self.cnt[k])
                return f
            block.sync(body('sync'))
            block.scalar(body('scalar'))
            block.vector(body('vector'))
            block.gpsimd(body('gpsimd'))
            block.tensor(body('tensor'))


def build_program(stage=99, dbg_cols=0):
    nc = bass.Bass("TRN2", target_bir_lowering=False)
    S = Sched(nc)

    def din(name, shape, dt=F32):
        return nc.dram_tensor(name, list(shape), dt, kind="ExternalInput").ap()

    x_own = din("x_own", [HALF, D])
    x_prev = din("x_prev", [HALF, D])
    flag_d = din("flag", [128, 1])
    ccol_d = din("c_col", [128, 8])
    w_ada_d = din("w_ada", [D, 6 * D])
    badacol_d = din("bada_col", [128, 32])
    badabc_d = din("bada_bc", [128, 2, D])
    gmix_d = din("gmix_col", [128, 8])
    gffn_d = din("gffn_col", [128, 8])
    gfin_d = din("gfin_bc", [128, D])
    win1_d = din("w_in_r1", [68, 128, 8, 128])
    win2_d = din("w_in_r2", [8, 128, 8, 256])
    ropec_d = din("rope_cos", [128, SEQ])
    ropes_d = din("rope_sin", [128, SEQ])
    dm_d = din("dmask", [128, 4, 128])
    rvec_d = din("rvec", [128, 16])
    zeta2_d = din("zeta2", [128, 4, 16])
    relb_d = din("rel_bias", [32, 12])
    ohf_d = din("ohf", [32, 3, 384])
    identf_d = din("identf", [128, 128])
    jf_d = din("jf", [128, 128])
    wreto_d = din("w_ret_o", [D, D])
    watto_d = din("w_att_o", [512, D])
    wo_d = din("w_o", [D, D])
    wr_d = din("w_router", [D, N_EXP])
    brbc_d = din("brouter_bc", [128, N_EXP])
    if stage > 5:
        w1r_d = din("w1r", [N_EXP, 8, 128, 8, 256])
        b1r_d = din("b1r", [128, N_EXP, 8, 2])
        w2_d = din("w_mlp2", [N_EXP, D, D])
        b2_d = din("b_mlp2", [N_EXP, D])
    out_d = nc.dram_tensor("out", [HALF, D], F32, kind="ExternalOutput").ap()
    fd_t = nc.dram_tensor("fd_scratch", [12, 384], F32)
    fd_d = fd_t.ap()
    dbg_d = None
    if dbg_cols:
        dbg_d = nc.dram_tensor("dbg", [128, dbg_cols], F32, kind="ExternalOutput").ap()

    root = contextlib.ExitStack()
    root.__enter__()

    def sb(st, name, shape, dt, side=None):
        name = "sb_" + name
        if side is None:
            return st.enter_context(nc.sbuf_tensor(name, list(shape), dt))
        return st.enter_context(nc.sbuf_tensor(name, list(shape), dt, side=side))

    PS = root.enter_context(nc.psum_tensor("psall", [128, 8, 512], F32))

    def bank(i):
        return PS[:, i, :]

    def bank_bf(i):
        return PS[:, i, :].bitcast(BF16)

    dma_n = [0]

    def dma(eng, out, in_, reads=(), writes=(), semname=None):
        if semname is None:
            semname = "u%d" % dma_n[0]
            dma_n[0] += 1
        return S.op(eng, lambda e: e.dma_start(out=out, in_=in_), reads=reads, writes=writes,
                    sem=('D', semname), inc=16)

    def V(fn, reads=(), writes=()):
        return S.op('vector', fn, reads, writes)

    def A(fn, reads=(), writes=()):
        return S.op('scalar', fn, reads, writes)

    def G(fn, reads=(), writes=()):
        return S.op('gpsimd', fn, reads, writes)

    def T(fn, reads=(), writes=()):
        return S.op('tensor', fn, reads, writes)

    flag = sb(root, "flag", [128, 1], F32)
    identb = sb(root, "identb", [128, 128], BF16)
    identf = sb(root, "identf", [128, 128], F32)
    onesb = sb(root, "onesb", [128, 128], BF16)
    onesf = sb(root, "onesf", [128, 128], F32)
    modcol = sb(root, "modcol", [128, 32], F32)
    scA = sb(root, "scA", [128, 8], F32)
    scAp = sb(root, "scAp", [128, 8], F32)
    shAp = sb(root, "shAp", [128, 8], F32)
    scF = sb(root, "scF", [128, 8], F32)
    gtA_bc = sb(root, "gtA_bc", [128, D], F32)
    gtF_bc = sb(root, "gtF_bc", [128, D], F32)
    ss = sb(root, "ss", [128, 64], F32)
    rs = sb(root, "rs", [128, 64], F32)
    shA = modcol[:, 0:8]
    shF = modcol[:, 16:24]

    dma('sync', flag[:], flag_d, writes=['flag'])
    dma('sync', identf[:], identf_d, writes=['identf'])
    dma('gpsimd', identb[:], identf_d, writes=['identb'])
    V(lambda e: e.memset(onesb[:], 1.0), writes=['onesb'])
    V(lambda e: e.memset(onesf[:], 1.0), writes=['onesf'])

    with contextlib.ExitStack() as p0:
        ccol = sb(p0, "ccol", [128, 8], F32)
        cs2 = sb(p0, "cs2", [128, 8, 2], F32)
        csb = sb(p0, "csb", [128, 8, 128], F32)
        badacol = sb(p0, "badacol", [128, 32], F32)
        badabc = sb(p0, "badabc", [128, 2, D], F32)
        gmix = sb(p0, "gmix", [128, 8], F32)
        gffn = sb(p0, "gffn", [128, 8], F32)
        wast = [sb(p0, "wast%d" % i, [128, 8, 512], F32) for i in range(3)]
        dma('sync', ccol[:], ccol_d, writes=['ccol'])
        dma('sync', badacol[:], badacol_d, writes=['badacol'])
        dma('sync', badabc[:], badabc_d, writes=['badabc'])
        dma('sync', gmix[:], gmix_d, writes=['gmix'])
        dma('sync', gffn[:], gffn_d, writes=['gffn'])
        for j in range(2):
            A(lambda e, j=j: e.activation(out=cs2[:, :, j], in_=ccol[:], func=AF.Silu),
              reads=['ccol'], writes=[('cs2', j)])
        for kc in range(8):
            V(lambda e, kc=kc: e.tensor_scalar(out=csb[:, kc, :], in0=onesf[:], scalar1=cs2[:, kc, 0:1],
                                               scalar2=None, op0=ALU.mult),
              reads=['onesf', ('cs2', 0)], writes=['csb'])
        w_ada_v = w_ada_d.rearrange("(kc p) n -> p kc n", p=128)
        colsec = {0: 0, 1: 1, 3: 2, 4: 3}
        for j in range(12):
            sec, half = j // 2, j % 2
            wt = wast[j % 3]
            dma('sync', wt[:], w_ada_v[:, :, j * 512:(j + 1) * 512], writes=[('wast', j % 3)],
                semname="wast%d" % (j % 3))
            if sec in colsec:
                si = colsec[sec]

                def mmcol(e, wt=wt, si=si, half=half):
                    ins = None
                    for cc in range(4):
                        c = half * 4 + cc
                        o = (si * 8 + c) * 2
                        for kc in range(8):
                            ins = e.matmul(bank(0)[:, o:o + 2], lhsT=wt[:, kc, cc * 128:(cc + 1) * 128],
                                           rhs=cs2[:, kc, :], start=(kc == 0), stop=(kc == 7))
                    return ins
                T(mmcol, reads=[('wast', j % 3), ('cs2', 0), ('cs2', 1)], writes=[('ps', 0)])
            else:
                bk = 1 + half
                which = 0 if sec == 2 else 1
                dst = gtA_bc if sec == 2 else gtF_bc

                def mmrow(e, wt=wt, bk=bk):
                    ins = None
                    for kc in range(8):
                        ins = e.matmul(bank(bk)[:, 0:512], lhsT=csb[:, kc, :], rhs=wt[:, kc, :],
                                       start=(kc == 0), stop=(kc == 7))
                    return ins
                T(mmrow, reads=[('wast', j % 3), 'csb'], writes=[('ps', bk)])
                V(lambda e, bk=bk, dst=dst, which=which, half=half: e.tensor_tensor(
                    out=dst[:, half * 512:(half + 1) * 512], in0=bank(bk)[:, 0:512],
                    in1=badabc[:, which, half * 512:(half + 1) * 512], op=ALU.add),
                  reads=[('ps', bk), 'badabc'], writes=[('gt', sec, half)])
        V(lambda e: e.tensor_tensor(out=modcol[:], in0=bank(0)[:, 0:64:2], in1=badacol[:], op=ALU.add),
          reads=[('ps', 0), 'badacol'], writes=['modcol'])
        V(lambda e: e.scalar_tensor_tensor(out=scA[:], in0=modcol[:, 8:16], scalar=1.0, in1=gmix[:],
                                           op0=ALU.add, op1=ALU.mult),
          reads=['modcol', 'gmix'], writes=['scA'])
        V(lambda e: e.scalar_tensor_tensor(out=scF[:], in0=modcol[:, 24:32], scalar=1.0, in1=gffn[:],
                                           op0=ALU.add, op1=ALU.mult),
          reads=['modcol', 'gffn'], writes=['scF'])
        V(lambda e: e.tensor_scalar(out=scAp[:], in0=scA[:], scalar1=flag[:, 0:1], scalar2=None, op0=ALU.mult),
          reads=['scA', 'flag'], writes=['scAp'])
        V(lambda e: e.tensor_scalar(out=shAp[:], in0=modcol[:, 0:8], scalar1=flag[:, 0:1], scalar2=None,
                                    op0=ALU.mult),
          reads=['modcol', 'flag'], writes=['shAp'])
        S.barrier()

    if stage == 0:
        dma('sync', dbg_d[:, 0:32], modcol[:], semname="out")
        dma('sync', dbg_d[:, 32:40], scA[:], semname="out")
        dma('sync', dbg_d[:, 40:48], scAp[:], semname="out")
        dma('sync', dbg_d[:, 48:56], scF[:], semname="out")
        dma('sync', dbg_d[:, 64:64 + 1024], gtA_bc[:], semname="out")
        dma('sync', dbg_d[:, 1088:1088 + 1024], gtF_bc[:], semname="out")
        S.emit(final_waits=[k for k in (('D', 'out'), ('D', 'outg')) if k in S.cnt])
        root.close()
        return nc

    mix = contextlib.ExitStack()
    mix.__enter__()
    HT = sb(mix, "HT", [128, 8, SEQ], BF16)
    RetT = sb(mix, "RetT", [128, 8, HALF], BF16)
    WB = [sb(mix, "WB%d" % i, [128, 8, 256], BF16) for i in range(NSLOT)]

    with contextlib.ExitStack() as p1:
        NXI, NXN = 6, 3
        xin = [sb(p1, "xin%d" % i, [128, D], F32) for i in range(NXI)]
        xn = [sb(p1, "xn%d" % i, [128, D], BF16) for i in range(NXN)]
        junk = sb(p1, "junk", [128, D], BF16)

        def st_dma(i):
            src = x_prev if i < 16 else x_own
            t = i % 16
            dma('sync', xin[i % NXI][:], src[t * 128:(t + 1) * 128, :], writes=[('xin', i % NXI)],
                semname="xin%d" % (i % NXI))

        def st_sq(i):
            A(lambda e: e.activation(out=junk[:], in_=xin[i % NXI][:], func=AF.Square, accum_out=ss[:, i:i + 1]),
              reads=[('xin', i % NXI)], writes=['junk', ('ss', i)])

        def st_ts(i):
            V(lambda e: e.tensor_scalar(out=rs[:, i:i + 1], in0=ss[:, i:i + 1], scalar1=1.0 / D, scalar2=EPS,
                                        op0=ALU.mult, op1=ALU.add),
              reads=[('ss', i)], writes=[('rs', i)])

        def st_sqrt(i):
            A(lambda e: e.activation(out=rs[:, i:i + 1], in_=rs[:, i:i + 1], func=AF.Sqrt),
              reads=[('rs', i)], writes=[('rs', i)])

        def st_xn(i):
            V(lambda e: e.reciprocal(out=rs[:, i:i + 1], in_=rs[:, i:i + 1]),
              reads=[('rs', i)], writes=[('rs', i)])
            V(lambda e: e.tensor_scalar(out=xn[i % NXN][:], in0=xin[i % NXI][:], scalar1=rs[:, i:i + 1],
                                        scalar2=None, op0=ALU.mult),
              reads=[('xin', i % NXI), ('rs', i)], writes=[('xn', i % NXN)])

        def st_tr(i):
            pT = bank_bf(i % 4)

            def tr(e):
                ins = None
                for kc in range(8):
                    ins = e.transpose(out=pT[:, kc * 128:(kc + 1) * 128],
                                      in_=xn[i % NXN][:, kc * 128:(kc + 1) * 128], identity=identb[:])
                return ins
            T(tr, reads=[('xn', i % NXN)], writes=[('ps', i % 4)])

        def st_ev(i):
            pT = bank_bf(i % 4)
            sc_, sh_ = (scAp, shAp) if i < 16 else (scA, shA)
            for kc in range(8):
                dst = HT[:, kc, i * 128:(i + 1) * 128]
                srcp = pT[:, kc * 128:(kc + 1) * 128]
                if i % 2 == 0:
                    A(lambda e, dst=dst, srcp=srcp, kc=kc: e.activation(
                        out=dst, in_=srcp, func=AF.Identity, bias=sh_[:, kc:kc + 1], scale=sc_[:, kc:kc + 1]),
                      reads=[('ps', i % 4)], writes=[('HTa', i)])
                else:
                    V(lambda e, dst=dst, srcp=srcp, kc=kc: e.tensor_scalar(
                        out=dst, in0=srcp, scalar1=sc_[:, kc:kc + 1], scalar2=sh_[:, kc:kc + 1],
                        op0=ALU.mult, op1=ALU.add),
                      reads=[('ps', i % 4)], writes=[('HTv', i)])
        st_dma(0)
        st_dma(1)
        stages = [(st_sq, 0), (st_ts, 1), (st_sqrt, 2), (st_xn, 3), (st_tr, 4), (st_ev, 5)]
        for j in range(32 + 5):
            if j + 2 < 32:
                st_dma(j + 2)
            for fn, lag in stages:
                i = j - lag
                if 0 <= i < 32:
                    fn(i)
        S.barrier()

    if dbg_cols:
        dma('gpsimd', dbg_d[:, 0:4096], HT[:, 0, :], semname="outg")
        dma('gpsimd', dbg_d[:, 4096:8192], HT[:, 5, :], semname="outg")
    if stage == 1:
        S.emit(final_waits=[k for k in (('D', 'out'), ('D', 'outg')) if k in S.cnt])
        mix.close()
        root.close()
        return nc

    loads = []
    for h in range(4):
        for k in range(4):
            loads.append((win1_d[h * 4 + k], 1))
        loads.append((win2_d[h * 2 + 0], 2))
        loads.append((win2_d[h * 2 + 1], 2))
    for hg in range(4):
        for g in range(3):
            for k in range(3):
                loads.append((win1_d[16 + (hg * 3 + g) * 3 + k], 1))
    wreto_v = wreto_d.rearrange("(kc p) n -> p kc n", p=128)
    watto_v = watto_d.rearrange("(kc p) n -> p kc n", p=128)
    for m in range(8):
        loads.append((win1_d[52 + m], 1))
        loads.append((win1_d[60 + m], 1))
        loads.append((wreto_v[:, :, m * 128:(m + 1) * 128], 1))
        loads.append((watto_v[:, :, m * 128:(m + 1) * 128], 3))
    ring = {'issued': 0, 'consumed': 0}
    N_MAIN_LOADS = 60

    def ring_issue():
        n = ring['issued']
        s = n % NSLOT
        src, kind = loads[n]
        if kind == 1:
            dst = WB[s][:, :, 0:128]
        elif kind == 2:
            dst = WB[s][:, :, :]
        else:
            dst = WB[s][:, 0:4, 0:128]
        dma('gpsimd', dst, src, writes=[('WB', s)], semname="WB%d" % s)
        ring['issued'] += 1

    def ring_get(hold=1):
        while ring['issued'] < min(N_MAIN_LOADS, ring['consumed'] + NSLOT - hold + 1):
            ring_issue()
        s = ring['consumed'] % NSLOT
        ring['consumed'] += 1
        return s

    pbank = [0]

    def next_pbank():
        b = pbank[0]
        pbank[0] = (pbank[0] + 1) % 4
        return b

    def fm_mm(bk, s, col0, ncols=512, nk=8, src=None, wcols=(0, 128)):
        src = HT if src is None else src

        def f(e):
            ins = None
            for kc in range(nk):
                ins = e.matmul(bank(bk)[:, 0:ncols], lhsT=WB[s][:, kc, wcols[0]:wcols[1]],
                               rhs=src[:, kc, col0:col0 + ncols], start=(kc == 0), stop=(kc == nk - 1))
            return ins
        return f

    def tm_mm(bk, s, tok_sl, ncols):
        def f(e):
            ins = None
            for kc in range(8):
                ins = e.matmul(bank(bk)[:, 0:ncols], lhsT=HT[:, kc, tok_sl], rhs=WB[s][:, kc, 0:ncols],
                               start=(kc == 0), stop=(kc == 7))
            return ins
        return f

    with contextlib.ExitStack() as rsc:
        ropeC = sb(rsc, "ropeC", [128, SEQ], BF16)
        ropeS = sb(rsc, "ropeS", [128, SEQ], BF16)
        Dm = sb(rsc, "Dm", [128, 4, 128], F32)
        rvec = sb(rsc, "rvec", [128, 16], F32)
        zeta2 = sb(rsc, "zeta2", [128, 4, 16], F32)
        qT = sb(rsc, "qT", [128, HALF], BF16)
        kT = sb(rsc, "kT", [128, SEQ], BF16)
        Vt = sb(rsc, "Vt", [128, 32, 256], BF16)
        SG = sb(rsc, "SG", [128, 16, 256], BF16)
        t1 = [sb(rsc, "t1_%d" % i, [128, 512], F32) for i in range(2)]
        t2 = [sb(rsc, "t2_%d" % i, [128, 512], F32) for i in range(2)]
        Kz = [sb(rsc, "Kz%d" % i, [128, 128], BF16) for i in range(2)]
        PT = [sb(rsc, "PT%d" % i, [128, 128], BF16) for i in range(2)]
        yI = [sb(rsc, "yI%d" % i, [128, 256], F32) for i in range(2)]
        yy = [sb(rsc, "yy%d" % i, [128, 256], F32) for i in range(2)]
        yn = [sb(rsc, "yn%d" % i, [128, 256], BF16) for i in range(2)]
        yg = [sb(rsc, "yg%d" % i, [128, 256], BF16) for i in range(2)]
        st6 = [sb(rsc, "st6_%d" % i, [128, 6], F32) for i in range(2)]
        mv = [sb(rsc, "mv%d" % i, [128, 2], F32) for i in range(2)]
        rstd = [sb(rsc, "rstd%d" % i, [128, 1], F32) for i in range(2)]
        nmr = [sb(rsc, "nmr%d" % i, [128, 1], F32) for i in range(2)]
        Rf = sb(rsc, "Rf", [128, 256], F32)
        Rb = sb(rsc, "Rb", [128, 256], BF16)
        dma('gpsimd', ropeC[:], ropec_d, writes=['ropeC'])
        dma('gpsimd', ropeS[:], ropes_d, writes=['ropeS'])
        dma('sync', Dm[:], dm_d, writes=['Dm'])
        dma('sync', rvec[:], rvec_d, writes=['rvec'])
        dma('sync', zeta2[:], zeta2_d, writes=['zeta2'])
        ecnt = [0]

        for h in range(4):
            sq, sqs, sk, sks = None, None, None, None
            for which in ('q', 'k'):
                sa = ring_get(1)
                sbw = ring_get(2)
                ntg = 4 if which == 'q' else 8
                for tg in range(ntg):
                    col0 = (HALF + tg * 512) if which == 'q' else tg * 512
                    bA, bB = next_pbank(), next_pbank()
                    T(fm_mm(bA, sa, col0), reads=[('WB', sa)], writes=[('ps', bA)])
                    T(fm_mm(bB, sbw, col0), reads=[('WB', sbw)], writes=[('ps', bB)])
                    par = ecnt[0] % 2
                    ecnt[0] += 1
                    V(lambda e, bA=bA, col0=col0, par=par: e.tensor_tensor(
                        out=t1[par][:], in0=bank(bA)[:, 0:512], in1=ropeC[:, col0:col0 + 512], op=ALU.mult),
                      reads=[('ps', bA), 'ropeC'], writes=[('t1', par)])
                    V(lambda e, bB=bB, col0=col0, par=par: e.tensor_tensor(
                        out=t2[par][:], in0=bank(bB)[:, 0:512], in1=ropeS[:, col0:col0 + 512], op=ALU.mult),
                      reads=[('ps', bB), 'ropeS'], writes=[('t2', par)])
                    if which == 'q':
                        dst = qT[:, tg * 512:(tg + 1) * 512]
                        key = ('qT', tg)
                    else:
                        dst = kT[:, tg * 512:(tg + 1) * 512]
                        key = ('kT', tg)
                    G(lambda e, dst=dst, par=par: e.tensor_tensor(out=dst, in0=t1[par][:], in1=t2[par][:], op=ALU.add),
                      reads=[('t1', par), ('t2', par)], writes=[key])
            sv = ring_get()
            for t in range(32):
                bk = next_pbank()
                T(tm_mm(bk, sv, slice(t * 128, (t + 1) * 128), 256), reads=[('WB', sv)], writes=[('ps', bk)])
                A(lambda e, bk=bk, t=t: e.activation(out=Vt[:, t, :], in_=bank(bk)[:, 0:256], func=AF.Copy),
                  reads=[('ps', bk)], writes=[('V', t)])
            sg_ = ring_get()
            for t in range(16):
                bk = next_pbank()
                T(tm_mm(bk, sg_, slice(HALF + t * 128, HALF + (t + 1) * 128), 256), reads=[('WB', sg_)],
                  writes=[('ps', bk)])
                A(lambda e, bk=bk, t=t: e.activation(out=SG[:, t, :], in_=bank(bk)[:, 0:256], func=AF.Silu),
                  reads=[('ps', bk)], writes=[('SG', t)])
            for n in range(16):
                p = n % 2
                ksl = slice(n * 128, (n + 1) * 128)
                T(lambda e, ksl=ksl: e.transpose(out=bank_bf(4)[:, 0:128], in_=kT[:, ksl], identity=identb[:]),
                  reads=[('kT', n // 4)], writes=[('ps', 4)])
                V(lambda e, p=p, h=h, n=n: e.tensor_scalar(out=Kz[p][:], in0=bank_bf(4)[:, 0:128],
                                                           scalar1=zeta2[:, h, n:n + 1], scalar2=None, op0=ALU.mult),
                  reads=[('ps', 4), 'zeta2'], writes=[('Kz', p)])
                T(lambda e, p=p, n=n: e.matmul(bank(2)[:, 0:256], lhsT=Kz[p][:], rhs=Vt[:, n, :],
                                               start=(n == 0), stop=(n == 15)),
                  reads=[('Kz', p), ('V', n)], writes=[('ps', 2)])
            V(lambda e: e.tensor_copy(out=Rb[:], in_=bank(2)[:, 0:256]), reads=[('ps', 2)], writes=['Rb'])
            V(lambda e: e.tensor_copy(out=Rf[:], in_=bank(2)[:, 0:256]), reads=[('ps', 2)], writes=['Rf'])
            for n in range(16, 32):
                own = True
                p = n % 2
                ksl = slice(n * 128, (n + 1) * 128)
                T(lambda e, ksl=ksl: e.transpose(out=bank_bf(4)[:, 0:128], in_=kT[:, ksl], identity=identb[:]),
                  reads=[('kT', n // 4)], writes=[('ps', 4)])
                V(lambda e, p=p, h=h: e.tensor_scalar(out=Kz[p][:], in0=bank_bf(4)[:, 0:128],
                                                      scalar1=rvec[:, h:h + 1], scalar2=None, op0=ALU.mult),
                  reads=[('ps', 4), 'rvec'], writes=[('Kz', p)])
                if own:
                    j = n - 16
                    qsl = slice(j * 128, (j + 1) * 128)
                    T(lambda e, ksl=ksl, qsl=qsl: e.matmul(bank(5)[:, 0:128], lhsT=kT[:, ksl], rhs=qT[:, qsl],
                                                           start=True, stop=True),
                      reads=[('kT', n // 4), ('qT', j // 4)], writes=[('ps', 5)])
                    V(lambda e, p=p, h=h: e.tensor_tensor(out=PT[p][:], in0=bank(5)[:, 0:128], in1=Dm[:, h, :],
                                                          op=ALU.mult),
                      reads=[('ps', 5), 'Dm'], writes=[('PT', p)])
                    bI = 6 + p
                    T(lambda e, p=p, n=n, bI=bI: e.matmul(bank(bI)[:, 0:256], lhsT=PT[p][:], rhs=Vt[:, n, :],
                                                          start=True, stop=True),
                      reads=[('PT', p), ('V', n)], writes=[('ps', bI)])
                    bC = 0 + p
                    T(lambda e, qsl=qsl, bC=bC: e.matmul(bank(bC)[:, 0:256], lhsT=qT[:, qsl], rhs=Rb[:],
                                                         start=True, stop=True),
                      reads=[('qT', j // 4), 'Rb'], writes=[('ps', bC)])
                    A(lambda e, p=p, bI=bI: e.activation(out=yI[p][:], in_=bank(bI)[:, 0:256], func=AF.Copy),
                      reads=[('ps', bI)], writes=[('yI', p)])
                    V(lambda e, p=p, bC=bC, h=h: e.scalar_tensor_tensor(
                        out=yy[p][:], in0=bank(bC)[:, 0:256], scalar=rvec[:, 4 + h:5 + h], in1=yI[p][:],
                        op0=ALU.mult, op1=ALU.add),
                      reads=[('ps', bC), ('yI', p), 'rvec'], writes=[('yy', p)])
                    V(lambda e, p=p: e.bn_stats(out=st6[p][:], in_=yy[p][:]), reads=[('yy', p)], writes=[('st6', p)])
                    V(lambda e, p=p: e.bn_aggr(out=mv[p][:], in_=st6[p][:]), reads=[('st6', p)], writes=[('mv', p)])
                    V(lambda e, p=p: e.tensor_scalar(out=rstd[p][:], in0=mv[p][:, 1:2], scalar1=GN_EPS, scalar2=None,
                                                     op0=ALU.add),
                      reads=[('mv', p)], writes=[('rstd', p)])
                    A(lambda e, p=p: e.activation(out=rstd[p][:], in_=rstd[p][:], func=AF.Sqrt),
                      reads=[('rstd', p)], writes=[('rstd', p)])
                    V(lambda e, p=p: e.reciprocal(out=rstd[p][:], in_=rstd[p][:]),
                      reads=[('rstd', p)], writes=[('rstd', p)])
                    V(lambda e, p=p: e.scalar_tensor_tensor(out=nmr[p][:], in0=mv[p][:, 0:1], scalar=-1.0,
                                                            in1=rstd[p][:], op0=ALU.mult, op1=ALU.mult),
                      reads=[('mv', p), ('rstd', p)], writes=[('nmr', p)])
                    A(lambda e, p=p: e.activation(out=yn[p][:], in_=yy[p][:], func=AF.Identity,
                                                  bias=nmr[p][:, 0:1], scale=rstd[p][:, 0:1]),
                      reads=[('yy', p), ('rstd', p), ('nmr', p)], writes=[('yn', p)])
                    G(lambda e, p=p, j=j: e.tensor_tensor(out=yg[p][:], in0=yn[p][:], in1=SG[:, j, :], op=ALU.mult),
                      reads=[('yn', p), ('SG', j)], writes=[('yg', p)])

                    def trY(e, p=p):
                        ins = None
                        for dc in range(2):
                            ins = e.transpose(out=bank_bf(3)[:, dc * 128:(dc + 1) * 128],
                                              in_=yg[p][:, dc * 128:(dc + 1) * 128], identity=identb[:])
                        return ins
                    T(trY, reads=[('yg', p)], writes=[('ps', 3)])
                    for dc in range(2):
                        A(lambda e, h=h, dc=dc, qsl=qsl: e.activation(
                            out=RetT[:, h * 2 + dc, qsl], in_=bank_bf(3)[:, dc * 128:(dc + 1) * 128], func=AF.Copy),
                          reads=[('ps', 3)], writes=[('RetT', j)])
                if n < 31:
                    T(lambda e, p=p, n=n: e.matmul(bank(2)[:, 0:256], lhsT=Kz[p][:], rhs=Vt[:, n, :],
                                                   start=True, stop=True),
                      reads=[('Kz', p), ('V', n)], writes=[('ps', 2)])
                    V(lambda e, h=h: e.scalar_tensor_tensor(out=Rf[:], in0=Rf[:], scalar=rvec[:, 8 + h:9 + h],
                                                            in1=bank(2)[:, 0:256], op0=ALU.mult, op1=ALU.add),
                      reads=['Rf', ('ps', 2), 'rvec'], writes=['Rf'])
                    A(lambda e: e.activation(out=Rb[:], in_=Rf[:], func=AF.Copy), reads=['Rf'], writes=['Rb'])
        S.barrier()

    if dbg_cols:
        for kc in range(8):
            dma('gpsimd', dbg_d[:, 8192 + kc * HALF:8192 + (kc + 1) * HALF], RetT[:, kc, :], semname="outg")
    if stage == 2:
        S.emit(final_waits=[k for k in (('D', 'out'), ('D', 'outg')) if k in S.cnt])
        mix.close()
        root.close()
        return nc

    AttT = sb(mix, "AttT", [128, 4, HALF], BF16)
    with contextlib.ExitStack() as asc:
        EB = sb(asc, "EB", [128, 12, 256], BF16)
        EBm = sb(asc, "EBm", [128, 12, 256], BF16)
        with contextlib.ExitStack() as ebs:
            relb = sb(ebs, "relb", [32, 12], F32)
            ohf = sb(ebs, "ohf", [32, 3, 384], F32)
            jf = sb(ebs, "jf", [128, 128], F32)
            Fsb = sb(ebs, "Fsb", [4, 3, 384], F32)
            Gh = [sb(ebs, "Gh%d" % i, [128, 256], F32) for i in range(2)]
            dma('sync', relb[:], relb_d, writes=['relb'])
            dma('sync', ohf[:], ohf_d, writes=['ohf'])
            dma('sync', jf[:], jf_d, writes=['jf'])
            A(lambda e: e.activation(out=relb[:], in_=relb[:], func=AF.Exp), reads=['relb'], writes=['relb'])
            for g in range(3):
                T(lambda e, g=g: e.matmul(bank(0)[0:4, 0:384], lhsT=relb[:, g * 4:(g + 1) * 4], rhs=ohf[:, g, :],
                                          start=True, stop=True),
                  reads=['relb', 'ohf'], writes=[('ps', 0)])
                V(lambda e, g=g: e.tensor_copy(out=Fsb[:, g, :], in_=bank(0)[0:4, 0:384]),
                  reads=[('ps', 0)], writes=['Fsb'])
            dma('sync', fd_d.rearrange("(g h) m -> h g m", g=3), Fsb[:], reads=['Fsb'], writes=['fd'])
            for idx in range(12):
                hk = bass.AP(tensor=fd_t, offset=idx * 384, ap=[[1, 128], [1, 256]])
                dma('sync', Gh[idx % 2][:], hk, reads=['fd'], writes=[('Gh', idx % 2)], semname="Gh%d" % (idx % 2))
                bk = 1 + idx % 2
                T(lambda e, idx=idx, bk=bk: e.matmul(bank(bk)[:, 0:256], lhsT=jf[:], rhs=Gh[idx % 2][:],
                                                     start=True, stop=True),
                  reads=['jf', ('Gh', idx % 2)], writes=[('ps', bk)])
                V(lambda e, idx=idx, bk=bk: e.tensor_copy(out=EB[:, idx, :], in_=bank(bk)[:, 0:256]),
                  reads=[('ps', bk)], writes=[('EBa', idx)])
                V(lambda e, idx=idx, bk=bk: e.tensor_copy(out=EBm[:, idx, 0:128], in_=bank(bk)[:, 0:128]),
                  reads=[('ps', bk)], writes=[('EBb', idx)])
                V(lambda e, idx=idx, bk=bk: e.tensor_scalar(out=EBm[:, idx, 128:256], in0=bank(bk)[:, 128:256],
                                                            scalar1=flag[:, 0:1], scalar2=None, op0=ALU.mult),
                  reads=[('ps', bk)], writes=[('EBv', idx)])
            S.barrier()

        if dbg_cols:
            for idx in range(12):
                dma('gpsimd', dbg_d[:, 24576 + idx * 256:24576 + (idx + 1) * 256], EB[:, idx, :], semname="outg")
        if stage == 25:
            S.emit(final_waits=[k for k in (('D', 'out'), ('D', 'outg')) if k in S.cnt])
            asc.close()
            mix.close()
            root.close()
            return nc

        qa = [sb(asc, "qa%d" % i, [128, HALF], BF16) for i in range(2)]
        ka = [sb(asc, "ka%d" % i, [128, SEQ], BF16) for i in range(2)]
        VA = sb(asc, "VA", [128, 32, 128], BF16)
        ND = sb(asc, "ND", [128, 2, HALF], F32)
        Et = [sb(asc, "Et%d" % i, [128, 256], F32) for i in range(2)]
        Pt = [sb(asc, "Pt%d" % i, [128, 256], BF16) for i in range(2)]
        blkc = [0]
        items = [(hg, g) for hg in range(4) for g in range(3)]

        def qk_ops(ii):
            hg, g = items[ii]
            par = ii % 2
            qT_, kT_ = qa[par], ka[par]
            ops = []
            st = {}

            def q_op(tg):
                def f():
                    if 'sq' not in st:
                        st['sq'] = ring_get(1)
                    s_q = st['sq']
                    bk = next_pbank()
                    T(fm_mm(bk, s_q, HALF + tg * 512), reads=[('WB', s_q)], writes=[('ps', bk)])
                    A(lambda e: e.activation(out=qT_[:, tg * 512:(tg + 1) * 512], in_=bank(bk)[:, 0:512],
                                             func=AF.Copy, scale=float(128 ** -0.5)),
                      reads=[('ps', bk)], writes=[('qa', par)])
                return f

            def k_op(tg):
                def f():
                    if 'sk' not in st:
                        st['sk'] = ring_get(1)
                    s_k = st['sk']
                    bk = next_pbank()
                    T(fm_mm(bk, s_k, tg * 512), reads=[('WB', s_k)], writes=[('ps', bk)])
                    V(lambda e: e.tensor_copy(out=kT_[:, tg * 512:(tg + 1) * 512], in_=bank(bk)[:, 0:512]),
                      reads=[('ps', bk)], writes=[('ka', par)])
                return f
            for tg in range(4):
                ops.append(q_op(tg))
            for tg in (list(range(8)) if g == 2 else list(range(3, 8))):
                ops.append(k_op(tg))
            return ops

        def v_proj(ii):
            hg, g = items[ii]
            w, r = ATT_PATTERNS[g]
            nbb = 16 // r
            s_v = ring_get(1)
            vblocks = []
            for bb in range(nbb):
                for c in range(r):
                    vblocks.append((bb * r + c, HALF + 128 * r * bb + c))
            for c in range(r):
                vblocks.append((16 + c, 128 * r * (nbb - 1) + c))
            for vi, (idx, st0) in enumerate(vblocks):
                bk = next_pbank()
                tsl = slice(st0, st0 + 127 * r + 1, r)
                T(tm_mm(bk, s_v, tsl, 128), reads=[('WB', s_v)], writes=[('ps', bk)])
                if vi % 2 == 0:
                    A(lambda e, bk=bk, idx=idx: e.activation(out=VA[:, idx, :], in_=bank(bk)[:, 0:128],
                                                             func=AF.Copy),
                      reads=[('ps', bk)], writes=[('VAa', idx)])
                else:
                    V(lambda e, bk=bk, idx=idx: e.tensor_copy(out=VA[:, idx, :], in_=bank(bk)[:, 0:128]),
                      reads=[('ps', bk)], writes=[('VAv', idx)])

        def block_ops(ii):
            hg, g = items[ii]
            w, r = ATT_PATTERNS[g]
            nbb = 16 // r
            par = ii % 2
            qT_, kT_ = qa[par], ka[par]
            ebi = g * 4 + hg
            ops = []
            for bb in range(nbb):
                for c in range(r):
                    def f(bb=bb, c=c):
                        bp = blkc[0] % 2
                        blkc[0] += 1
                        q0 = 128 * r * bb + c
                        qsl = slice(q0, q0 + 127 * r + 1, r)
                        o0 = HALF + q0
                        osl = slice(o0, o0 + 127 * r + 1, r)
                        if bb > 0:
                            p0 = o0 - 128 * r
                            pidx = (bb - 1) * r + c
                        else:
                            p0 = 128 * r * (nbb - 1) + c
                            pidx = 16 + c
                        psl = slice(p0, p0 + 127 * r + 1, r)
                        oidx = bb * r + c
                        bS = 4 + bp
                        bN = 6 + bp

                        def mmS(e):
                            e.matmul(bank(bS)[:, 0:128], lhsT=kT_[:, osl], rhs=qT_[:, qsl], start=True, stop=True)
                            return e.matmul(bank(bS)[:, 128:256], lhsT=kT_[:, psl], rhs=qT_[:, qsl],
                                            start=True, stop=True)
                        T(mmS, reads=[('ka', par), ('qa', par)], writes=[('ps', bS)])
                        A(lambda e: e.activation(out=Et[bp][:], in_=bank(bS)[:, 0:256], func=AF.Exp),
                          reads=[('ps', bS)], writes=[('Et', bp)])
                        EBt = EBm if bb == 0 else EB
                        V(lambda e: e.tensor_tensor(out=Pt[bp][:], in0=Et[bp][:], in1=EBt[:, ebi, :], op=ALU.mult),
                          reads=[('Et', bp)], writes=[('Pt', bp)])

                        def mmN(e):
                            e.matmul(bank(bN)[:, 0:128], lhsT=VA[:, oidx, :], rhs=Pt[bp][:, 0:128],
                                     start=True, stop=False)
                            e.matmul(bank(bN)[:, 0:128], lhsT=VA[:, pidx, :], rhs=Pt[bp][:, 128:256],
                                     start=False, stop=True)
                            e.matmul(bank(bN)[:, 128:256], lhsT=onesb[:], rhs=Pt[bp][:, 0:128],
                                     start=True, stop=False)
                            return e.matmul(bank(bN)[:, 128:256], lhsT=onesb[:], rhs=Pt[bp][:, 128:256],
                                            start=False, stop=True)
                        T(mmN, reads=[('Pt', bp), ('VAa', oidx), ('VAv', oidx), ('VAa', pidx), ('VAv', pidx)],
                          writes=[('ps', bN)])
                        src3 = bank(bN)[:, 0:256].rearrange("p (a q) -> p a q", a=2)
                        if g == 0:
                            V(lambda e: e.tensor_copy(out=ND[:, :, qsl], in_=src3),
                              reads=[('ps', bN)], writes=['ND'])
                        else:
                            V(lambda e: e.tensor_tensor(out=ND[:, :, qsl], in0=src3, in1=ND[:, :, qsl], op=ALU.add),
                              reads=[('ps', bN), 'ND'], writes=['ND'])
                    ops.append(f)
            return ops

        for f in qk_ops(0):
            f()
        v_proj(0)
        for ii in range(len(items)):
            hg, g = items[ii]
            blks = block_ops(ii)
            nxt = qk_ops(ii + 1) if ii + 1 < len(items) else []
            ni = 0
            for bi, f in enumerate(blks):
                f()
                want = (len(nxt) * (bi + 1)) // len(blks)
                while ni < want:
                    nxt[ni]()
                    ni += 1
            while ni < len(nxt):
                nxt[ni]()
                ni += 1
            if g == 2:
                V(lambda e: e.reciprocal(out=ND[:, 1, :], in_=ND[:, 1, :]), reads=['ND'], writes=['ND'])
                V(lambda e, hg=hg: e.tensor_tensor(out=AttT[:, hg, :], in0=ND[:, 0, :], in1=ND[:, 1, :], op=ALU.mult),
                  reads=['ND'], writes=['AttT'])
            if ii + 1 < len(items):
                v_proj(ii + 1)
        S.barrier()

    if dbg_cols:
        for kc in range(4):
            dma('gpsimd', dbg_d[:, 27648 + kc * HALF:27648 + (kc + 1) * HALF], AttT[:, kc, :], semname="outg")
    if stage == 3:
        S.emit(final_waits=[k for k in (('D', 'out'), ('D', 'outg')) if k in S.cnt])
        mix.close()
        root.close()
        return nc

    mrg = contextlib.ExitStack()
    mrg.__enter__()
    MergedT = sb(mrg, "MergedT", [128, 8, HALF], BF16, side="right")
    with contextlib.ExitStack() as msc:
        sgA = [sb(msc, "sgA%d" % i, [128, 512], F32) for i in range(2)]
        sgB = [sb(msc, "sgB%d" % i, [128, 512], F32) for i in range(2)]
        m1 = [sb(msc, "m1_%d" % i, [128, 512], F32) for i in range(2)]
        m2 = [sb(msc, "m2_%d" % i, [128, 512], F32) for i in range(2)]
        WBm = WB + [sb(msc, "WBm%d" % i, [128, 8, 256], BF16) for i in range(4)]
        mloads = loads[N_MAIN_LOADS:]
        assert len(mloads) == 32 and ring['issued'] == ring['consumed'] == N_MAIN_LOADS
        mr = {'issued': 0, 'consumed': 0}

        def mring_get(hold):
            while mr['issued'] < min(len(mloads), mr['consumed'] + 8 - hold + 1):
                n = mr['issued']
                s = n % 8
                src_, kind = mloads[n]
                dst = WBm[s][:, :, 0:128] if kind == 1 else WBm[s][:, 0:4, 0:128]
                dma('gpsimd', dst, src_, writes=[('WBm', s)], semname="WBm%d" % s)
                mr['issued'] += 1
            s = mr['consumed'] % 8
            mr['consumed'] += 1
            return s
        WB_save = WB

        def fm_mm_m(bk, s, col0, nk=8, src=None):
            src = HT if src is None else src

            def f(e):
                ins = None
                for kc in range(nk):
                    ins = e.matmul(bank(bk)[:, 0:512], lhsT=WBm[s][:, kc, 0:128],
                                   rhs=src[:, kc, col0:col0 + 512], start=(kc == 0), stop=(kc == nk - 1))
                return ins
            return f
        it = 0
        for m in range(8):
            s_ga = mring_get(1)
            s_gb = mring_get(2)
            s_wr = mring_get(3)
            s_wa = mring_get(4)
            for tg in range(4):
                p = it % 2
                it += 1
                bs = 0 if p == 0 else 4
                T(fm_mm_m(bs + 0, s_ga, HALF + tg * 512), reads=[('WBm', s_ga)], writes=[('ps', bs + 0)])
                T(fm_mm_m(bs + 1, s_gb, HALF + tg * 512), reads=[('WBm', s_gb)], writes=[('ps', bs + 1)])
                T(fm_mm_m(bs + 2, s_wr, tg * 512, src=RetT), reads=[('WBm', s_wr)], writes=[('ps', bs + 2)])
                T(fm_mm_m(bs + 3, s_wa, tg * 512, nk=4, src=AttT), reads=[('WBm', s_wa)], writes=[('ps', bs + 3)])
                A(lambda e, p=p, bs=bs: e.activation(out=sgA[p][:], in_=bank(bs)[:, 0:512], func=AF.Sigmoid),
                  reads=[('ps', bs)], writes=[('sgA', p)])
                A(lambda e, p=p, bs=bs: e.activation(out=sgB[p][:], in_=bank(bs + 1)[:, 0:512], func=AF.Sigmoid),
                  reads=[('ps', bs + 1)], writes=[('sgB', p)])
                V(lambda e, p=p, bs=bs: e.tensor_tensor(out=m1[p][:], in0=bank(bs + 2)[:, 0:512], in1=sgA[p][:],
                                                        op=ALU.mult),
                  reads=[('ps', bs + 2), ('sgA', p)], writes=[('m1', p)])
                V(lambda e, p=p, bs=bs: e.tensor_tensor(out=m2[p][:], in0=bank(bs + 3)[:, 0:512], in1=sgB[p][:],
                                                        op=ALU.mult),
                  reads=[('ps', bs + 3), ('sgB', p)], writes=[('m2', p)])
                G(lambda e, p=p, m=m, tg=tg: e.tensor_tensor(out=MergedT[:, m, tg * 512:(tg + 1) * 512],
                                                             in0=m1[p][:], in1=m2[p][:], op=ALU.add),
                  reads=[('m1', p), ('m2', p)], writes=[('MergedT', m, tg)])
        S.barrier()
    mix.close()

    X = sb(root, "X", [128, NT, D], F32)
    with contextlib.ExitStack() as p3:
        WO = sb(p3, "WO", [128, 8, D], BF16)
        wst = [sb(p3, "wst%d" % i, [128, D], F32) for i in range(2)]
        for t in range(NT):
            dma('sync', X[:, t, :], x_own[t * 128:(t + 1) * 128, :], writes=[('X', t, 0), ('X', t, 1)])
        for kc in range(8):
            dma('sync', wst[kc % 2][:], wo_d[kc * 128:(kc + 1) * 128, :], writes=[('wst', kc % 2)],
                semname="wst%d" % (kc % 2))
            G(lambda e, kc=kc: e.tensor_tensor(out=WO[:, kc, :], in0=wst[kc % 2][:], in1=gtA_bc[:], op=ALU.mult),
              reads=[('wst', kc % 2)], writes=['WO'])
        for t in range(NT):
            for nh in range(2):
                bk = next_pbank()

                def mmo(e, bk=bk, t=t, nh=nh):
                    ins = None
                    for kc in range(8):
                        ins = e.matmul(bank(bk)[:, 0:512], lhsT=MergedT[:, kc, t * 128:(t + 1) * 128],
                                       rhs=WO[:, kc, nh * 512:(nh + 1) * 512], start=(kc == 0), stop=(kc == 7))
                    return ins
                T(mmo, reads=['WO'], writes=[('ps', bk)])
                V(lambda e, bk=bk, t=t, nh=nh: e.tensor_tensor(out=X[:, t, nh * 512:(nh + 1) * 512],
                                                               in0=bank(bk)[:, 0:512],
                                                               in1=X[:, t, nh * 512:(nh + 1) * 512], op=ALU.add),
                  reads=[('ps', bk), ('X', t, nh)], writes=[('X', t, nh)])
        S.barrier()

    mrg.close()

    if dbg_cols:
        for t in range(NT):
            dma('sync', dbg_d[:, 35840 + t * D:35840 + (t + 1) * D], X[:, t, :], semname="out")
    if stage == 4:
        S.emit(final_waits=[k for k in (('D', 'out'), ('D', 'outg')) if k in S.cnt])
        root.close()
        return nc

    H2T = sb(root, "H2T", [128, 8, HALF], BF16)
    Gt = sb(root, "Gt", [128, NT, N_EXP], F32)
    with contextlib.ExitStack() as p4:
        xnf = [sb(p4, "xnf%d" % i, [128, D], F32) for i in range(3)]
        h2f = [sb(p4, "h2f%d" % i, [128, 8, 128], F32) for i in range(3)]
        junk4 = sb(p4, "junk4", [128, D], BF16)
        WR = sb(p4, "WR", [128, 8, N_EXP], F32)
        brbc = sb(p4, "brbc", [128, N_EXP], F32)
        Lg = [sb(p4, "Lg%d" % i, [128, N_EXP], F32) for i in range(2)]
        m8 = [sb(p4, "m8_%d" % i, [128, 8], F32) for i in range(2)]
        ngm = [sb(p4, "ngm%d" % i, [128, 1], F32) for i in range(2)]
        msk = [sb(p4, "msk%d" % i, [128, N_EXP], F32) for i in range(2)]
        Ex = [sb(p4, "Ex%d" % i, [128, N_EXP], F32) for i in range(2)]
        sm = [sb(p4, "sm%d" % i, [128, 1], F32) for i in range(2)]
        dma('sync', WR[:], wr_d.rearrange("(kc p) n -> p kc n", p=128), writes=['WR'])
        dma('sync', brbc[:], brbc_d, writes=['brbc'])
        NP4 = 3

        def s4_sq(t):
            i = 32 + t
            A(lambda e: e.activation(out=junk4[:], in_=X[:, t, :], func=AF.Square, accum_out=ss[:, i:i + 1]),
              reads=[('X', t, 0), ('X', t, 1)], writes=['junk4', ('ss', i)])

        def s4_ts(t):
            i = 32 + t
            V(lambda e: e.tensor_scalar(out=rs[:, i:i + 1], in0=ss[:, i:i + 1], scalar1=1.0 / D, scalar2=EPS,
                                        op0=ALU.mult, op1=ALU.add),
              reads=[('ss', i)], writes=[('rs', i)])

        def s4_sqrt(t):
            i = 32 + t
            A(lambda e: e.activation(out=rs[:, i:i + 1], in_=rs[:, i:i + 1], func=AF.Sqrt),
              reads=[('rs', i)], writes=[('rs', i)])

        def s4_xn(t):
            i = 32 + t
            p = t % NP4
            V(lambda e: e.reciprocal(out=rs[:, i:i + 1], in_=rs[:, i:i + 1]),
              reads=[('rs', i)], writes=[('rs', i)])
            V(lambda e: e.tensor_scalar(out=xnf[p][:], in0=X[:, t, :], scalar1=rs[:, i:i + 1],
                                        scalar2=None, op0=ALU.mult),
              reads=[('X', t, 0), ('X', t, 1), ('rs', i)], writes=[('xnf', p)])

        def s4_b0(t):
            return (0, 2, 6)[t % NP4]

        def s4_tr(t):
            p = t % NP4
            b0 = s4_b0(t)

            def tr2(e):
                ins = None
                for kc in range(8):
                    ins = e.transpose(out=PS[:, b0 + kc // 4, (kc % 4) * 128:(kc % 4 + 1) * 128],
                                      in_=xnf[p][:, kc * 128:(kc + 1) * 128], identity=identf[:])
                return ins
            T(tr2, reads=[('xnf', p)], writes=[('ps', b0), ('ps', b0 + 1)])

        def s4_ev(t):
            p = t % NP4
            b0 = s4_b0(t)
            for kc in range(8):
                srcp = PS[:, b0 + kc // 4, (kc % 4) * 128:(kc % 4 + 1) * 128]
                if t % 2 == 0:
                    A(lambda e, srcp=srcp, kc=kc: e.activation(out=h2f[p][:, kc, :], in_=srcp, func=AF.Identity,
                                                               bias=shF[:, kc:kc + 1], scale=scF[:, kc:kc + 1]),
                      reads=[('ps', b0), ('ps', b0 + 1)], writes=[('h2fa', p)])
                else:
                    V(lambda e, srcp=srcp, kc=kc: e.tensor_scalar(out=h2f[p][:, kc, :], in0=srcp,
                                                                  scalar1=scF[:, kc:kc + 1],
                                                                  scalar2=shF[:, kc:kc + 1],
                                                                  op0=ALU.mult, op1=ALU.add),
                      reads=[('ps', b0), ('ps', b0 + 1)], writes=[('h2fv', p)])

        def s4_rt(t):
            p = t % NP4
            G(lambda e: e.tensor_copy(out=H2T[:, :, t * 128:(t + 1) * 128], in_=h2f[p][:]),
              reads=[('h2fa', p), ('h2fv', p)], writes=[('H2T', t)])
            bl = 4 + t % 2

            def mmr(e):
                ins = None
                for kc in range(8):
                    ins = e.matmul(bank(bl)[:, 0:N_EXP], lhsT=h2f[p][:, kc, :], rhs=WR[:, kc, :],
                                   start=(kc == 0), stop=(kc == 7))
                return ins
            T(mmr, reads=[('h2fa', p), ('h2fv', p), 'WR'], writes=[('ps', bl)])

        def s4_gate(t):
            q = t % 2
            bl = 4 + t % 2
            V(lambda e: e.tensor_tensor(out=Lg[q][:], in0=bank(bl)[:, 0:N_EXP], in1=brbc[:], op=ALU.add),
              reads=[('ps', bl), 'brbc'], writes=[('Lg', q)])
            V(lambda e: e.max(out=m8[q][:], in_=Lg[q][:]), reads=[('Lg', q)], writes=[('m8', q)])
            V(lambda e: e.tensor_scalar(out=ngm[q][:], in0=m8[q][:, 0:1], scalar1=-1.0, scalar2=None, op0=ALU.mult),
              reads=[('m8', q)], writes=[('ngm', q)])
            V(lambda e: e.tensor_scalar(out=msk[q][:], in0=Lg[q][:], scalar1=m8[q][:, 3:4], scalar2=None,
                                        op0=ALU.is_ge),
              reads=[('Lg', q), ('m8', q)], writes=[('msk', q)])
            A(lambda e: e.activation(out=Ex[q][:], in_=Lg[q][:], func=AF.Exp, bias=ngm[q][:, 0:1], scale=1.0),
              reads=[('Lg', q), ('ngm', q)], writes=[('Ex', q)])

        def s4_gate2(t):
            q = t % 2
            V(lambda e: e.tensor_tensor(out=Ex[q][:], in0=Ex[q][:], in1=msk[q][:], op=ALU.mult),
              reads=[('Ex', q), ('msk', q)], writes=[('Ex', q)])
            V(lambda e: e.tensor_reduce(out=sm[q][:], in_=Ex[q][:], axis=AX.X, op=ALU.add),
              reads=[('Ex', q)], writes=[('sm', q)])
            V(lambda e: e.reciprocal(out=sm[q][:], in_=sm[q][:]), reads=[('sm', q)], writes=[('sm', q)])
            V(lambda e: e.tensor_scalar(out=Gt[:, t, :], in0=Ex[q][:], scalar1=sm[q][:, 0:1], scalar2=None,
                                        op0=ALU.mult),
              reads=[('Ex', q), ('sm', q)], writes=[('Gt', t)])
        stages4 = [(s4_sq, 0), (s4_ts, 1), (s4_sqrt, 2), (s4_xn, 3), (s4_tr, 4), (s4_ev, 5), (s4_rt, 6),
                   (s4_gate, 7), (s4_gate2, 8)]
        for j in range(NT + 8):
            for fn, lag in stages4:
                t = j - lag
                if 0 <= t < NT:
                    fn(t)
        S.barrier()

    if dbg_cols:
        dma('sync', dbg_d[:, 52224:52224 + NT * N_EXP], Gt[:].rearrange("p t e -> p (t e)"), semname="out")
        dma('gpsimd', dbg_d[:, 52736:52736 + HALF], H2T[:, 0, :], semname="outg")
    if stage == 5:
        S.emit(final_waits=[k for k in (('D', 'out'), ('D', 'outg')) if k in S.cnt])
        root.close()
        return nc

    with contextlib.ExitStack() as p5:
        aT = sb(p5, "aT", [128, 8, HALF], BF16)
        W2 = sb(p5, "W2", [128, 8, D], BF16)
        W1 = [sb(p5, "W1_%d" % i, [128, 8, 256], BF16) for i in range(3)]
        w2st = [sb(p5, "w2st%d" % i, [128, D], F32) for i in range(2)]
        b1 = sb(p5, "b1", [128, N_EXP, 8, 2], F32)
        tA = [sb(p5, "tA%d" % i, [128, 512], F32) for i in range(2)]
        tB = [sb(p5, "tB%d" % i, [128, 512], F32) for i in range(2)]
        tC = [sb(p5, "tC%d" % i, [128, 512], F32) for i in range(2)]
        tD = [sb(p5, "tD%d" % i, [128, 512], F32) for i in range(2)]
        dma('sync', b1[:], b1r_d, writes=['b1'])
        n_exp_run = N_EXP
        w1n = [0]
        total_w1 = n_exp_run * 8

        def w1_issue():
            n = w1n[0]
            e_, fc_ = n // 8, n % 8
            s = n % 3
            dma('gpsimd', W1[s][:], w1r_d[e_, fc_], writes=[('W1', s)], semname="W1_%d" % s)
            w1n[0] += 1
        w1c = [0]

        def w1_get():
            while w1n[0] < min(total_w1, w1c[0] + 3):
                w1_issue()
            s = w1c[0] % 3
            w1c[0] += 1
            return s
        ac = [0]
        sc_ = [0]
        for ex in range(n_exp_run):
            def w2_dma(fc2, ex=ex):
                q = (ex * 8 + fc2) % 2
                dma('sync', w2st[q][:], w2_d[ex, fc2 * 128:(fc2 + 1) * 128, :], writes=[('w2st', q)],
                    semname="w2st%d" % q)

            def w2_chunk(fc2, ex=ex):
                q = (ex * 8 + fc2) % 2
                if fc2 + 1 < 8:
                    w2_dma(fc2 + 1)
                V(lambda e: e.tensor_tensor(out=W2[:, fc2, :], in0=w2st[q][:], in1=gtF_bc[:], op=ALU.mult),
                  reads=[('w2st', q)], writes=['W2'])
            w2_dma(0)
            w2_chunk(0)
            for fc in range(8):
                s = w1_get()
                for tg in range(4):
                    p = ac[0] % 2
                    ac[0] += 1
                    bg, bl_ = (0, 1) if p == 0 else (2, 3)

                    def mm1(e, s=s, tg=tg, bg=bg, bl_=bl_):
                        ins = None
                        for kc in range(8):
                            e.matmul(bank(bg)[:, 0:512], lhsT=W1[s][:, kc, 0:128],
                                     rhs=H2T[:, kc, tg * 512:(tg + 1) * 512], start=(kc == 0), stop=(kc == 7))
                        for kc in range(8):
                            ins = e.matmul(bank(bl_)[:, 0:512], lhsT=W1[s][:, kc, 128:256],
                                           rhs=H2T[:, kc, tg * 512:(tg + 1) * 512], start=(kc == 0), stop=(kc == 7))
                        return ins
                    T(mm1, reads=[('W1', s)], writes=[('ps', bg), ('ps', bl_)])
                    V(lambda e, p=p, bg=bg, ex=ex, fc=fc: e.tensor_scalar(
                        out=tA[p][:], in0=bank(bg)[:, 0:512], scalar1=b1[:, ex, fc, 0:1], scalar2=7.0,
                        op0=ALU.add, op1=ALU.min),
                      reads=[('ps', bg), 'b1'], writes=[('tA', p)])
                    A(lambda e, p=p: e.activation(out=tB[p][:], in_=tA[p][:], func=AF.Sigmoid, scale=1.702),
                      reads=[('tA', p)], writes=[('tB', p)])
                    V(lambda e, p=p, bl_=bl_, ex=ex, fc=fc: e.tensor_scalar(
                        out=tC[p][:], in0=bank(bl_)[:, 0:512], scalar1=b1[:, ex, fc, 1:2], scalar2=7.0,
                        op0=ALU.add, op1=ALU.min),
                      reads=[('ps', bl_), 'b1'], writes=[('tC', p)])
                    V(lambda e, p=p: e.tensor_scalar(out=tC[p][:], in0=tC[p][:], scalar1=-7.0, scalar2=1.0,
                                                     op0=ALU.max, op1=ALU.add),
                      reads=[('tC', p)], writes=[('tC', p)])
                    G(lambda e, p=p: e.tensor_tensor(out=tD[p][:], in0=tA[p][:], in1=tB[p][:], op=ALU.mult),
                      reads=[('tA', p), ('tB', p)], writes=[('tD', p)])
                    G(lambda e, p=p, fc=fc, tg=tg: e.tensor_tensor(out=aT[:, fc, tg * 512:(tg + 1) * 512],
                                                                   in0=tD[p][:], in1=tC[p][:], op=ALU.mult),
                      reads=[('tD', p), ('tC', p)], writes=[('aT', tg)])
                if fc + 1 < 8:
                    w2_chunk(fc + 1)
            for t in range(NT):
                for nh in range(2):
                    bk = 4 + sc_[0] % 2
                    sc_[0] += 1

                    def mm2(e, bk=bk, t=t, nh=nh):
                        ins = None
                        for fc in range(8):
                            ins = e.matmul(bank(bk)[:, 0:512], lhsT=aT[:, fc, t * 128:(t + 1) * 128],
                                           rhs=W2[:, fc, nh * 512:(nh + 1) * 512], start=(fc == 0), stop=(fc == 7))
                        return ins
                    T(mm2, reads=[('aT', t // 4), 'W2'], writes=[('ps', bk)])
                    V(lambda e, bk=bk, t=t, nh=nh, ex=ex: e.scalar_tensor_tensor(
                        out=X[:, t, nh * 512:(nh + 1) * 512], in0=bank(bk)[:, 0:512], scalar=Gt[:, t, ex:ex + 1],
                        in1=X[:, t, nh * 512:(nh + 1) * 512], op0=ALU.mult, op1=ALU.add),
                      reads=[('ps', bk), ('X', t, nh)], writes=[('X', t, nh)])
        S.barrier()

    with contextlib.ExitStack() as p5b:
        B2s = sb(p5b, "B2s", [N_EXP, D], F32)
        GT = sb(p5b, "GT", [N_EXP, NT, 128], F32)
        dma('sync', B2s[:], b2_d, writes=['B2s'])
        V(lambda e: e.tensor_tensor(out=B2s[:], in0=B2s[:], in1=gtF_bc[0:N_EXP, :], op=ALU.mult),
          reads=['B2s'], writes=['B2s'])
        for t in range(NT):
            bk = 6 + t % 2
            T(lambda e, t=t, bk=bk: e.transpose(out=bank(bk)[0:N_EXP, 0:128], in_=Gt[:, t, :], identity=identf[:]),
              writes=[('ps', bk)])
            V(lambda e, t=t, bk=bk: e.tensor_copy(out=GT[:, t, :], in_=bank(bk)[0:N_EXP, 0:128]),
              reads=[('ps', bk)], writes=[('GT', t)])
        for t in range(NT):
            for nh in range(2):
                bk = next_pbank()
                T(lambda e, t=t, nh=nh, bk=bk: e.matmul(bank(bk)[:, 0:512], lhsT=GT[:, t, :],
                                                        rhs=B2s[:, nh * 512:(nh + 1) * 512], start=True, stop=True),
                  reads=[('GT', t), 'B2s'], writes=[('ps', bk)])
                V(lambda e, t=t, nh=nh, bk=bk: e.tensor_tensor(out=X[:, t, nh * 512:(nh + 1) * 512],
                                                               in0=bank(bk)[:, 0:512],
                                                               in1=X[:, t, nh * 512:(nh + 1) * 512], op=ALU.add),
                  reads=[('ps', bk), ('X', t, nh)], writes=[('X', t, nh)])
        S.barrier()

    with contextlib.ExitStack() as p6:
        gfin = sb(p6, "gfin", [128, D], F32)
        ot = [sb(p6, "ot%d" % i, [128, D], F32) for i in range(3)]
        junk6 = sb(p6, "junk6", [128, D], BF16)
        dma('sync', gfin[:], gfin_d, writes=['gfin'])
        NO6 = 3

        def s6_sq(t):
            i = 48 + t
            A(lambda e: e.activation(out=junk6[:], in_=X[:, t, :], func=AF.Square, accum_out=ss[:, i:i + 1]),
              reads=[('X', t, 0), ('X', t, 1)], writes=['junk6', ('ss', i)])

        def s6_ts(t):
            i = 48 + t
            V(lambda e: e.tensor_scalar(out=rs[:, i:i + 1], in0=ss[:, i:i + 1], scalar1=1.0 / D, scalar2=EPS,
                                        op0=ALU.mult, op1=ALU.add),
              reads=[('ss', i)], writes=[('rs', i)])

        def s6_sqrt(t):
            i = 48 + t
            A(lambda e: e.activation(out=rs[:, i:i + 1], in_=rs[:, i:i + 1], func=AF.Sqrt),
              reads=[('rs', i)], writes=[('rs', i)])

        def s6_out(t):
            i = 48 + t
            p = t % NO6
            V(lambda e: e.reciprocal(out=rs[:, i:i + 1], in_=rs[:, i:i + 1]),
              reads=[('rs', i)], writes=[('rs', i)])
            V(lambda e: e.scalar_tensor_tensor(out=ot[p][:], in0=X[:, t, :], scalar=rs[:, i:i + 1],
                                               in1=gfin[:], op0=ALU.mult, op1=ALU.mult),
              reads=[('X', t, 0), ('X', t, 1), ('rs', i), 'gfin'], writes=[('ot', p)])
            dma('sync', out_d[t * 128:(t + 1) * 128, :], ot[p][:], reads=[('ot', p)], semname="out%d" % p)
        stages6 = [(s6_sq, 0), (s6_ts, 1), (s6_sqrt, 2), (s6_out, 3)]
        for j in range(NT + 3):
            for fn, lag in stages6:
                t = j - lag
                if 0 <= t < NT:
                    fn(t)
    S.emit(final_waits=[k for k in (('D', 'out'), ('D', 'outg'), ('D', 'out0'), ('D', 'out1'), ('D', 'out2')) if k in S.cnt])
    root.close()
    return nc


def _t5_bucket(dist):
    dist = np.asarray(dist, dtype=np.int64)
    small = dist < 16
    ratio = np.log(np.maximum(dist, 1).astype(np.float32) / np.float32(16)) / np.float32(math.log(2048 / 16))
    large = 16 + (ratio.astype(np.float32) * np.float32(16)).astype(np.int32)
    large = np.minimum(large, 31)
    return np.where(small, dist, large)


def _constants():
    cst = {}
    half = 64
    inv = (10000.0 ** (-np.arange(half, dtype=np.float32) / half)).astype(np.float32)
    pos = np.arange(SEQ, dtype=np.float32)
    ang = pos[None, :] * inv[:, None]
    cos = np.cos(ang).astype(np.float32)
    sin = np.sin(ang).astype(np.float32)
    cst['cos_full'] = np.concatenate([cos, cos], axis=0)
    cst['sin_full'] = np.concatenate([-sin, sin], axis=0)
    Hh = 4
    log_g = np.log1p(-np.exp2(-5.0 - np.arange(Hh, dtype=np.float64)))
    i = np.arange(128, dtype=np.float64)
    scale = 128 ** -0.5
    dm = np.zeros((128, 4, 128), np.float32)
    for h in range(Hh):
        diff = i[None, :] - i[:, None]
        dm[:, h, :] = np.where(diff >= 0, np.exp(np.maximum(diff, 0) * log_g[h]) * scale, 0.0)
    rvec = np.zeros((128, 16), np.float32)
    for h in range(Hh):
        rvec[:, h] = np.exp((127.0 - i) * log_g[h]) * scale
        rvec[:, 4 + h] = np.exp((i + 1.0) * log_g[h])
        rvec[:, 8 + h] = np.exp(128.0 * log_g[h])
    cst['dmask'] = dm
    cst['rvec'] = rvec
    z2 = np.zeros((128, 4, 16), np.float32)
    for h in range(Hh):
        for n in range(16):
            z2[:, h, n] = np.exp((127.0 - i) * log_g[h] + 128.0 * (15 - n) * log_g[h]) * scale
    cst['zeta2'] = z2
    ohf = np.zeros((32, 3, 384), np.float32)
    for g, (w, r) in enumerate(ATT_PATTERNS):
        j = np.arange(129)
        bk = _t5_bucket(r * j)
        for jj in range(129):
            ohf[bk[jj], g, 127 + jj] = 1.0
    cst['ohf'] = ohf
    cst['identf'] = np.eye(128, dtype=np.float32)
    cst['jf'] = np.ascontiguousarray(np.eye(128, dtype=np.float32)[::-1])
    return cst


def _prep_shared(inp):
    sh = {}
    w_in = inp['w_in'][0]

    def chunk(cols):
        return w_in[:, cols].reshape(8, 128, len(cols)).transpose(1, 0, 2)

    def rng(a, n=128):
        return np.arange(a, a + n)
    r1 = []
    r2 = []
    for h in range(4):
        q0 = C_RQ + h * 128
        k0 = C_RK + h * 128
        r1.append(chunk(rng(q0)))
        r1.append(chunk(np.concatenate([rng(q0 + 64, 64), rng(q0, 64)])))
        r1.append(chunk(rng(k0)))
        r1.append(chunk(np.concatenate([rng(k0 + 64, 64), rng(k0, 64)])))
        r2.append(chunk(rng(C_RV + h * 256, 256)))
        r2.append(chunk(rng(C_RG + h * 256, 256)))
    for hg in range(4):
        for g in range(3):
            hd = g * 4 + hg
            r1.append(chunk(rng(C_AQ + hd * 128)))
            r1.append(chunk(rng(C_AK + hd * 128)))
            r1.append(chunk(rng(C_AV + hd * 128)))
    for m in range(8):
        r1.append(chunk(rng(C_GA + m * 128)))
    for m in range(8):
        r1.append(chunk(rng(C_GB + m * 128)))
    sh['w_in_r1'] = np.ascontiguousarray(np.stack(r1, axis=0))
    sh['w_in_r2'] = np.ascontiguousarray(np.stack(r2, axis=0))
    b_ada = inp['b_ada'][0]
    secs = [0, 1, 3, 4]
    bc = np.zeros((128, 32), np.float32)
    for si, s in enumerate(secs):
        bc[:, si * 8:(si + 1) * 8] = b_ada[s * 1024:(s + 1) * 1024].reshape(8, 128).T
    sh['bada_col'] = bc
    bb = np.stack([b_ada[2048:3072], b_ada[5120:6144]], axis=0)
    sh['bada_bc'] = np.ascontiguousarray(np.broadcast_to(bb[None], (128, 2, 1024)))
    sh['gmix_col'] = np.ascontiguousarray(inp['g_mix'][0].reshape(8, 128).T)
    sh['gffn_col'] = np.ascontiguousarray(inp['g_ffn'][0].reshape(8, 128).T)
    sh['gfin_bc'] = np.ascontiguousarray(np.broadcast_to(inp['g_final'][None, :], (128, 1024)))
    sh['w_ada'] = np.ascontiguousarray(inp['w_ada'][0])
    sh['rel_bias'] = np.ascontiguousarray(inp['rel_bias'])
    sh['w_ret_o'] = np.ascontiguousarray(inp['w_ret_o'][0])
    sh['w_att_o'] = np.ascontiguousarray(inp['w_att_o'][0])
    sh['w_o'] = np.ascontiguousarray(inp['w_o'][0])
    sh['w_router'] = np.ascontiguousarray(inp['w_router'][0])
    sh['brouter_bc'] = np.ascontiguousarray(np.broadcast_to(inp['b_router'][0][None, :], (128, 32)))
    w1 = inp['w_mlp1'][0].reshape(32, 8, 128, 8, 128, 2)
    sh['w1r'] = np.ascontiguousarray(w1.transpose(0, 3, 2, 1, 5, 4)).reshape(32, 8, 128, 8, 256)
    sh['b1r'] = np.ascontiguousarray(inp['b_mlp1'][0].reshape(32, 8, 128, 2).transpose(2, 0, 1, 3))
    sh['w_mlp2'] = np.ascontiguousarray(inp['w_mlp2'][0])
    sh['b_mlp2'] = np.ascontiguousarray(inp['b_mlp2'][0])
    return sh


def make_in_maps(inp):
    inp = {k: np.asarray(v, dtype=np.float32) for k, v in inp.items()}
    cst = _constants()
    sh = _prep_shared(inp)
    x = inp['x']
    c = inp['c']
    zeros_prev = np.zeros((HALF, D), np.float32)
    in_maps = []
    for core in range(8):
        b, hf = core // 2, core % 2
        m = dict(sh)
        m['x_own'] = np.ascontiguousarray(x[b, hf * HALF:(hf + 1) * HALF])
        m['x_prev'] = np.ascontiguousarray(x[b, 0:HALF]) if hf == 1 else zeros_prev
        m['flag'] = np.full((128, 1), float(hf), np.float32)
        m['c_col'] = np.ascontiguousarray(c[b].reshape(8, 128).T)
        if hf == 1:
            m['rope_cos'] = cst['cos_full']
            m['rope_sin'] = cst['sin_full']
        else:
            m['rope_cos'] = np.ascontiguousarray(np.concatenate([cst['cos_full'][:, :HALF]] * 2, axis=1))
            m['rope_sin'] = np.ascontiguousarray(np.concatenate([cst['sin_full'][:, :HALF]] * 2, axis=1))
        for k in ('dmask', 'rvec', 'zeta2', 'ohf', 'identf', 'jf'):
            m[k] = cst[k]
        in_maps.append(m)
    return in_maps


def kernel(**inputs):
    in_maps = make_in_maps(inputs)
    nc = build_program()
    res = run_bass_kernel_spmd(nc, in_maps, core_ids=list(range(8)))
    out = np.zeros((4, SEQ, D), np.float32)
    for core in range(8):
        b, hf = core // 2, core % 2
        out[b, hf * HALF:(hf + 1) * HALF] = res.results[core]["out"]
    return out
```

```python
import contextlib
import math
import numpy as np
import concourse.bass as bass
import concourse.mybir as mybir
from concourse.bass_utils import run_bass_kernel_spmd

F32 = mybir.dt.float32
BF16 = mybir.dt.bfloat16
AF = mybir.ActivationFunctionType
ALU = mybir.AluOpType
AX = mybir.AxisListType

D = 1024
SEQ = 4096
HALF = 2048
NT = 16
EPS = 1e-6
GN_EPS = 1e-5
N_EXP = 32
ATT_PATTERNS = ((128, 1), (512, 4), (2048, 16))
NSLOT = 4

C_RQ, C_RK, C_RV, C_RG = 0, 512, 1024, 2048
C_AQ, C_AK, C_AV = 3072, 4608, 6144
C_GA, C_GB = 7680, 8704


class Sched:
    ENG = ['sync', 'scalar', 'vector', 'gpsimd', 'tensor']

    def __init__(self, nc):
        self.nc = nc
        self.ops = {e: [] for e in self.ENG}
        self.cnt = {}
        self.waited = {e: {} for e in self.ENG}
        self.lastw = {}
        self.readers = {}

    def op(self, eng, fn, reads=(), writes=(), sem=None, inc=1):
        deps = {}
        own = ('E', eng)

        def add(d, raw):
            if d is None:
                return
            k, v = d
            if k == own and not raw:
                return
            if deps.get(k, 0) < v:
                deps[k] = v
        for b in reads:
            add(self.lastw.get(b), True)
        waw_own = eng in ('vector', 'gpsimd', 'scalar')
        for b in writes:
            add(self.lastw.get(b), waw_own)
            for r in self.readers.get(b, ()):
                add(r, False)
        waits = []
        for k, v in deps.items():
            if self.waited[eng].get(k, 0) < v:
                self.waited[eng][k] = v
                waits.append((k, v))
        semkey = sem if sem is not None else own
        self.cnt[semkey] = self.cnt.get(semkey, 0) + inc
        val = self.cnt[semkey]
        self.ops[eng].append((waits, fn, semkey, inc))
        for b in reads:
            self.readers.setdefault(b, []).append((semkey, val))
        for b in writes:
            self.lastw[b] = (semkey, val)
            self.readers[b] = []
        return val

    def barrier(self):
        for e in self.ENG:
            waits = []
            for k, v in self.cnt.items():
                if self.waited[e].get(k, 0) < v:
                    self.waited[e][k] = v
                    waits.append((k, v))
            if waits:
                self.ops[e].append((waits, None, None, 0))
        self.lastw = {}
        self.readers = {}

    def emit(self, final_waits=()):
        nc = self.nc
        keys = list(self.cnt.keys())
        with contextlib.ExitStack() as st:
            sems = {}
            for i, k in enumerate(keys):
                sems[k] = st.enter_context(nc.semaphore("s%d" % i))
            block = st.enter_context(nc.Block())

            def body(engname):
                def f(e):
                    for waits, fn, semkey, inc in self.ops[engname]:
                        for k, v in waits:
                            e.wait_ge(sems[k], v)
                        if fn is None:
                            continue
                        ins = fn(e)
                        ins.then_inc(sems[semkey], inc)
                    if engname == 'sync':
                        for k in final_waits:
                            e.wait_ge(sems[k], self.cnt[k])
                return f
            block.sync(body('sync'))
            block.scalar(body('scalar'))
            block.vector(body('vector'))
            block.gpsimd(body('gpsimd'))
            block.tensor(body('tensor'))


def build_program(stage=99, dbg_cols=0):
    nc = bass.Bass("TRN2", target_bir_lowering=False)
    S = Sched(nc)

    def din(name, shape, dt=F32):
        return nc.dram_tensor(name, list(shape), dt, kind="ExternalInput").ap()

    x_own = din("x_own", [HALF, D])
    x_prev = din("x_prev", [HALF, D])
    flag_d = din("flag", [128, 1])
    ccol_d = din("c_col", [128, 8])
    w_ada_d = din("w_ada", [D, 6 * D])
    badacol_d = din("bada_col", [128, 32])
    badabc_d = din("bada_bc", [128, 2, D])
    gmix_d = din("gmix_col", [128, 8])
    gffn_d = din("gffn_col", [128, 8])
    gfin_d = din("gfin_bc", [128, D])
    win1_d = din("w_in_r1", [68, 128, 8, 128])
    win2_d = din("w_in_r2", [8, 128, 8, 256])
    ropec_d = din("rope_cos", [128, SEQ])
    ropes_d = din("rope_sin", [128, SEQ])
    dm_d = din("dmask", [128, 4, 128])
    rvec_d = din("rvec", [128, 16])
    zeta2_d = din("zeta2", [128, 4, 16])
    relb_d = din("rel_bias", [32, 12])
    ohf_d = din("ohf", [32, 3, 384])
    identf_d = din("identf", [128, 128])
    jf_d = din("jf", [128, 128])
    wreto_d = din("w_ret_o", [D, D])
    watto_d = din("w_att_o", [512, D])
    wo_d = din("w_o", [D, D])
    wr_d = din("w_router", [D, N_EXP])
    brbc_d = din("brouter_bc", [128, N_EXP])
    if stage > 5:
        w1r_d = din("w1r", [N_EXP, 8, 128, 8, 256])
        b1r_d = din("b1r", [128, N_EXP, 8, 2])
        w2_d = din("w_mlp2", [N_EXP, D, D])
        b2_d = din("b_mlp2", [N_EXP, D])
    out_d = nc.dram_tensor("out", [HALF, D], F32, kind="ExternalOutput").ap()
    fd_t = nc.dram_tensor("fd_scratch", [12, 384], F32)
    fd_d = fd_t.ap()
    dbg_d = None
    if dbg_cols:
        dbg_d = nc.dram_tensor("dbg", [128, dbg_cols], F32, kind="ExternalOutput").ap()

    root = contextlib.ExitStack()
    root.__enter__()

    def sb(st, name, shape, dt, side=None):
        name = "sb_" + name
        if side is None:
            return st.enter_context(nc.sbuf_tensor(name, list(shape), dt))
        return st.enter_context(nc.sbuf_tensor(name, list(shape), dt, side=side))

    PS = root.enter_context(nc.psum_tensor("psall", [128, 8, 512], F32))

    def bank(i):
        return PS[:, i, :]

    def bank_bf(i):
        return PS[:, i, :].bitcast(BF16)

    dma_n = [0]

    def dma(eng, out, in_, reads=(), writes=(), semname=None):
        if semname is None:
            semname = "u%d" % dma_n[0]
            dma_n[0] += 1
        return S.op(eng, lambda e: e.dma_start(out=out, in_=in_), reads=reads, writes=writes,
                    sem=('D', semname), inc=16)

    def V(fn, reads=(), writes=()):
        return S.op('vector', fn, reads, writes)

    def A(fn, reads=(), writes=()):
        return S.op('scalar', fn, reads, writes)

    def G(fn, reads=(), writes=()):
        return S.op('gpsimd', fn, reads, writes)

    def T(fn, reads=(), writes=()):
        return S.op('tensor', fn, reads, writes)

    flag = sb(root, "flag", [128, 1], F32)
    identb = sb(root, "identb", [128, 128], BF16)
    identf = sb(root, "identf", [128, 128], F32)
    onesb = sb(root, "onesb", [128, 128], BF16)
    onesf = sb(root, "onesf", [128, 128], F32)
    modcol = sb(root, "modcol", [128, 32], F32)
    scA = sb(root, "scA", [128, 8], F32)
    scAp = sb(root, "scAp", [128, 8], F32)
    shAp = sb(root, "shAp", [128, 8], F32)
    scF = sb(root, "scF", [128, 8], F32)
    gtA_bc = sb(root, "gtA_bc", [128, D], F32)
    gtF_bc = sb(root, "gtF_bc", [128, D], F32)
    ss = sb(root, "ss", [128, 64], F32)
    rs = sb(root, "rs", [128, 64], F32)
    shA = modcol[:, 0:8]
    shF = modcol[:, 16:24]

    dma('sync', flag[:], flag_d, writes=['flag'])
    dma('sync', identf[:], identf_d, writes=['identf'])
    dma('gpsimd', identb[:], identf_d, writes=['identb'])
    V(lambda e: e.memset(onesb[:], 1.0), writes=['onesb'])
    V(lambda e: e.memset(onesf[:], 1.0), writes=['onesf'])

    with contextlib.ExitStack() as p0:
        ccol = sb(p0, "ccol", [128, 8], F32)
        cs2 = sb(p0, "cs2", [128, 8, 2], F32)
        csb = sb(p0, "csb", [128, 8, 128], F32)
        badacol = sb(p0, "badacol", [128, 32], F32)
        badabc = sb(p0, "badabc", [128, 2, D], F32)
        gmix = sb(p0, "gmix", [128, 8], F32)
        gffn = sb(p0, "gffn", [128, 8], F32)
        wast = [sb(p0, "wast%d" % i, [128, 8, 512], F32) for i in range(3)]
        dma('sync', ccol[:], ccol_d, writes=['ccol'])
        dma('sync', badacol[:], badacol_d, writes=['badacol'])
        dma('sync', badabc[:], badabc_d, writes=['badabc'])
        dma('sync', gmix[:], gmix_d, writes=['gmix'])
        dma('sync', gffn[:], gffn_d, writes=['gffn'])
        for j in range(2):
            A(lambda e, j=j: e.activation(out=cs2[:, :, j], in_=ccol[:], func=AF.Silu),
              reads=['ccol'], writes=[('cs2', j)])
        for kc in range(8):
            V(lambda e, kc=kc: e.tensor_scalar(out=csb[:, kc, :], in0=onesf[:], scalar1=cs2[:, kc, 0:1],
                                               scalar2=None, op0=ALU.mult),
              reads=['onesf', ('cs2', 0)], writes=['csb'])
        w_ada_v = w_ada_d.rearrange("(kc p) n -> p kc n", p=128)
        colsec = {0: 0, 1: 1, 3: 2, 4: 3}
        for j in range(12):
            sec, half = j // 2, j % 2
            wt = wast[j % 3]
            dma('sync', wt[:], w_ada_v[:, :, j * 512:(j + 1) * 512], writes=[('wast', j % 3)],
                semname="wast%d" % (j % 3))
            if sec in colsec:
                si = colsec[sec]

                def mmcol(e, wt=wt, si=si, half=half):
                    ins = None
                    for cc in range(4):
                        c = half * 4 + cc
                        o = (si * 8 + c) * 2
                        for kc in range(8):
                            ins = e.matmul(bank(0)[:, o:o + 2], lhsT=wt[:, kc, cc * 128:(cc + 1) * 128],
                                           rhs=cs2[:, kc, :], start=(kc == 0), stop=(kc == 7))
                    return ins
                T(mmcol, reads=[('wast', j % 3), ('cs2', 0), ('cs2', 1)], writes=[('ps', 0)])
            else:
                bk = 1 + half
                which = 0 if sec == 2 else 1
                dst = gtA_bc if sec == 2 else gtF_bc

                def mmrow(e, wt=wt, bk=bk):
                    ins = None
                    for kc in range(8):
                        ins = e.matmul(bank(bk)[:, 0:512], lhsT=csb[:, kc, :], rhs=wt[:, kc, :],
                                       start=(kc == 0), stop=(kc == 7))
                    return ins
                T(mmrow, reads=[('wast', j % 3), 'csb'], writes=[('ps', bk)])
                V(lambda e, bk=bk, dst=dst, which=which, half=half: e.tensor_tensor(
                    out=dst[:, half * 512:(half + 1) * 512], in0=bank(bk)[:, 0:512],
                    in1=badabc[:, which, half * 512:(half + 1) * 512], op=ALU.add),
                  reads=[('ps', bk), 'badabc'], writes=[('gt', sec, half)])
        V(lambda e: e.tensor_tensor(out=modcol[:], in0=bank(0)[:, 0:64:2], in1=badacol[:], op=ALU.add),
          reads=[('ps', 0), 'badacol'], writes=['modcol'])
        V(lambda e: e.scalar_tensor_tensor(out=scA[:], in0=modcol[:, 8:16], scalar=1.0, in1=gmix[:],
                                           op0=ALU.add, op1=ALU.mult),
          reads=['modcol', 'gmix'], writes=['scA'])
        V(lambda e: e.scalar_tensor_tensor(out=scF[:], in0=modcol[:, 24:32], scalar=1.0, in1=gffn[:],
                                           op0=ALU.add, op1=ALU.mult),
          reads=['modcol', 'gffn'], writes=['scF'])
        V(lambda e: e.tensor_scalar(out=scAp[:], in0=scA[:], scalar1=flag[:, 0:1], scalar2=None, op0=ALU.mult),
          reads=['scA', 'flag'], writes=['scAp'])
        V(lambda e: e.tensor_scalar(out=shAp[:], in0=modcol[:, 0:8], scalar1=flag[:, 0:1], scalar2=None,
                                    op0=ALU.mult),
          reads=['modcol', 'flag'], writes=['shAp'])
        S.barrier()

    if stage == 0:
        dma('sync', dbg_d[:, 0:32], modcol[:], semname="out")
        dma('sync', dbg_d[:, 32:40], scA[:], semname="out")
        dma('sync', dbg_d[:, 40:48], scAp[:], semname="out")
        dma('sync', dbg_d[:, 48:56], scF[:], semname="out")
        dma('sync', dbg_d[:, 64:64 + 1024], gtA_bc[:], semname="out")
        dma('sync', dbg_d[:, 1088:1088 + 1024], gtF_bc[:], semname="out")
        S.emit(final_waits=[k for k in (('D', 'out'), ('D', 'outg')) if k in S.cnt])
        root.close()
        return nc

    mix = contextlib.ExitStack()
    mix.__enter__()
    HT = sb(mix, "HT", [128, 8, SEQ], BF16)
    RetT = sb(mix, "RetT", [128, 8, HALF], BF16)
    WB = [sb(mix, "WB%d" % i, [128, 8, 256], BF16) for i in range(NSLOT)]

    ropesc = contextlib.ExitStack()
    ropesc.__enter__()
    ropeC = sb(ropesc, "ropeC", [128, SEQ], BF16, side="right")
    ropeS = sb(ropesc, "ropeS", [128, SEQ], BF16, side="right")
    dma('gpsimd', ropeC[:], ropec_d, writes=['ropeC'])
    dma('gpsimd', ropeS[:], ropes_d, writes=['ropeS'])

    with contextlib.ExitStack() as p1:
        NXI, NXN = 6, 3
        xin = [sb(p1, "xin%d" % i, [128, D], F32) for i in range(NXI)]
        xn = [sb(p1, "xn%d" % i, [128, D], BF16) for i in range(NXN)]
        junk = sb(p1, "junk", [128, D], BF16)

        def st_dma(i):
            src = x_prev if i < 16 else x_own
            t = i % 16
            dma('sync', xin[i % NXI][:], src[t * 128:(t + 1) * 128, :], writes=[('xin', i % NXI)],
                semname="xin%d" % (i % NXI))

        def st_sq(i):
            A(lambda e: e.activation(out=junk[:], in_=xin[i % NXI][:], func=AF.Square, accum_out=ss[:, i:i + 1]),
              reads=[('xin', i % NXI)], writes=['junk', ('ss', i)])

        def st_ts(i):
            V(lambda e: e.tensor_scalar(out=rs[:, i:i + 1], in0=ss[:, i:i + 1], scalar1=1.0 / D, scalar2=EPS,
                                        op0=ALU.mult, op1=ALU.add),
              reads=[('ss', i)], writes=[('rs', i)])

        def st_sqrt(i):
            A(lambda e: e.activation(out=rs[:, i:i + 1], in_=rs[:, i:i + 1], func=AF.Sqrt),
              reads=[('rs', i)], writes=[('rs', i)])

        def st_xn(i):
            V(lambda e: e.reciprocal(out=rs[:, i:i + 1], in_=rs[:, i:i + 1]),
              reads=[('rs', i)], writes=[('rs', i)])
            V(lambda e: e.tensor_scalar(out=xn[i % NXN][:], in0=xin[i % NXI][:], scalar1=rs[:, i:i + 1],
                                        scalar2=None, op0=ALU.mult),
              reads=[('xin', i % NXI), ('rs', i)], writes=[('xn', i % NXN)])

        def st_tr(i):
            pT = bank_bf(i % 4)

            def tr(e):
                ins = None
                for kc in range(8):
                    ins = e.transpose(out=pT[:, kc * 128:(kc + 1) * 128],
                                      in_=xn[i % NXN][:, kc * 128:(kc + 1) * 128], identity=identb[:])
                return ins
            T(tr, reads=[('xn', i % NXN)], writes=[('ps', i % 4)])

        def st_ev(i):
            pT = bank_bf(i % 4)
            sc_, sh_ = (scAp, shAp) if i < 16 else (scA, shA)
            for kc in range(8):
                dst = HT[:, kc, i * 128:(i + 1) * 128]
                srcp = pT[:, kc * 128:(kc + 1) * 128]
                if i % 2 == 0:
                    A(lambda e, dst=dst, srcp=srcp, kc=kc: e.activation(
                        out=dst, in_=srcp, func=AF.Identity, bias=sh_[:, kc:kc + 1], scale=sc_[:, kc:kc + 1]),
                      reads=[('ps', i % 4)], writes=[('HTa', i)])
                else:
                    V(lambda e, dst=dst, srcp=srcp, kc=kc: e.tensor_scalar(
                        out=dst, in0=srcp, scalar1=sc_[:, kc:kc + 1], scalar2=sh_[:, kc:kc + 1],
                        op0=ALU.mult, op1=ALU.add),
                      reads=[('ps', i % 4)], writes=[('HTv', i)])
        st_dma(0)
        st_dma(1)
        stages = [(st_sq, 0), (st_ts, 1), (st_sqrt, 2), (st_xn, 3), (st_tr, 4), (st_ev, 5)]
        for j in range(32 + 5):
            if j + 2 < 32:
                st_dma(j + 2)
            for fn, lag in stages:
                i = j - lag
                if 0 <= i < 32:
                    fn(i)
        S.barrier()

    if dbg_cols:
        dma('gpsimd', dbg_d[:, 0:4096], HT[:, 0, :], semname="outg")
        dma('gpsimd', dbg_d[:, 4096:8192], HT[:, 5, :], semname="outg")
    if stage == 1:
        S.emit(final_waits=[k for k in (('D', 'out'), ('D', 'outg')) if k in S.cnt])
        mix.close()
        root.close()
        return nc

    loads = []
    for h in range(4):
        for k in range(4):
            loads.append((win1_d[h * 4 + k], 1))
        loads.append((win2_d[h * 2 + 0], 2))
        loads.append((win2_d[h * 2 + 1], 2))
    for hg in range(4):
        for g in range(3):
            for k in range(3):
                loads.append((win1_d[16 + (hg * 3 + g) * 3 + k], 1))
    wreto_v = wreto_d.rearrange("(kc p) n -> p kc n", p=128)
    watto_v = watto_d.rearrange("(kc p) n -> p kc n", p=128)
    for m in range(8):
        loads.append((win1_d[52 + m], 1))
        loads.append((win1_d[60 + m], 1))
        loads.append((wreto_v[:, :, m * 128:(m + 1) * 128], 1))
        loads.append((watto_v[:, :, m * 128:(m + 1) * 128], 3))
    ring = {'issued': 0, 'consumed': 0}
    N_MAIN_LOADS = 60

    def ring_issue():
        n = ring['issued']
        s = n % NSLOT
        src, kind = loads[n]
        if kind == 1:
            dst = WB[s][:, :, 0:128]
        elif kind == 2:
            dst = WB[s][:, :, :]
        else:
            dst = WB[s][:, 0:4, 0:128]
        dma('gpsimd', dst, src, writes=[('WB', s)], semname="WB%d" % s)
        ring['issued'] += 1

    def ring_get(hold=1):
        while ring['issued'] < min(N_MAIN_LOADS, ring['consumed'] + NSLOT - hold + 1):
            ring_issue()
        s = ring['consumed'] % NSLOT
        ring['consumed'] += 1
        return s

    pbank = [0]

    def next_pbank():
        b = pbank[0]
        pbank[0] = (pbank[0] + 1) % 4
        return b

    def fm_mm(bk, s, col0, ncols=512, nk=8, src=None, wcols=(0, 128)):
        src = HT if src is None else src

        def f(e):
            ins = None
            for kc in range(nk):
                ins = e.matmul(bank(bk)[:, 0:ncols], lhsT=WB[s][:, kc, wcols[0]:wcols[1]],
                               rhs=src[:, kc, col0:col0 + ncols], start=(kc == 0), stop=(kc == nk - 1))
            return ins
        return f

    def tm_mm(bk, s, tok_sl, ncols):
        def f(e):
            ins = None
            for kc in range(8):
                ins = e.matmul(bank(bk)[:, 0:ncols], lhsT=HT[:, kc, tok_sl], rhs=WB[s][:, kc, 0:ncols],
                               start=(kc == 0), stop=(kc == 7))
            return ins
        return f

    with contextlib.ExitStack() as rsc:
        Dm = sb(rsc, "Dm", [128, 4, 128], F32)
        rvec = sb(rsc, "rvec", [128, 16], F32)
        zeta2 = sb(rsc, "zeta2", [128, 4, 16], F32)
        qT = sb(rsc, "qT", [128, HALF], BF16)
        kT = sb(rsc, "kT", [128, SEQ], BF16)
        Vt = sb(rsc, "Vt", [128, 32, 256], BF16)
        SG = sb(rsc, "SG", [128, 16, 256], BF16)
        t1 = [sb(rsc, "t1_%d" % i, [128, 512], F32) for i in range(2)]
        t2 = [sb(rsc, "t2_%d" % i, [128, 512], F32) for i in range(2)]
        Kz = [sb(rsc, "Kz%d" % i, [128, 128], BF16) for i in range(2)]
        PT = [sb(rsc, "PT%d" % i, [128, 128], BF16) for i in range(2)]
        yI = [sb(rsc, "yI%d" % i, [128, 256], F32) for i in range(2)]
        yy = [sb(rsc, "yy%d" % i, [128, 256], F32) for i in range(2)]
        yn = [sb(rsc, "yn%d" % i, [128, 256], BF16) for i in range(2)]
        yg = [sb(rsc, "yg%d" % i, [128, 256], BF16) for i in range(2)]
        st6 = [sb(rsc, "st6_%d" % i, [128, 6], F32) for i in range(2)]
        mv = [sb(rsc, "mv%d" % i, [128, 2], F32) for i in range(2)]
        rstd = [sb(rsc, "rstd%d" % i, [128, 1], F32) for i in range(2)]
        nmr = [sb(rsc, "nmr%d" % i, [128, 1], F32) for i in range(2)]
        Rf = sb(rsc, "Rf", [128, 256], F32)
        Rb = sb(rsc, "Rb", [128, 256], BF16)
        dma('sync', Dm[:], dm_d, writes=['Dm'])
        dma('sync', rvec[:], rvec_d, writes=['rvec'])
        dma('sync', zeta2[:], zeta2_d, writes=['zeta2'])
        ecnt = [0]

        for h in range(4):
            sq, sqs, sk, sks = None, None, None, None
            for which in ('q', 'k'):
                sa = ring_get(1)
                sbw = ring_get(2)
                ntg = 4 if which == 'q' else 8
                for tg in range(ntg):
                    col0 = (HALF + tg * 512) if which == 'q' else tg * 512
                    bA, bB = next_pbank(), next_pbank()
                    T(fm_mm(bA, sa, col0), reads=[('WB', sa)], writes=[('ps', bA)])
                    T(fm_mm(bB, sbw, col0), reads=[('WB', sbw)], writes=[('ps', bB)])
                    par = ecnt[0] % 2
                    ecnt[0] += 1
                    V(lambda e, bA=bA, col0=col0, par=par: e.tensor_tensor(
                        out=t1[par][:], in0=bank(bA)[:, 0:512], in1=ropeC[:, col0:col0 + 512], op=ALU.mult),
                      reads=[('ps', bA), 'ropeC'], writes=[('t1', par)])
                    V(lambda e, bB=bB, col0=col0, par=par: e.tensor_tensor(
                        out=t2[par][:], in0=bank(bB)[:, 0:512], in1=ropeS[:, col0:col0 + 512], op=ALU.mult),
                      reads=[('ps', bB), 'ropeS'], writes=[('t2', par)])
                    if which == 'q':
                        dst = qT[:, tg * 512:(tg + 1) * 512]
                        key = ('qT', tg)
                    else:
                        dst = kT[:, tg * 512:(tg + 1) * 512]
                        key = ('kT', tg)
                    G(lambda e, dst=dst, par=par: e.tensor_tensor(out=dst, in0=t1[par][:], in1=t2[par][:], op=ALU.add),
                      reads=[('t1', par), ('t2', par)], writes=[key])
            sv = ring_get()
            for t in range(32):
                bk = next_pbank()
                T(tm_mm(bk, sv, slice(t * 128, (t + 1) * 128), 256), reads=[('WB', sv)], writes=[('ps', bk)])
                A(lambda e, bk=bk, t=t: e.activation(out=Vt[:, t, :], in_=bank(bk)[:, 0:256], func=AF.Copy),
                  reads=[('ps', bk)], writes=[('V', t)])
            sg_ = ring_get()
            for t in range(16):
                bk = next_pbank()
                T(tm_mm(bk, sg_, slice(HALF + t * 128, HALF + (t + 1) * 128), 256), reads=[('WB', sg_)],
                  writes=[('ps', bk)])
                A(lambda e, bk=bk, t=t: e.activation(out=SG[:, t, :], in_=bank(bk)[:, 0:256], func=AF.Silu),
                  reads=[('ps', bk)], writes=[('SG', t)])
            for n in range(16):
                p = n % 2
                ksl = slice(n * 128, (n + 1) * 128)
                T(lambda e, ksl=ksl: e.transpose(out=bank_bf(4)[:, 0:128], in_=kT[:, ksl], identity=identb[:]),
                  reads=[('kT', n // 4)], writes=[('ps', 4)])
                V(lambda e, p=p, h=h, n=n: e.tensor_scalar(out=Kz[p][:], in0=bank_bf(4)[:, 0:128],
                                                           scalar1=zeta2[:, h, n:n + 1], scalar2=None, op0=ALU.mult),
                  reads=[('ps', 4), 'zeta2'], writes=[('Kz', p)])
                T(lambda e, p=p, n=n: e.matmul(bank(2)[:, 0:256], lhsT=Kz[p][:], rhs=Vt[:, n, :],
                                               start=(n == 0), stop=(n == 15)),
                  reads=[('Kz', p), ('V', n)], writes=[('ps', 2)])
            V(lambda e: e.tensor_copy(out=Rb[:], in_=bank(2)[:, 0:256]), reads=[('ps', 2)], writes=['Rb'])
            V(lambda e: e.tensor_copy(out=Rf[:], in_=bank(2)[:, 0:256]), reads=[('ps', 2)], writes=['Rf'])
            for n in range(16, 32):
                own = True
                p = n % 2
                ksl = slice(n * 128, (n + 1) * 128)
                T(lambda e, ksl=ksl: e.transpose(out=bank_bf(4)[:, 0:128], in_=kT[:, ksl], identity=identb[:]),
                  reads=[('kT', n // 4)], writes=[('ps', 4)])
                V(lambda e, p=p, h=h: e.tensor_scalar(out=Kz[p][:], in0=bank_bf(4)[:, 0:128],
                                                      scalar1=rvec[:, h:h + 1], scalar2=None, op0=ALU.mult),
                  reads=[('ps', 4), 'rvec'], writes=[('Kz', p)])
                if own:
                    j = n - 16
                    qsl = slice(j * 128, (j + 1) * 128)
                    T(lambda e, ksl=ksl, qsl=qsl: e.matmul(bank(5)[:, 0:128], lhsT=kT[:, ksl], rhs=qT[:, qsl],
                                                           start=True, stop=True),
                      reads=[('kT', n // 4), ('qT', j // 4)], writes=[('ps', 5)])
                    V(lambda e, p=p, h=h: e.tensor_tensor(out=PT[p][:], in0=bank(5)[:, 0:128], in1=Dm[:, h, :],
                                                          op=ALU.mult),
                      reads=[('ps', 5), 'Dm'], writes=[('PT', p)])
                    bI = 6 + p
                    T(lambda e, p=p, n=n, bI=bI: e.matmul(bank(bI)[:, 0:256], lhsT=PT[p][:], rhs=Vt[:, n, :],
                                                          start=True, stop=True),
                      reads=[('PT', p), ('V', n)], writes=[('ps', bI)])
                    bC = 0 + p
                    T(lambda e, qsl=qsl, bC=bC: e.matmul(bank(bC)[:, 0:256], lhsT=qT[:, qsl], rhs=Rb[:],
                                                         start=True, stop=True),
                      reads=[('qT', j // 4), 'Rb'], writes=[('ps', bC)])
                    A(lambda e, p=p, bI=bI: e.activation(out=yI[p][:], in_=bank(bI)[:, 0:256], func=AF.Copy),
                      reads=[('ps', bI)], writes=[('yI', p)])
                    V(lambda e, p=p, bC=bC, h=h: e.scalar_tensor_tensor(
                        out=yy[p][:], in0=bank(bC)[:, 0:256], scalar=rvec[:, 4 + h:5 + h], in1=yI[p][:],
                        op0=ALU.mult, op1=ALU.add),
                      reads=[('ps', bC), ('yI', p), 'rvec'], writes=[('yy', p)])
                    V(lambda e, p=p: e.bn_stats(out=st6[p][:], in_=yy[p][:]), reads=[('yy', p)], writes=[('st6', p)])
                    V(lambda e, p=p: e.bn_aggr(out=mv[p][:], in_=st6[p][:]), reads=[('st6', p)], writes=[('mv', p)])
                    V(lambda e, p=p: e.tensor_scalar(out=rstd[p][:], in0=mv[p][:, 1:2], scalar1=GN_EPS, scalar2=None,
                                                     op0=ALU.add),
                      reads=[('mv', p)], writes=[('rstd', p)])
                    A(lambda e, p=p: e.activation(out=rstd[p][:], in_=rstd[p][:], func=AF.Sqrt),
                      reads=[('rstd', p)], writes=[('rstd', p)])
                    V(lambda e, p=p: e.reciprocal(out=rstd[p][:], in_=rstd[p][:]),
                      reads=[('rstd', p)], writes=[('rstd', p)])
                    V(lambda e, p=p: e.scalar_tensor_tensor(out=nmr[p][:], in0=mv[p][:, 0:1], scalar=-1.0,
                                                            in1=rstd[p][:], op0=ALU.mult, op1=ALU.mult),
                      reads=[('mv', p), ('rstd', p)], writes=[('nmr', p)])
                    A(lambda e, p=p: e.activation(out=yn[p][:], in_=yy[p][:], func=AF.Identity,
                                                  bias=nmr[p][:, 0:1], scale=rstd[p][:, 0:1]),
                      reads=[('yy', p), ('rstd', p), ('nmr', p)], writes=[('yn', p)])
                    G(lambda e, p=p, j=j: e.tensor_tensor(out=yg[p][:], in0=yn[p][:], in1=SG[:, j, :], op=ALU.mult),
                      reads=[('yn', p), ('SG', j)], writes=[('yg', p)])

                    def trY(e, p=p):
                        ins = None
                        for dc in range(2):
                            ins = e.transpose(out=bank_bf(3)[:, dc * 128:(dc + 1) * 128],
                                              in_=yg[p][:, dc * 128:(dc + 1) * 128], identity=identb[:])
                        return ins
                    T(trY, reads=[('yg', p)], writes=[('ps', 3)])
                    for dc in range(2):
                        A(lambda e, h=h, dc=dc, qsl=qsl: e.activation(
                            out=RetT[:, h * 2 + dc, qsl], in_=bank_bf(3)[:, dc * 128:(dc + 1) * 128], func=AF.Copy),
                          reads=[('ps', 3)], writes=[('RetT', j)])
                if n < 31:
                    T(lambda e, p=p, n=n: e.matmul(bank(2)[:, 0:256], lhsT=Kz[p][:], rhs=Vt[:, n, :],
                                                   start=True, stop=True),
                      reads=[('Kz', p), ('V', n)], writes=[('ps', 2)])
                    V(lambda e, h=h: e.scalar_tensor_tensor(out=Rf[:], in0=Rf[:], scalar=rvec[:, 8 + h:9 + h],
                                                            in1=bank(2)[:, 0:256], op0=ALU.mult, op1=ALU.add),
                      reads=['Rf', ('ps', 2), 'rvec'], writes=['Rf'])
                    A(lambda e: e.activation(out=Rb[:], in_=Rf[:], func=AF.Copy), reads=['Rf'], writes=['Rb'])
        S.barrier()

    ropesc.close()

    if dbg_cols:
        for kc in range(8):
            dma('gpsimd', dbg_d[:, 8192 + kc * HALF:8192 + (kc + 1) * HALF], RetT[:, kc, :], semname="outg")
    if stage == 2:
        S.emit(final_waits=[k for k in (('D', 'out'), ('D', 'outg')) if k in S.cnt])
        mix.close()
        root.close()
        return nc

    AttT = sb(mix, "AttT", [128, 4, HALF], BF16)
    with contextlib.ExitStack() as asc:
        EB = sb(asc, "EB", [128, 12, 256], BF16)
        EBm = sb(asc, "EBm", [128, 12, 256], BF16)
        with contextlib.ExitStack() as ebs:
            relb = sb(ebs, "relb", [32, 12], F32)
            ohf = sb(ebs, "ohf", [32, 3, 384], F32)
            jf = sb(ebs, "jf", [128, 128], F32)
            Fsb = sb(ebs, "Fsb", [4, 3, 384], F32)
            Gh = [sb(ebs, "Gh%d" % i, [128, 256], F32) for i in range(2)]
            dma('sync', relb[:], relb_d, writes=['relb'])
            dma('sync', ohf[:], ohf_d, writes=['ohf'])
            dma('sync', jf[:], jf_d, writes=['jf'])
            A(lambda e: e.activation(out=relb[:], in_=relb[:], func=AF.Exp), reads=['relb'], writes=['relb'])
            for g in range(3):
                T(lambda e, g=g: e.matmul(bank(0)[0:4, 0:384], lhsT=relb[:, g * 4:(g + 1) * 4], rhs=ohf[:, g, :],
                                          start=True, stop=True),
                  reads=['relb', 'ohf'], writes=[('ps', 0)])
                V(lambda e, g=g: e.tensor_copy(out=Fsb[:, g, :], in_=bank(0)[0:4, 0:384]),
                  reads=[('ps', 0)], writes=['Fsb'])
            dma('sync', fd_d.rearrange("(g h) m -> h g m", g=3), Fsb[:], reads=['Fsb'], writes=['fd'])
            for idx in range(12):
                hk = bass.AP(tensor=fd_t, offset=idx * 384, ap=[[1, 128], [1, 256]])
                dma('sync', Gh[idx % 2][:], hk, reads=['fd'], writes=[('Gh', idx % 2)], semname="Gh%d" % (idx % 2))
                bk = 1 + idx % 2
                T(lambda e, idx=idx, bk=bk: e.matmul(bank(bk)[:, 0:256], lhsT=jf[:], rhs=Gh[idx % 2][:],
                                                     start=True, stop=True),
                  reads=['jf', ('Gh', idx % 2)], writes=[('ps', bk)])
                V(lambda e, idx=idx, bk=bk: e.tensor_copy(out=EB[:, idx, :], in_=bank(bk)[:, 0:256]),
                  reads=[('ps', bk)], writes=[('EBa', idx)])
                V(lambda e, idx=idx, bk=bk: e.tensor_copy(out=EBm[:, idx, 0:128], in_=bank(bk)[:, 0:128]),
                  reads=[('ps', bk)], writes=[('EBb', idx)])
                V(lambda e, idx=idx, bk=bk: e.tensor_scalar(out=EBm[:, idx, 128:256], in0=bank(bk)[:, 128:256],
                                                            scalar1=flag[:, 0:1], scalar2=None, op0=ALU.mult),
                  reads=[('ps', bk)], writes=[('EBv', idx)])
            S.barrier()

        if dbg_cols:
            for idx in range(12):
                dma('gpsimd', dbg_d[:, 24576 + idx * 256:24576 + (idx + 1) * 256], EB[:, idx, :], semname="outg")
        if stage == 25:
            S.emit(final_waits=[k for k in (('D', 'out'), ('D', 'outg')) if k in S.cnt])
            asc.close()
            mix.close()
            root.close()
            return nc

        qa = [sb(asc, "qa%d" % i, [128, HALF], BF16) for i in range(2)]
        ka = [sb(asc, "ka%d" % i, [128, SEQ], BF16) for i in range(2)]
        VA = sb(asc, "VA", [128, 32, 128], BF16)
        ND = sb(asc, "ND", [128, 2, HALF], F32)
        Et = [sb(asc, "Et%d" % i, [128, 256], F32) for i in range(2)]
        Pt = [sb(asc, "Pt%d" % i, [128, 256], BF16) for i in range(2)]
        blkc = [0]
        items = [(hg, g) for hg in range(4) for g in range(3)]

        def qk_ops(ii):
            hg, g = items[ii]
            par = ii % 2
            qT_, kT_ = qa[par], ka[par]
            ops = []
            st = {}

            def q_op(tg):
                def f():
                    if 'sq' not in st:
                        st['sq'] = ring_get(1)
                    s_q = st['sq']
                    bk = next_pbank()
                    T(fm_mm(bk, s_q, HALF + tg * 512), reads=[('WB', s_q)], writes=[('ps', bk)])
                    A(lambda e: e.activation(out=qT_[:, tg * 512:(tg + 1) * 512], in_=bank(bk)[:, 0:512],
                                             func=AF.Copy, scale=float(128 ** -0.5)),
                      reads=[('ps', bk)], writes=[('qa', par)])
                return f

            def k_op(tg):
                def f():
                    if 'sk' not in st:
                        st['sk'] = ring_get(1)
                    s_k = st['sk']
                    bk = next_pbank()
                    T(fm_mm(bk, s_k, tg * 512), reads=[('WB', s_k)], writes=[('ps', bk)])
                    V(lambda e: e.tensor_copy(out=kT_[:, tg * 512:(tg + 1) * 512], in_=bank(bk)[:, 0:512]),
                      reads=[('ps', bk)], writes=[('ka', par)])
                return f
            for tg in range(4):
                ops.append(q_op(tg))
            for tg in (list(range(8)) if g == 2 else list(range(3, 8))):
                ops.append(k_op(tg))
            return ops

        def v_proj(ii):
            hg, g = items[ii]
            w, r = ATT_PATTERNS[g]
            nbb = 16 // r
            s_v = ring_get(1)
            vblocks = []
            for bb in range(nbb):
                for c in range(r):
                    vblocks.append((bb * r + c, HALF + 128 * r * bb + c))
            for c in range(r):
                vblocks.append((16 + c, 128 * r * (nbb - 1) + c))
            for vi, (idx, st0) in enumerate(vblocks):
                bk = next_pbank()
                tsl = slice(st0, st0 + 127 * r + 1, r)
                T(tm_mm(bk, s_v, tsl, 128), reads=[('WB', s_v)], writes=[('ps', bk)])
                if vi % 2 == 0:
                    A(lambda e, bk=bk, idx=idx: e.activation(out=VA[:, idx, :], in_=bank(bk)[:, 0:128],
                                                             func=AF.Copy),
                      reads=[('ps', bk)], writes=[('VAa', idx)])
                else:
                    V(lambda e, bk=bk, idx=idx: e.tensor_copy(out=VA[:, idx, :], in_=bank(bk)[:, 0:128]),
                      reads=[('ps', bk)], writes=[('VAv', idx)])

        def block_ops(ii):
            hg, g = items[ii]
            w, r = ATT_PATTERNS[g]
            nbb = 16 // r
            par = ii % 2
            qT_, kT_ = qa[par], ka[par]
            ebi = g * 4 + hg
            ops = []
            for bb in range(nbb):
                for c in range(r):
                    def f(bb=bb, c=c):
                        bp = blkc[0] % 2
                        blkc[0] += 1
                        q0 = 128 * r * bb + c
                        qsl = slice(q0, q0 + 127 * r + 1, r)
                        o0 = HALF + q0
                        osl = slice(o0, o0 + 127 * r + 1, r)
                        if bb > 0:
                            p0 = o0 - 128 * r
                            pidx = (bb - 1) * r + c
                        else:
                            p0 = 128 * r * (nbb - 1) + c
                            pidx = 16 + c
                        psl = slice(p0, p0 + 127 * r + 1, r)
                        oidx = bb * r + c
                        bS = 4 + bp
                        bN = 6 + bp

                        def mmS(e):
                            e.matmul(bank(bS)[:, 0:128], lhsT=kT_[:, osl], rhs=qT_[:, qsl], start=True, stop=True)
                            return e.matmul(bank(bS)[:, 128:256], lhsT=kT_[:, psl], rhs=qT_[:, qsl],
                                            start=True, stop=True)
                        T(mmS, reads=[('ka', par), ('qa', par)], writes=[('ps', bS)])
                        A(lambda e: e.activation(out=Et[bp][:], in_=bank(bS)[:, 0:256], func=AF.Exp),
                          reads=[('ps', bS)], writes=[('Et', bp)])
                        EBt = EBm if bb == 0 else EB
                        V(lambda e: e.tensor_tensor(out=Pt[bp][:], in0=Et[bp][:], in1=EBt[:, ebi, :], op=ALU.mult),
                          reads=[('Et', bp)], writes=[('Pt', bp)])

                        def mmN(e):
                            e.matmul(bank(bN)[:, 0:128], lhsT=VA[:, oidx, :], rhs=Pt[bp][:, 0:128],
                                     start=True, stop=False)
                            e.matmul(bank(bN)[:, 0:128], lhsT=VA[:, pidx, :], rhs=Pt[bp][:, 128:256],
                                     start=False, stop=True)
                            e.matmul(bank(bN)[:, 128:256], lhsT=onesb[:], rhs=Pt[bp][:, 0:128],
                                     start=True, stop=False)
                            return e.matmul(bank(bN)[:, 128:256], lhsT=onesb[:], rhs=Pt[bp][:, 128:256],
                                            start=False, stop=True)
                        T(mmN, reads=[('Pt', bp), ('VAa', oidx), ('VAv', oidx), ('VAa', pidx), ('VAv', pidx)],
                          writes=[('ps', bN)])
                        src3 = bank(bN)[:, 0:256].rearrange("p (a q) -> p a q", a=2)
                        if g == 0:
                            V(lambda e: e.tensor_copy(out=ND[:, :, qsl], in_=src3),
                              reads=[('ps', bN)], writes=['ND'])
                        else:
                            V(lambda e: e.tensor_tensor(out=ND[:, :, qsl], in0=src3, in1=ND[:, :, qsl], op=ALU.add),
                              reads=[('ps', bN), 'ND'], writes=['ND'])
                    ops.append(f)
            return ops

        for f in qk_ops(0):
            f()
        v_proj(0)
        for ii in range(len(items)):
            hg, g = items[ii]
            blks = block_ops(ii)
            nxt = qk_ops(ii + 1) if ii + 1 < len(items) else []
            ni = 0
            for bi, f in enumerate(blks):
                f()
                want = (len(nxt) * (bi + 1)) // len(blks)
                while ni < want:
                    nxt[ni]()
                    ni += 1
            while ni < len(nxt):
                nxt[ni]()
                ni += 1
            if g == 2:
                V(lambda e: e.reciprocal(out=ND[:, 1, :], in_=ND[:, 1, :]), reads=['ND'], writes=['ND'])
                V(lambda e, hg=hg: e.tensor_tensor(out=AttT[:, hg, :], in0=ND[:, 0, :], in1=ND[:, 1, :], op=ALU.mult),
                  reads=['ND'], writes=['AttT'])
            if ii + 1 < len(items):
                v_proj(ii + 1)
        S.barrier()

    if dbg_cols:
        for kc in range(4):
            dma('gpsimd', dbg_d[:, 27648 + kc * HALF:27648 + (kc + 1) * HALF], AttT[:, kc, :], semname="outg")
    if stage == 3:
        S.emit(final_waits=[k for k in (('D', 'out'), ('D', 'outg')) if k in S.cnt])
        mix.close()
        root.close()
        return nc

    mrg = contextlib.ExitStack()
    mrg.__enter__()
    MergedT = sb(mrg, "MergedT", [128, 8, HALF], BF16, side="right")
    with contextlib.ExitStack() as msc:
        sgA = [sb(msc, "sgA%d" % i, [128, 512], F32) for i in range(2)]
        sgB = [sb(msc, "sgB%d" % i, [128, 512], F32) for i in range(2)]
        m1 = [sb(msc, "m1_%d" % i, [128, 512], F32) for i in range(2)]
        m2 = [sb(msc, "m2_%d" % i, [128, 512], F32) for i in range(2)]
        WBm = WB + [sb(msc, "WBm%d" % i, [128, 8, 256], BF16) for i in range(4)]
        mloads = loads[N_MAIN_LOADS:]
        assert len(mloads) == 32 and ring['issued'] == ring['consumed'] == N_MAIN_LOADS
        mr = {'issued': 0, 'consumed': 0}

        def mring_get(hold):
            while mr['issued'] < min(len(mloads), mr['consumed'] + 8 - hold + 1):
                n = mr['issued']
                s = n % 8
                src_, kind = mloads[n]
                dst = WBm[s][:, :, 0:128] if kind == 1 else WBm[s][:, 0:4, 0:128]
                dma('gpsimd', dst, src_, writes=[('WBm', s)], semname="WBm%d" % s)
                mr['issued'] += 1
            s = mr['consumed'] % 8
            mr['consumed'] += 1
            return s
        WB_save = WB

        def fm_mm_m(bk, s, col0, nk=8, src=None):
            src = HT if src is None else src

            def f(e):
                ins = None
                for kc in range(nk):
                    ins = e.matmul(bank(bk)[:, 0:512], lhsT=WBm[s][:, kc, 0:128],
                                   rhs=src[:, kc, col0:col0 + 512], start=(kc == 0), stop=(kc == nk - 1))
                return ins
            return f
        it = 0
        for m in range(8):
            s_ga = mring_get(1)
            s_gb = mring_get(2)
            s_wr = mring_get(3)
            s_wa = mring_get(4)
            for tg in range(4):
                p = it % 2
                it += 1
                bs = 0 if p == 0 else 4
                T(fm_mm_m(bs + 0, s_ga, HALF + tg * 512), reads=[('WBm', s_ga)], writes=[('ps', bs + 0)])
                T(fm_mm_m(bs + 1, s_gb, HALF + tg * 512), reads=[('WBm', s_gb)], writes=[('ps', bs + 1)])
                T(fm_mm_m(bs + 2, s_wr, tg * 512, src=RetT), reads=[('WBm', s_wr)], writes=[('ps', bs + 2)])
                T(fm_mm_m(bs + 3, s_wa, tg * 512, nk=4, src=AttT), reads=[('WBm', s_wa)], writes=[('ps', bs + 3)])
                A(lambda e, p=p, bs=bs: e.activation(out=sgA[p][:], in_=bank(bs)[:, 0:512], func=AF.Sigmoid),
                  reads=[('ps', bs)], writes=[('sgA', p)])
                A(lambda e, p=p, bs=bs: e.activation(out=sgB[p][:], in_=bank(bs + 1)[:, 0:512], func=AF.Sigmoid),
                  reads=[('ps', bs + 1)], writes=[('sgB', p)])
                V(lambda e, p=p, bs=bs: e.tensor_tensor(out=m1[p][:], in0=bank(bs + 2)[:, 0:512], in1=sgA[p][:],
                                                        op=ALU.mult),
                  reads=[('ps', bs + 2), ('sgA', p)], writes=[('m1', p)])
                V(lambda e, p=p, bs=bs: e.tensor_tensor(out=m2[p][:], in0=bank(bs + 3)[:, 0:512], in1=sgB[p][:],
                                                        op=ALU.mult),
                  reads=[('ps', bs + 3), ('sgB', p)], writes=[('m2', p)])
                G(lambda e, p=p, m=m, tg=tg: e.tensor_tensor(out=MergedT[:, m, tg * 512:(tg + 1) * 512],
                                                             in0=m1[p][:], in1=m2[p][:], op=ALU.add),
                  reads=[('m1', p), ('m2', p)], writes=[('MergedT', m, tg)])
        S.barrier()
    mix.close()

    X = sb(root, "X", [128, NT, D], F32)
    with contextlib.ExitStack() as p3:
        WO = sb(p3, "WO", [128, 8, D], BF16)
        wst = [sb(p3, "wst%d" % i, [128, D], F32) for i in range(2)]
        for kc in range(8):
            dma('sync', wst[kc % 2][:], wo_d[kc * 128:(kc + 1) * 128, :], writes=[('wst', kc % 2)],
                semname="wst%d" % (kc % 2))
            if kc % 2 == 0:
                G(lambda e, kc=kc: e.tensor_tensor(out=WO[:, kc, :], in0=wst[kc % 2][:], in1=gtA_bc[:], op=ALU.mult),
                  reads=[('wst', kc % 2)], writes=[('WOg', kc)])
            else:
                V(lambda e, kc=kc: e.tensor_tensor(out=WO[:, kc, :], in0=wst[kc % 2][:], in1=gtA_bc[:], op=ALU.mult),
                  reads=[('wst', kc % 2)], writes=[('WOv', kc)])
        for t in range(NT):
            dma('sync', X[:, t, :], x_own[t * 128:(t + 1) * 128, :], writes=[('X', t, 0), ('X', t, 1)])
        for t in range(NT):
            for nh in range(2):
                bk = next_pbank()

                def mmo(e, bk=bk, t=t, nh=nh):
                    ins = None
                    for kc in range(8):
                        ins = e.matmul(bank(bk)[:, 0:512], lhsT=MergedT[:, kc, t * 128:(t + 1) * 128],
                                       rhs=WO[:, kc, nh * 512:(nh + 1) * 512], start=(kc == 0), stop=(kc == 7))
                    return ins
                T(mmo, reads=[('WOg', 0), ('WOv', 1), ('WOg', 2), ('WOv', 3), ('WOg', 4), ('WOv', 5), ('WOg', 6), ('WOv', 7)],
                  writes=[('ps', bk)])
                V(lambda e, bk=bk, t=t, nh=nh: e.tensor_tensor(out=X[:, t, nh * 512:(nh + 1) * 512],
                                                               in0=bank(bk)[:, 0:512],
                                                               in1=X[:, t, nh * 512:(nh + 1) * 512], op=ALU.add),
                  reads=[('ps', bk), ('X', t, nh)], writes=[('X', t, nh)])
        S.barrier()

    mrg.close()

    if dbg_cols:
        for t in range(NT):
            dma('sync', dbg_d[:, 35840 + t * D:35840 + (t + 1) * D], X[:, t, :], semname="out")
    if stage == 4:
        S.emit(final_waits=[k for k in (('D', 'out'), ('D', 'outg')) if k in S.cnt])
        root.close()
        return nc

    H2T = sb(root, "H2T", [128, 8, HALF], BF16)
    Gt = sb(root, "Gt", [128, NT, N_EXP], F32)
    with contextlib.ExitStack() as p4:
        xnf = [sb(p4, "xnf%d" % i, [128, D], F32) for i in range(3)]
        h2f = [sb(p4, "h2f%d" % i, [128, 8, 128], F32) for i in range(3)]
        junk4 = sb(p4, "junk4", [128, D], BF16)
        WR = sb(p4, "WR", [128, 8, N_EXP], F32)
        brbc = sb(p4, "brbc", [128, N_EXP], F32)
        Lg = [sb(p4, "Lg%d" % i, [128, N_EXP], F32) for i in range(2)]
        m8 = [sb(p4, "m8_%d" % i, [128, 8], F32) for i in range(2)]
        ngm = [sb(p4, "ngm%d" % i, [128, 1], F32) for i in range(2)]
        msk = [sb(p4, "msk%d" % i, [128, N_EXP], F32) for i in range(2)]
        Ex = [sb(p4, "Ex%d" % i, [128, N_EXP], F32) for i in range(2)]
        sm = [sb(p4, "sm%d" % i, [128, 1], F32) for i in range(2)]
        dma('sync', WR[:], wr_d.rearrange("(kc p) n -> p kc n", p=128), writes=['WR'])
        dma('sync', brbc[:], brbc_d, writes=['brbc'])
        NP4 = 3

        def s4_sq(t):
            i = 32 + t
            A(lambda e: e.activation(out=junk4[:], in_=X[:, t, :], func=AF.Square, accum_out=ss[:, i:i + 1]),
              reads=[('X', t, 0), ('X', t, 1)], writes=['junk4', ('ss', i)])

        def s4_ts(t):
            i = 32 + t
            V(lambda e: e.tensor_scalar(out=rs[:, i:i + 1], in0=ss[:, i:i + 1], scalar1=1.0 / D, scalar2=EPS,
                                        op0=ALU.mult, op1=ALU.add),
              reads=[('ss', i)], writes=[('rs', i)])

        def s4_sqrt(t):
            i = 32 + t
            A(lambda e: e.activation(out=rs[:, i:i + 1], in_=rs[:, i:i + 1], func=AF.Sqrt),
              reads=[('rs', i)], writes=[('rs', i)])

        def s4_xn(t):
            i = 32 + t
            p = t % NP4
            V(lambda e: e.reciprocal(out=rs[:, i:i + 1], in_=rs[:, i:i + 1]),
              reads=[('rs', i)], writes=[('rs', i)])
            V(lambda e: e.tensor_scalar(out=xnf[p][:], in0=X[:, t, :], scalar1=rs[:, i:i + 1],
                                        scalar2=None, op0=ALU.mult),
              reads=[('X', t, 0), ('X', t, 1), ('rs', i)], writes=[('xnf', p)])

        def s4_b0(t):
            return (0, 2, 6)[t % NP4]

        def s4_tr(t):
            p = t % NP4
            b0 = s4_b0(t)

            def tr2(e):
                ins = None
                for kc in range(8):
                    ins = e.transpose(out=PS[:, b0 + kc // 4, (kc % 4) * 128:(kc % 4 + 1) * 128],
                                      in_=xnf[p][:, kc * 128:(kc + 1) * 128], identity=identf[:])
                return ins
            T(tr2, reads=[('xnf', p)], writes=[('ps', b0), ('ps', b0 + 1)])

        def s4_ev(t):
            p = t % NP4
            b0 = s4_b0(t)
            for kc in range(8):
                srcp = PS[:, b0 + kc // 4, (kc % 4) * 128:(kc % 4 + 1) * 128]
                if t % 2 == 0:
                    A(lambda e, srcp=srcp, kc=kc: e.activation(out=h2f[p][:, kc, :], in_=srcp, func=AF.Identity,
                                                               bias=shF[:, kc:kc + 1], scale=scF[:, kc:kc + 1]),
                      reads=[('ps', b0), ('ps', b0 + 1)], writes=[('h2fa', p)])
                else:
                    V(lambda e, srcp=srcp, kc=kc: e.tensor_scalar(out=h2f[p][:, kc, :], in0=srcp,
                                                                  scalar1=scF[:, kc:kc + 1],
                                                                  scalar2=shF[:, kc:kc + 1],
                                                                  op0=ALU.mult, op1=ALU.add),
                      reads=[('ps', b0), ('ps', b0 + 1)], writes=[('h2fv', p)])

        def s4_rt(t):
            p = t % NP4
            G(lambda e: e.tensor_copy(out=H2T[:, :, t * 128:(t + 1) * 128], in_=h2f[p][:]),
              reads=[('h2fa', p), ('h2fv', p)], writes=[('H2T', t)])
            bl = 4 + t % 2

            def mmr(e):
                ins = None
                for kc in range(8):
                    ins = e.matmul(bank(bl)[:, 0:N_EXP], lhsT=h2f[p][:, kc, :], rhs=WR[:, kc, :],
                                   start=(kc == 0), stop=(kc == 7))
                return ins
            T(mmr, reads=[('h2fa', p), ('h2fv', p), 'WR'], writes=[('ps', bl)])

        def s4_gate(t):
            q = t % 2
            bl = 4 + t % 2
            V(lambda e: e.tensor_tensor(out=Lg[q][:], in0=bank(bl)[:, 0:N_EXP], in1=brbc[:], op=ALU.add),
              reads=[('ps', bl), 'brbc'], writes=[('Lg', q)])
            V(lambda e: e.max(out=m8[q][:], in_=Lg[q][:]), reads=[('Lg', q)], writes=[('m8', q)])
            V(lambda e: e.tensor_scalar(out=ngm[q][:], in0=m8[q][:, 0:1], scalar1=-1.0, scalar2=None, op0=ALU.mult),
              reads=[('m8', q)], writes=[('ngm', q)])
            V(lambda e: e.tensor_scalar(out=msk[q][:], in0=Lg[q][:], scalar1=m8[q][:, 3:4], scalar2=None,
                                        op0=ALU.is_ge),
              reads=[('Lg', q), ('m8', q)], writes=[('msk', q)])
            A(lambda e: e.activation(out=Ex[q][:], in_=Lg[q][:], func=AF.Exp, bias=ngm[q][:, 0:1], scale=1.0),
              reads=[('Lg', q), ('ngm', q)], writes=[('Ex', q)])

        def s4_gate2(t):
            q = t % 2
            V(lambda e: e.tensor_tensor(out=Ex[q][:], in0=Ex[q][:], in1=msk[q][:], op=ALU.mult),
              reads=[('Ex', q), ('msk', q)], writes=[('Ex', q)])
            V(lambda e: e.tensor_reduce(out=sm[q][:], in_=Ex[q][:], axis=AX.X, op=ALU.add),
              reads=[('Ex', q)], writes=[('sm', q)])
            V(lambda e: e.reciprocal(out=sm[q][:], in_=sm[q][:]), reads=[('sm', q)], writes=[('sm', q)])
            V(lambda e: e.tensor_scalar(out=Gt[:, t, :], in0=Ex[q][:], scalar1=sm[q][:, 0:1], scalar2=None,
                                        op0=ALU.mult),
              reads=[('Ex', q), ('sm', q)], writes=[('Gt', t)])
        stages4 = [(s4_sq, 0), (s4_ts, 1), (s4_sqrt, 2), (s4_xn, 3), (s4_tr, 4), (s4_ev, 5), (s4_rt, 6),
                   (s4_gate, 7), (s4_gate2, 8)]
        for j in range(NT + 8):
            for fn, lag in stages4:
                t = j - lag
                if 0 <= t < NT:
                    fn(t)
        S.barrier()

    if dbg_cols:
        dma('sync', dbg_d[:, 52224:52224 + NT * N_EXP], Gt[:].rearrange("p t e -> p (t e)"), semname="out")
        dma('gpsimd', dbg_d[:, 52736:52736 + HALF], H2T[:, 0, :], semname="outg")
    if stage == 5:
        S.emit(final_waits=[k for k in (('D', 'out'), ('D', 'outg')) if k in S.cnt])
        root.close()
        return nc

    with contextlib.ExitStack() as p5:
        aT = sb(p5, "aT", [128, 8, HALF], BF16)
        W2 = sb(p5, "W2", [128, 8, D], BF16)
        W1 = [sb(p5, "W1_%d" % i, [128, 8, 256], BF16) for i in range(3)]
        w2st = [sb(p5, "w2st%d" % i, [128, D], F32) for i in range(2)]
        b1 = sb(p5, "b1", [128, N_EXP, 8, 2], F32)
        tA = [sb(p5, "tA%d" % i, [128, 512], F32) for i in range(2)]
        tB = [sb(p5, "tB%d" % i, [128, 512], F32) for i in range(2)]
        tC = [sb(p5, "tC%d" % i, [128, 512], F32) for i in range(2)]
        tD = [sb(p5, "tD%d" % i, [128, 512], F32) for i in range(2)]
        dma('sync', b1[:], b1r_d, writes=['b1'])
        n_exp_run = N_EXP
        w1n = [0]
        total_w1 = n_exp_run * 8

        def w1_issue():
            n = w1n[0]
            e_, fc_ = n // 8, n % 8
            s = n % 3
            dma('gpsimd', W1[s][:], w1r_d[e_, fc_], writes=[('W1', s)], semname="W1_%d" % s)
            w1n[0] += 1
        w1c = [0]

        def w1_get():
            while w1n[0] < min(total_w1, w1c[0] + 3):
                w1_issue()
            s = w1c[0] % 3
            w1c[0] += 1
            return s
        ac = [0]
        sc_ = [0]
        for ex in range(n_exp_run):
            def w2_dma(fc2, ex=ex):
                q = (ex * 8 + fc2) % 2
                dma('sync', w2st[q][:], w2_d[ex, fc2 * 128:(fc2 + 1) * 128, :], writes=[('w2st', q)],
                    semname="w2st%d" % q)

            def w2_chunk(fc2, ex=ex):
                q = (ex * 8 + fc2) % 2
                if fc2 + 1 < 8:
                    w2_dma(fc2 + 1)
                V(lambda e: e.tensor_tensor(out=W2[:, fc2, :], in0=w2st[q][:], in1=gtF_bc[:], op=ALU.mult),
                  reads=[('w2st', q)], writes=['W2'])
            w2_dma(0)
            w2_chunk(0)
            for fc in range(8):
                s = w1_get()
                for tg in range(4):
                    p = ac[0] % 2
                    ac[0] += 1
                    bg, bl_ = (0, 1) if p == 0 else (2, 3)

                    def mm1(e, s=s, tg=tg, bg=bg, bl_=bl_):
                        ins = None
                        for kc in range(8):
                            e.matmul(bank(bg)[:, 0:512], lhsT=W1[s][:, kc, 0:128],
                                     rhs=H2T[:, kc, tg * 512:(tg + 1) * 512], start=(kc == 0), stop=(kc == 7))
                        for kc in range(8):
                            ins = e.matmul(bank(bl_)[:, 0:512], lhsT=W1[s][:, kc, 128:256],
                                           rhs=H2T[:, kc, tg * 512:(tg + 1) * 512], start=(kc == 0), stop=(kc == 7))
                        return ins
                    T(mm1, reads=[('W1', s)], writes=[('ps', bg), ('ps', bl_)])
                    V(lambda e, p=p, bg=bg, ex=ex, fc=fc: e.tensor_scalar(
                        out=tA[p][:], in0=bank(bg)[:, 0:512], scalar1=b1[:, ex, fc, 0:1], scalar2=7.0,
                        op0=ALU.add, op1=ALU.min),
                      reads=[('ps', bg), 'b1'], writes=[('tA', p)])
                    A(lambda e, p=p: e.activation(out=tB[p][:], in_=tA[p][:], func=AF.Sigmoid, scale=1.702),
                      reads=[('tA', p)], writes=[('tB', p)])
                    V(lambda e, p=p, bl_=bl_, ex=ex, fc=fc: e.tensor_scalar(
                        out=tC[p][:], in0=bank(bl_)[:, 0:512], scalar1=b1[:, ex, fc, 1:2], scalar2=7.0,
                        op0=ALU.add, op1=ALU.min),
                      reads=[('ps', bl_), 'b1'], writes=[('tC', p)])
                    V(lambda e, p=p: e.tensor_scalar(out=tC[p][:], in0=tC[p][:], scalar1=-7.0, scalar2=1.0,
                                                     op0=ALU.max, op1=ALU.add),
                      reads=[('tC', p)], writes=[('tC', p)])
                    G(lambda e, p=p: e.tensor_tensor(out=tD[p][:], in0=tA[p][:], in1=tB[p][:], op=ALU.mult),
                      reads=[('tA', p), ('tB', p)], writes=[('tD', p)])
                    G(lambda e, p=p, fc=fc, tg=tg: e.tensor_tensor(out=aT[:, fc, tg * 512:(tg + 1) * 512],
                                                                   in0=tD[p][:], in1=tC[p][:], op=ALU.mult),
                      reads=[('tD', p), ('tC', p)], writes=[('aT', tg)])
                if fc + 1 < 8:
                    w2_chunk(fc + 1)
            for t in range(NT):
                for nh in range(2):
                    bk = 4 + sc_[0] % 2
                    sc_[0] += 1

                    def mm2(e, bk=bk, t=t, nh=nh):
                        ins = None
                        for fc in range(8):
                            ins = e.matmul(bank(bk)[:, 0:512], lhsT=aT[:, fc, t * 128:(t + 1) * 128],
                                           rhs=W2[:, fc, nh * 512:(nh + 1) * 512], start=(fc == 0), stop=(fc == 7))
                        return ins
                    T(mm2, reads=[('aT', t // 4), 'W2'], writes=[('ps', bk)])
                    V(lambda e, bk=bk, t=t, nh=nh, ex=ex: e.scalar_tensor_tensor(
                        out=X[:, t, nh * 512:(nh + 1) * 512], in0=bank(bk)[:, 0:512], scalar=Gt[:, t, ex:ex + 1],
                        in1=X[:, t, nh * 512:(nh + 1) * 512], op0=ALU.mult, op1=ALU.add),
                      reads=[('ps', bk), ('X', t, nh)], writes=[('X', t, nh)])
        S.barrier()

    with contextlib.ExitStack() as p5b:
        B2s = sb(p5b, "B2s", [N_EXP, D], F32)
        GT = sb(p5b, "GT", [N_EXP, NT, 128], F32)
        dma('sync', B2s[:], b2_d, writes=['B2s'])
        V(lambda e: e.tensor_tensor(out=B2s[:], in0=B2s[:], in1=gtF_bc[0:N_EXP, :], op=ALU.mult),
          reads=['B2s'], writes=['B2s'])
        for t in range(NT):
            bk = 6 + t % 2
            T(lambda e, t=t, bk=bk: e.transpose(out=bank(bk)[0:N_EXP, 0:128], in_=Gt[:, t, :], identity=identf[:]),
              writes=[('ps', bk)])
            V(lambda e, t=t, bk=bk: e.tensor_copy(out=GT[:, t, :], in_=bank(bk)[0:N_EXP, 0:128]),
              reads=[('ps', bk)], writes=[('GT', t)])
        for t in range(NT):
            for nh in range(2):
                bk = next_pbank()
                T(lambda e, t=t, nh=nh, bk=bk: e.matmul(bank(bk)[:, 0:512], lhsT=GT[:, t, :],
                                                        rhs=B2s[:, nh * 512:(nh + 1) * 512], start=True, stop=True),
                  reads=[('GT', t), 'B2s'], writes=[('ps', bk)])
                V(lambda e, t=t, nh=nh, bk=bk: e.tensor_tensor(out=X[:, t, nh * 512:(nh + 1) * 512],
                                                               in0=bank(bk)[:, 0:512],
                                                               in1=X[:, t, nh * 512:(nh + 1) * 512], op=ALU.add),
                  reads=[('ps', bk), ('X', t, nh)], writes=[('X', t, nh)])
        S.barrier()

    with contextlib.ExitStack() as p6:
        gfin = sb(p6, "gfin", [128, D], F32)
        ot = [sb(p6, "ot%d" % i, [128, D], F32) for i in range(3)]
        junk6 = sb(p6, "junk6", [128, D], BF16)
        dma('sync', gfin[:], gfin_d, writes=['gfin'])
        NO6 = 3

        def s6_sq(t):
            i = 48 + t
            A(lambda e: e.activation(out=junk6[:], in_=X[:, t, :], func=AF.Square, accum_out=ss[:, i:i + 1]),
              reads=[('X', t, 0), ('X', t, 1)], writes=['junk6', ('ss', i)])

        def s6_ts(t):
            i = 48 + t
            V(lambda e: e.tensor_scalar(out=rs[:, i:i + 1], in0=ss[:, i:i + 1], scalar1=1.0 / D, scalar2=EPS,
                                        op0=ALU.mult, op1=ALU.add),
              reads=[('ss', i)], writes=[('rs', i)])

        def s6_sqrt(t):
            i = 48 + t
            A(lambda e: e.activation(out=rs[:, i:i + 1], in_=rs[:, i:i + 1], func=AF.Sqrt),
              reads=[('rs', i)], writes=[('rs', i)])

        def s6_out(t):
            i = 48 + t
            p = t % NO6
            V(lambda e: e.reciprocal(out=rs[:, i:i + 1], in_=rs[:, i:i + 1]),
              reads=[('rs', i)], writes=[('rs', i)])
            V(lambda e: e.scalar_tensor_tensor(out=ot[p][:], in0=X[:, t, :], scalar=rs[:, i:i + 1],
                                               in1=gfin[:], op0=ALU.mult, op1=ALU.mult),
              reads=[('X', t, 0), ('X', t, 1), ('rs', i), 'gfin'], writes=[('ot', p)])
            dma('sync', out_d[t * 128:(t + 1) * 128, :], ot[p][:], reads=[('ot', p)], semname="out%d" % p)
        stages6 = [(s6_sq, 0), (s6_ts, 1), (s6_sqrt, 2), (s6_out, 3)]
        for j in range(NT + 3):
            for fn, lag in stages6:
                t = j - lag
                if 0 <= t < NT:
                    fn(t)
    S.emit(final_waits=[k for k in (('D', 'out'), ('D', 'outg'), ('D', 'out0'), ('D', 'out1'), ('D', 'out2')) if k in S.cnt])
    root.close()
    return nc


def _t5_bucket(dist):
    dist = np.asarray(dist, dtype=np.int64)
    small = dist < 16
    ratio = np.log(np.maximum(dist, 1).astype(np.float32) / np.float32(16)) / np.float32(math.log(2048 / 16))
    large = 16 + (ratio.astype(np.float32) * np.float32(16)).astype(np.int32)
    large = np.minimum(large, 31)
    return np.where(small, dist, large)


def _constants():
    cst = {}
    half = 64
    inv = (10000.0 ** (-np.arange(half, dtype=np.float32) / half)).astype(np.float32)
    pos = np.arange(SEQ, dtype=np.float32)
    ang = pos[None, :] * inv[:, None]
    cos = np.cos(ang).astype(np.float32)
    sin = np.sin(ang).astype(np.float32)
    cst['cos_full'] = np.concatenate([cos, cos], axis=0)
    cst['sin_full'] = np.concatenate([-sin, sin], axis=0)
    Hh = 4
    log_g = np.log1p(-np.exp2(-5.0 - np.arange(Hh, dtype=np.float64)))
    i = np.arange(128, dtype=np.float64)
    scale = 128 ** -0.5
    dm = np.zeros((128, 4, 128), np.float32)
    for h in range(Hh):
        diff = i[None, :] - i[:, None]
        dm[:, h, :] = np.where(diff >= 0, np.exp(np.maximum(diff, 0) * log_g[h]) * scale, 0.0)
    rvec = np.zeros((128, 16), np.float32)
    for h in range(Hh):
        rvec[:, h] = np.exp((127.0 - i) * log_g[h]) * scale
        rvec[:, 4 + h] = np.exp((i + 1.0) * log_g[h])
        rvec[:, 8 + h] = np.exp(128.0 * log_g[h])
    cst['dmask'] = dm
    cst['rvec'] = rvec
    z2 = np.zeros((128, 4, 16), np.float32)
    for h in range(Hh):
        for n in range(16):
            z2[:, h, n] = np.exp((127.0 - i) * log_g[h] + 128.0 * (15 - n) * log_g[h]) * scale
    cst['zeta2'] = z2
    ohf = np.zeros((32, 3, 384), np.float32)
    for g, (w, r) in enumerate(ATT_PATTERNS):
        j = np.arange(129)
        bk = _t5_bucket(r * j)
        for jj in range(129):
            ohf[bk[jj], g, 127 + jj] = 1.0
    cst['ohf'] = ohf
    cst['identf'] = np.eye(128, dtype=np.float32)
    cst['jf'] = np.ascontiguousarray(np.eye(128, dtype=np.float32)[::-1])
    return cst


def _prep_shared(inp):
    sh = {}
    w_in = inp['w_in'][0]

    def chunk(cols):
        return w_in[:, cols].reshape(8, 128, len(cols)).transpose(1, 0, 2)

    def rng(a, n=128):
        return np.arange(a, a + n)
    r1 = []
    r2 = []
    for h in range(4):
        q0 = C_RQ + h * 128
        k0 = C_RK + h * 128
        r1.append(chunk(rng(q0)))
        r1.append(chunk(np.concatenate([rng(q0 + 64, 64), rng(q0, 64)])))
        r1.append(chunk(rng(k0)))
        r1.append(chunk(np.concatenate([rng(k0 + 64, 64), rng(k0, 64)])))
        r2.append(chunk(rng(C_RV + h * 256, 256)))
        r2.append(chunk(rng(C_RG + h * 256, 256)))
    for hg in range(4):
        for g in range(3):
            hd = g * 4 + hg
            r1.append(chunk(rng(C_AQ + hd * 128)))
            r1.append(chunk(rng(C_AK + hd * 128)))
            r1.append(chunk(rng(C_AV + hd * 128)))
    for m in range(8):
        r1.append(chunk(rng(C_GA + m * 128)))
    for m in range(8):
        r1.append(chunk(rng(C_GB + m * 128)))
    sh['w_in_r1'] = np.ascontiguousarray(np.stack(r1, axis=0))
    sh['w_in_r2'] = np.ascontiguousarray(np.stack(r2, axis=0))
    b_ada = inp['b_ada'][0]
    secs = [0, 1, 3, 4]
    bc = np.zeros((128, 32), np.float32)
    for si, s in enumerate(secs):
        bc[:, si * 8:(si + 1) * 8] = b_ada[s * 1024:(s + 1) * 1024].reshape(8, 128).T
    sh['bada_col'] = bc
    bb = np.stack([b_ada[2048:3072], b_ada[5120:6144]], axis=0)
    sh['bada_bc'] = np.ascontiguousarray(np.broadcast_to(bb[None], (128, 2, 1024)))
    sh['gmix_col'] = np.ascontiguousarray(inp['g_mix'][0].reshape(8, 128).T)
    sh['gffn_col'] = np.ascontiguousarray(inp['g_ffn'][0].reshape(8, 128).T)
    sh['gfin_bc'] = np.ascontiguousarray(np.broadcast_to(inp['g_final'][None, :], (128, 1024)))
    sh['w_ada'] = np.ascontiguousarray(inp['w_ada'][0])
    sh['rel_bias'] = np.ascontiguousarray(inp['rel_bias'])
    sh['w_ret_o'] = np.ascontiguousarray(inp['w_ret_o'][0])
    sh['w_att_o'] = np.ascontiguousarray(inp['w_att_o'][0])
    sh['w_o'] = np.ascontiguousarray(inp['w_o'][0])
    sh['w_router'] = np.ascontiguousarray(inp['w_router'][0])
    sh['brouter_bc'] = np.ascontiguousarray(np.broadcast_to(inp['b_router'][0][None, :], (128, 32)))
    w1 = inp['w_mlp1'][0].reshape(32, 8, 128, 8, 128, 2)
    sh['w1r'] = np.ascontiguousarray(w1.transpose(0, 3, 2, 1, 5, 4)).reshape(32, 8, 128, 8, 256)
    sh['b1r'] = np.ascontiguousarray(inp['b_mlp1'][0].reshape(32, 8, 128, 2).transpose(2, 0, 1, 3))
    sh['w_mlp2'] = np.ascontiguousarray(inp['w_mlp2'][0])
    sh['b_mlp2'] = np.ascontiguousarray(inp['b_mlp2'][0])
    return sh


def make_in_maps(inp):
    inp = {k: np.asarray(v, dtype=np.float32) for k, v in inp.items()}
    cst = _constants()
    sh = _prep_shared(inp)
    x = inp['x']
    c = inp['c']
    zeros_prev = np.zeros((HALF, D), np.float32)
    in_maps = []
    for core in range(8):
        b, hf = core // 2, core % 2
        m = dict(sh)
        m['x_own'] = np.ascontiguousarray(x[b, hf * HALF:(hf + 1) * HALF])
        m['x_prev'] = np.ascontiguousarray(x[b, 0:HALF]) if hf == 1 else zeros_prev
        m['flag'] = np.full((128, 1), float(hf), np.float32)
        m['c_col'] = np.ascontiguousarray(c[b].reshape(8, 128).T)
        if hf == 1:
            m['rope_cos'] = cst['cos_full']
            m['rope_sin'] = cst['sin_full']
        else:
            m['rope_cos'] = np.ascontiguousarray(np.concatenate([cst['cos_full'][:, :HALF]] * 2, axis=1))
            m['rope_sin'] = np.ascontiguousarray(np.concatenate([cst['sin_full'][:, :HALF]] * 2, axis=1))
        for k in ('dmask', 'rvec', 'zeta2', 'ohf', 'identf', 'jf'):
            m[k] = cst[k]
        in_maps.append(m)
    return in_maps


def kernel(**inputs):
    in_maps = make_in_maps(inputs)
    nc = build_program()
    res = run_bass_kernel_spmd(nc, in_maps, core_ids=list(range(8)))
    out = np.zeros((4, SEQ, D), np.float32)
    for core in range(8):
        b, hf = core // 2, core % 2
        out[b, hf * HALF:(hf + 1) * HALF] = res.results[core]["out"]
    return out
```

```python
import contextlib
import math
import numpy as np
import concourse.bass as bass
import concourse.mybir as mybir
from concourse.bass_utils import run_bass_kernel_spmd

F32 = mybir.dt.float32
BF16 = mybir.dt.bfloat16
AF = mybir.ActivationFunctionType
ALU = mybir.AluOpType
AX = mybir.AxisListType

D = 1024
SEQ = 4096
HALF = 2048
NT = 16
EPS = 1e-6
GN_EPS = 1e-5
N_EXP = 32
ATT_PATTERNS = ((128, 1), (512, 4), (2048, 16))
NSLOT = 4

C_RQ, C_RK, C_RV, C_RG = 0, 512, 1024, 2048
C_AQ, C_AK, C_AV = 3072, 4608, 6144
C_GA, C_GB = 7680, 8704


class Sched:
    ENG = ['sync', 'scalar', 'vector', 'gpsimd', 'tensor']

    def __init__(self, nc):
        self.nc = nc
        self.ops = {e: [] for e in self.ENG}
        self.cnt = {}
        self.waited = {e: {} for e in self.ENG}
        self.lastw = {}
        self.readers = {}

    def op(self, eng, fn, reads=(), writes=(), sem=None, inc=1):
        deps = {}
        own = ('E', eng)

        def add(d, raw):
            if d is None:
                return
            k, v = d
            if k == own and not raw:
                return
            if deps.get(k, 0) < v:
                deps[k] = v
        for b in reads:
            add(self.lastw.get(b), True)
        waw_own = eng in ('vector', 'gpsimd', 'scalar')
        for b in writes:
            add(self.lastw.get(b), waw_own)
            for r in self.readers.get(b, ()):
                add(r, False)
        waits = []
        for k, v in deps.items():
            if self.waited[eng].get(k, 0) < v:
                self.waited[eng][k] = v
                waits.append((k, v))
        semkey = sem if sem is not None else own
        self.cnt[semkey] = self.cnt.get(semkey, 0) + inc
        val = self.cnt[semkey]
        self.ops[eng].append((waits, fn, semkey, inc))
        for b in reads:
            self.readers.setdefault(b, []).append((semkey, val))
        for b in writes:
            self.lastw[b] = (semkey, val)
            self.readers[b] = []
        return val

    def barrier(self):
        for e in self.ENG:
            waits = []
            for k, v in self.cnt.items():
                if self.waited[e].get(k, 0) < v:
                    self.waited[e][k] = v
                    waits.append((k, v))
            if waits:
                self.ops[e].append((waits, None, None, 0))
        self.lastw = {}
        self.readers = {}

    def emit(self, final_waits=()):
        nc = self.nc
        keys = list(self.cnt.keys())
        with contextlib.ExitStack() as st:
            sems = {}
            for i, k in enumerate(keys):
                sems[k] = st.enter_context(nc.semaphore("s%d" % i))
            block = st.enter_context(nc.Block())

            def body(engname):
                def f(e):
                    for waits, fn, semkey, inc in self.ops[engname]:
                        for k, v in waits:
                            e.wait_ge(sems[k], v)
                        if fn is None:
                            continue
                        ins = fn(e)
                        ins.then_inc(sems[semkey], inc)
                    if engname == 'sync':
                        for k in final_waits:
                            e.wait_ge(sems[k], self.cnt[k])
                return f
            block.sync(body('sync'))
            block.scalar(body('scalar'))
            block.vector(body('vector'))
            block.gpsimd(body('gpsimd'))
            block.tensor(body('tensor'))


def build_program(stage=99, dbg_cols=0):
    nc = bass.Bass("TRN2", target_bir_lowering=False)
    S = Sched(nc)

    def din(name, shape, dt=F32):
        return nc.dram_tensor(name, list(shape), dt, kind="ExternalInput").ap()

    x_own = din("x_own", [HALF, D])
    x_prev = din("x_prev", [HALF, D])
    flag_d = din("flag", [128, 1])
    ccol_d = din("c_col", [128, 8])
    w_ada_d = din("w_ada", [D, 6 * D])
    badacol_d = din("bada_col", [128, 32])
    badabc_d = din("bada_bc", [128, 2, D])
    gmix_d = din("gmix_col", [128, 8])
    gffn_d = din("gffn_col", [128, 8])
    gfin_d = din("gfin_bc", [128, D])
    win1_d = din("w_in_r1", [68, 128, 8, 128])
    win2_d = din("w_in_r2", [8, 128, 8, 256])
    ropec_d = din("rope_cos", [128, SEQ])
    ropes_d = din("rope_sin", [128, SEQ])
    dm_d = din("dmask", [128, 4, 128])
    rvec_d = din("rvec", [128, 16])
    zeta2_d = din("zeta2", [128, 4, 16])
    relb_d = din("rel_bias", [32, 12])
    ohf_d = din("ohf", [32, 3, 384])
    identf_d = din("identf", [128, 128])
    jf_d = din("jf", [128, 128])
    wreto_d = din("w_ret_o", [D, D])
    watto_d = din("w_att_o", [512, D])
    wo_d = din("w_o", [D, D])
    wr_d = din("w_router", [D, N_EXP])
    brbc_d = din("brouter_bc", [128, N_EXP])
    if stage > 5:
        w1r_d = din("w1r", [N_EXP, 8, 128, 8, 256])
        b1r_d = din("b1r", [128, N_EXP, 8, 2])
        w2_d = din("w_mlp2", [N_EXP, D, D])
        b2_d = din("b_mlp2", [N_EXP, D])
    out_d = nc.dram_tensor("out", [HALF, D], F32, kind="ExternalOutput").ap()
    fd_t = nc.dram_tensor("fd_scratch", [12, 384], F32)
    fd_d = fd_t.ap()
    dbg_d = None
    if dbg_cols:
        dbg_d = nc.dram_tensor("dbg", [128, dbg_cols], F32, kind="ExternalOutput").ap()

    root = contextlib.ExitStack()
    root.__enter__()

    def sb(st, name, shape, dt, side=None):
        name = "sb_" + name
        if side is None:
            return st.enter_context(nc.sbuf_tensor(name, list(shape), dt))
        return st.enter_context(nc.sbuf_tensor(name, list(shape), dt, side=side))

    PS = root.enter_context(nc.psum_tensor("psall", [128, 8, 512], F32))

    def bank(i):
        return PS[:, i, :]

    def bank_bf(i):
        return PS[:, i, :].bitcast(BF16)

    dma_n = [0]

    def dma(eng, out, in_, reads=(), writes=(), semname=None):
        if semname is None:
            semname = "u%d" % dma_n[0]
            dma_n[0] += 1
        return S.op(eng, lambda e: e.dma_start(out=out, in_=in_), reads=reads, writes=writes,
                    sem=('D', semname), inc=16)

    def V(fn, reads=(), writes=()):
        return S.op('vector', fn, reads, writes)

    def A(fn, reads=(), writes=()):
        return S.op('scalar', fn, reads, writes)

    def G(fn, reads=(), writes=()):
        return S.op('gpsimd', fn, reads, writes)

    def T(fn, reads=(), writes=()):
        return S.op('tensor', fn, reads, writes)

    flag = sb(root, "flag", [128, 1], F32)
    identb = sb(root, "identb", [128, 128], BF16)
    identf = sb(root, "identf", [128, 128], F32)
    onesb = sb(root, "onesb", [128, 128], BF16)
    onesf = sb(root, "onesf", [128, 128], F32)
    modcol = sb(root, "modcol", [128, 32], F32)
    scA = sb(root, "scA", [128, 8], F32)
    scAp = sb(root, "scAp", [128, 8], F32)
    shAp = sb(root, "shAp", [128, 8], F32)
    scF = sb(root, "scF", [128, 8], F32)
    gtA_bc = sb(root, "gtA_bc", [128, D], F32)
    gtF_bc = sb(root, "gtF_bc", [128, D], F32)
    ss = sb(root, "ss", [128, 64], F32)
    rs = sb(root, "rs", [128, 64], F32)
    shA = modcol[:, 0:8]
    shF = modcol[:, 16:24]

    dma('sync', flag[:], flag_d, writes=['flag'])
    dma('sync', identf[:], identf_d, writes=['identf'])
    dma('gpsimd', identb[:], identf_d, writes=['identb'])
    V(lambda e: e.memset(onesb[:], 1.0), writes=['onesb'])
    V(lambda e: e.memset(onesf[:], 1.0), writes=['onesf'])

    with contextlib.ExitStack() as p0:
        ccol = sb(p0, "ccol", [128, 8], F32)
        cs2 = sb(p0, "cs2", [128, 8, 2], F32)
        csb = sb(p0, "csb", [128, 8, 128], F32)
        badacol = sb(p0, "badacol", [128, 32], F32)
        badabc = sb(p0, "badabc", [128, 2, D], F32)
        gmix = sb(p0, "gmix", [128, 8], F32)
        gffn = sb(p0, "gffn", [128, 8], F32)
        wast = [sb(p0, "wast%d" % i, [128, 8, 512], F32) for i in range(3)]
        dma('sync', ccol[:], ccol_d, writes=['ccol'])
        dma('sync', badacol[:], badacol_d, writes=['badacol'])
        dma('sync', badabc[:], badabc_d, writes=['badabc'])
        dma('sync', gmix[:], gmix_d, writes=['gmix'])
        dma('sync', gffn[:], gffn_d, writes=['gffn'])
        for j in range(2):
            A(lambda e, j=j: e.activation(out=cs2[:, :, j], in_=ccol[:], func=AF.Silu),
              reads=['ccol'], writes=[('cs2', j)])
        for kc in range(8):
            V(lambda e, kc=kc: e.tensor_scalar(out=csb[:, kc, :], in0=onesf[:], scalar1=cs2[:, kc, 0:1],
                                               scalar2=None, op0=ALU.mult),
              reads=['onesf', ('cs2', 0)], writes=['csb'])
        w_ada_v = w_ada_d.rearrange("(kc p) n -> p kc n", p=128)
        colsec = {0: 0, 1: 1, 3: 2, 4: 3}
        for j in range(12):
            sec, half = j // 2, j % 2
            wt = wast[j % 3]
            dma('sync', wt[:], w_ada_v[:, :, j * 512:(j + 1) * 512], writes=[('wast', j % 3)],
                semname="wast%d" % (j % 3))
            if sec in colsec:
                si = colsec[sec]

                def mmcol(e, wt=wt, si=si, half=half):
                    ins = None
                    for cc in range(4):
                        c = half * 4 + cc
                        o = (si * 8 + c) * 2
                        for kc in range(8):
                            ins = e.matmul(bank(0)[:, o:o + 2], lhsT=wt[:, kc, cc * 128:(cc + 1) * 128],
                                           rhs=cs2[:, kc, :], start=(kc == 0), stop=(kc == 7))
                    return ins
                T(mmcol, reads=[('wast', j % 3), ('cs2', 0), ('cs2', 1)], writes=[('ps', 0)])
            else:
                bk = 1 + half
                which = 0 if sec == 2 else 1
                dst = gtA_bc if sec == 2 else gtF_bc

                def mmrow(e, wt=wt, bk=bk):
                    ins = None
                    for kc in range(8):
                        ins = e.matmul(bank(bk)[:, 0:512], lhsT=csb[:, kc, :], rhs=wt[:, kc, :],
                                       start=(kc == 0), stop=(kc == 7))
                    return ins
                T(mmrow, reads=[('wast', j % 3), 'csb'], writes=[('ps', bk)])
                V(lambda e, bk=bk, dst=dst, which=which, half=half: e.tensor_tensor(
                    out=dst[:, half * 512:(half + 1) * 512], in0=bank(bk)[:, 0:512],
                    in1=badabc[:, which, half * 512:(half + 1) * 512], op=ALU.add),
                  reads=[('ps', bk), 'badabc'], writes=[('gt', sec, half)])
        V(lambda e: e.tensor_tensor(out=modcol[:], in0=bank(0)[:, 0:64:2], in1=badacol[:], op=ALU.add),
          reads=[('ps', 0), 'badacol'], writes=['modcol'])
        V(lambda e: e.scalar_tensor_tensor(out=scA[:], in0=modcol[:, 8:16], scalar=1.0, in1=gmix[:],
                                           op0=ALU.add, op1=ALU.mult),
          reads=['modcol', 'gmix'], writes=['scA'])
        V(lambda e: e.scalar_tensor_tensor(out=scF[:], in0=modcol[:, 24:32], scalar=1.0, in1=gffn[:],
                                           op0=ALU.add, op1=ALU.mult),
          reads=['modcol', 'gffn'], writes=['scF'])
        V(lambda e: e.tensor_scalar(out=scAp[:], in0=scA[:], scalar1=flag[:, 0:1], scalar2=None, op0=ALU.mult),
          reads=['scA', 'flag'], writes=['scAp'])
        V(lambda e: e.tensor_scalar(out=shAp[:], in0=modcol[:, 0:8], scalar1=flag[:, 0:1], scalar2=None,
                                    op0=ALU.mult),
          reads=['modcol', 'flag'], writes=['shAp'])
        S.barrier()

    if stage == 0:
        dma('sync', dbg_d[:, 0:32], modcol[:], semname="out")
        dma('sync', dbg_d[:, 32:40], scA[:], semname="out")
        dma('sync', dbg_d[:, 40:48], scAp[:], semname="out")
        dma('sync', dbg_d[:, 48:56], scF[:], semname="out")
        dma('sync', dbg_d[:, 64:64 + 1024], gtA_bc[:], semname="out")
        dma('sync', dbg_d[:, 1088:1088 + 1024], gtF_bc[:], semname="out")
        S.emit(final_waits=[k for k in (('D', 'out'), ('D', 'outg')) if k in S.cnt])
        root.close()
        return nc

    mix = contextlib.ExitStack()
    mix.__enter__()
    HT = sb(mix, "HT", [128, 8, SEQ], BF16)
    RetT = sb(mix, "RetT", [128, 8, HALF], BF16)
    WB = [sb(mix, "WB%d" % i, [128, 8, 256], BF16) for i in range(NSLOT)]

    ropesc = contextlib.ExitStack()
    ropesc.__enter__()
    ropeC = sb(ropesc, "ropeC", [128, SEQ], BF16, side="right")
    ropeS = sb(ropesc, "ropeS", [128, SEQ], BF16, side="right")
    dma('gpsimd', ropeC[:], ropec_d, writes=['ropeC'])
    dma('gpsimd', ropeS[:], ropes_d, writes=['ropeS'])

    with contextlib.ExitStack() as p1:
        NXI, NXN = 6, 3
        xin = [sb(p1, "xin%d" % i, [128, D], F32) for i in range(NXI)]
        xn = [sb(p1, "xn%d" % i, [128, D], BF16) for i in range(NXN)]
        junk = sb(p1, "junk", [128, D], BF16)

        def st_dma(i):
            src = x_prev if i < 16 else x_own
            t = i % 16
            dma('sync', xin[i % NXI][:], src[t * 128:(t + 1) * 128, :], writes=[('xin', i % NXI)],
                semname="xin%d" % (i % NXI))

        def st_sq(i):
            A(lambda e: e.activation(out=junk[:], in_=xin[i % NXI][:], func=AF.Square, accum_out=ss[:, i:i + 1]),
              reads=[('xin', i % NXI)], writes=['junk', ('ss', i)])

        def st_ts(i):
            V(lambda e: e.tensor_scalar(out=rs[:, i:i + 1], in0=ss[:, i:i + 1], scalar1=1.0 / D, scalar2=EPS,
                                        op0=ALU.mult, op1=ALU.add),
              reads=[('ss', i)], writes=[('rs', i)])

        def st_sqrt(i):
            A(lambda e: e.activation(out=rs[:, i:i + 1], in_=rs[:, i:i + 1], func=AF.Sqrt),
              reads=[('rs', i)], writes=[('rs', i)])

        def st_xn(i):
            V(lambda e: e.reciprocal(out=rs[:, i:i + 1], in_=rs[:, i:i + 1]),
              reads=[('rs', i)], writes=[('rs', i)])
            V(lambda e: e.tensor_scalar(out=xn[i % NXN][:], in0=xin[i % NXI][:], scalar1=rs[:, i:i + 1],
                                        scalar2=None, op0=ALU.mult),
              reads=[('xin', i % NXI), ('rs', i)], writes=[('xn', i % NXN)])

        def st_tr(i):
            pT = bank_bf(i % 4)

            def tr(e):
                ins = None
                for kc in range(8):
                    ins = e.transpose(out=pT[:, kc * 128:(kc + 1) * 128],
                                      in_=xn[i % NXN][:, kc * 128:(kc + 1) * 128], identity=identb[:])
                return ins
            T(tr, reads=[('xn', i % NXN)], writes=[('ps', i % 4)])

        def st_ev(i):
            pT = bank_bf(i % 4)
            sc_, sh_ = (scAp, shAp) if i < 16 else (scA, shA)
            for kc in range(8):
                dst = HT[:, kc, i * 128:(i + 1) * 128]
                srcp = pT[:, kc * 128:(kc + 1) * 128]
                if i % 2 == 0:
                    A(lambda e, dst=dst, srcp=srcp, kc=kc: e.activation(
                        out=dst, in_=srcp, func=AF.Identity, bias=sh_[:, kc:kc + 1], scale=sc_[:, kc:kc + 1]),
                      reads=[('ps', i % 4)], writes=[('HTa', i)])
                else:
                    V(lambda e, dst=dst, srcp=srcp, kc=kc: e.tensor_scalar(
                        out=dst, in0=srcp, scalar1=sc_[:, kc:kc + 1], scalar2=sh_[:, kc:kc + 1],
                        op0=ALU.mult, op1=ALU.add),
                      reads=[('ps', i % 4)], writes=[('HTv', i)])
        st_dma(0)
        st_dma(1)
        stages = [(st_sq, 0), (st_ts, 1), (st_sqrt, 2), (st_xn, 3), (st_tr, 4), (st_ev, 5)]
        for j in range(32 + 5):
            if j + 2 < 32:
                st_dma(j + 2)
            for fn, lag in stages:
                i = j - lag
                if 0 <= i < 32:
                    fn(i)
        S.barrier()

    if dbg_cols:
        dma('gpsimd', dbg_d[:, 0:4096], HT[:, 0, :], semname="outg")
        dma('gpsimd', dbg_d[:, 4096:8192], HT[:, 5, :], semname="outg")
    if stage == 1:
        S.emit(final_waits=[k for k in (('D', 'out'), ('D', 'outg')) if k in S.cnt])
        mix.close()
        root.close()
        return nc

    loads = []
    for h in range(4):
        for k in range(4):
            loads.append((win1_d[h * 4 + k], 1))
        loads.append((win2_d[h * 2 + 0], 2))
        loads.append((win2_d[h * 2 + 1], 2))
    for hg in range(4):
        for g in range(3):
            for k in range(3):
                loads.append((win1_d[16 + (hg * 3 + g) * 3 + k], 1))
    wreto_v = wreto_d.rearrange("(kc p) n -> p kc n", p=128)
    watto_v = watto_d.rearrange("(kc p) n -> p kc n", p=128)
    for m in range(8):
        loads.append((win1_d[52 + m], 1))
        loads.append((win1_d[60 + m], 1))
        loads.append((wreto_v[:, :, m * 128:(m + 1) * 128], 1))
        loads.append((watto_v[:, :, m * 128:(m + 1) * 128], 3))
    ring = {'issued': 0, 'consumed': 0}
    N_MAIN_LOADS = 60

    def ring_issue():
        n = ring['issued']
        s = n % NSLOT
        src, kind = loads[n]
        if kind == 1:
            dst = WB[s][:, :, 0:128]
        elif kind == 2:
            dst = WB[s][:, :, :]
        else:
            dst = WB[s][:, 0:4, 0:128]
        dma('gpsimd', dst, src, writes=[('WB', s)], semname="WB%d" % s)
        ring['issued'] += 1

    def ring_get(hold=1):
        while ring['issued'] < min(N_MAIN_LOADS, ring['consumed'] + NSLOT - hold + 1):
            ring_issue()
        s = ring['consumed'] % NSLOT
        ring['consumed'] += 1
        return s

    pbank = [0]

    def next_pbank():
        b = pbank[0]
        pbank[0] = (pbank[0] + 1) % 4
        return b

    def fm_mm(bk, s, col0, ncols=512, nk=8, src=None, wcols=(0, 128)):
        src = HT if src is None else src

        def f(e):
            ins = None
            for kc in range(nk):
                ins = e.matmul(bank(bk)[:, 0:ncols], lhsT=WB[s][:, kc, wcols[0]:wcols[1]],
                               rhs=src[:, kc, col0:col0 + ncols], start=(kc == 0), stop=(kc == nk - 1))
            return ins
        return f

    def tm_mm(bk, s, tok_sl, ncols):
        def f(e):
            ins = None
            for kc in range(8):
                ins = e.matmul(bank(bk)[:, 0:ncols], lhsT=HT[:, kc, tok_sl], rhs=WB[s][:, kc, 0:ncols],
                               start=(kc == 0), stop=(kc == 7))
            return ins
        return f

    with contextlib.ExitStack() as rsc:
        Dm = sb(rsc, "Dm", [128, 4, 128], F32)
        rvec = sb(rsc, "rvec", [128, 16], F32)
        zeta2 = sb(rsc, "zeta2", [128, 4, 16], F32)
        qT = sb(rsc, "qT", [128, HALF], BF16)
        kT = sb(rsc, "kT", [128, SEQ], BF16)
        Vt = sb(rsc, "Vt", [128, 32, 256], BF16)
        SG = sb(rsc, "SG", [128, 16, 256], BF16)
        t1 = [sb(rsc, "t1_%d" % i, [128, 512], F32) for i in range(2)]
        t2 = [sb(rsc, "t2_%d" % i, [128, 512], F32) for i in range(2)]
        Kz = [sb(rsc, "Kz%d" % i, [128, 128], BF16) for i in range(2)]
        PT = [sb(rsc, "PT%d" % i, [128, 128], BF16) for i in range(2)]
        yI = [sb(rsc, "yI%d" % i, [128, 256], F32) for i in range(2)]
        yy = [sb(rsc, "yy%d" % i, [128, 256], F32) for i in range(2)]
        yn = [sb(rsc, "yn%d" % i, [128, 256], BF16) for i in range(2)]
        yg = [sb(rsc, "yg%d" % i, [128, 256], BF16) for i in range(2)]
        st6 = [sb(rsc, "st6_%d" % i, [128, 6], F32) for i in range(2)]
        mv = [sb(rsc, "mv%d" % i, [128, 2], F32) for i in range(2)]
        rstd = [sb(rsc, "rstd%d" % i, [128, 1], F32) for i in range(2)]
        nmr = [sb(rsc, "nmr%d" % i, [128, 1], F32) for i in range(2)]
        Rf = sb(rsc, "Rf", [128, 256], F32)
        Rb = sb(rsc, "Rb", [128, 256], BF16)
        dma('sync', Dm[:], dm_d, writes=['Dm'])
        dma('sync', rvec[:], rvec_d, writes=['rvec'])
        dma('sync', zeta2[:], zeta2_d, writes=['zeta2'])
        ecnt = [0]

        for h in range(4):
            sq, sqs, sk, sks = None, None, None, None
            for which in ('q', 'k'):
                sa = ring_get(1)
                sbw = ring_get(2)
                ntg = 4 if which == 'q' else 8
                for tg in range(ntg):
                    col0 = (HALF + tg * 512) if which == 'q' else tg * 512
                    bA, bB = next_pbank(), next_pbank()
                    T(fm_mm(bA, sa, col0), reads=[('WB', sa)], writes=[('ps', bA)])
                    T(fm_mm(bB, sbw, col0), reads=[('WB', sbw)], writes=[('ps', bB)])
                    par = ecnt[0] % 2
                    ecnt[0] += 1
                    V(lambda e, bA=bA, col0=col0, par=par: e.tensor_tensor(
                        out=t1[par][:], in0=bank(bA)[:, 0:512], in1=ropeC[:, col0:col0 + 512], op=ALU.mult),
                      reads=[('ps', bA), 'ropeC'], writes=[('t1', par)])
                    V(lambda e, bB=bB, col0=col0, par=par: e.tensor_tensor(
                        out=t2[par][:], in0=bank(bB)[:, 0:512], in1=ropeS[:, col0:col0 + 512], op=ALU.mult),
                      reads=[('ps', bB), 'ropeS'], writes=[('t2', par)])
                    if which == 'q':
                        dst = qT[:, tg * 512:(tg + 1) * 512]
                        key = ('qT', tg)
                    else:
                        dst = kT[:, tg * 512:(tg + 1) * 512]
                        key = ('kT', tg)
                    G(lambda e, dst=dst, par=par: e.tensor_tensor(out=dst, in0=t1[par][:], in1=t2[par][:], op=ALU.add),
                      reads=[('t1', par), ('t2', par)], writes=[key])
            sv = ring_get()
            for t in range(32):
                bk = next_pbank()
                T(tm_mm(bk, sv, slice(t * 128, (t + 1) * 128), 256), reads=[('WB', sv)], writes=[('ps', bk)])
                A(lambda e, bk=bk, t=t: e.activation(out=Vt[:, t, :], in_=bank(bk)[:, 0:256], func=AF.Copy),
                  reads=[('ps', bk)], writes=[('V', t)])
            sg_ = ring_get()
            for t in range(16):
                bk = next_pbank()
                T(tm_mm(bk, sg_, slice(HALF + t * 128, HALF + (t + 1) * 128), 256), reads=[('WB', sg_)],
                  writes=[('ps', bk)])
                A(lambda e, bk=bk, t=t: e.activation(out=SG[:, t, :], in_=bank(bk)[:, 0:256], func=AF.Silu),
                  reads=[('ps', bk)], writes=[('SG', t)])
            for n in range(16):
                p = n % 2
                ksl = slice(n * 128, (n + 1) * 128)
                T(lambda e, ksl=ksl: e.transpose(out=bank_bf(4)[:, 0:128], in_=kT[:, ksl], identity=identb[:]),
                  reads=[('kT', n // 4)], writes=[('ps', 4)])
                V(lambda e, p=p, h=h, n=n: e.tensor_scalar(out=Kz[p][:], in0=bank_bf(4)[:, 0:128],
                                                           scalar1=zeta2[:, h, n:n + 1], scalar2=None, op0=ALU.mult),
                  reads=[('ps', 4), 'zeta2'], writes=[('Kz', p)])
                T(lambda e, p=p, n=n: e.matmul(bank(2)[:, 0:256], lhsT=Kz[p][:], rhs=Vt[:, n, :],
                                               start=(n == 0), stop=(n == 15)),
                  reads=[('Kz', p), ('V', n)], writes=[('ps', 2)])
            V(lambda e: e.tensor_copy(out=Rb[:], in_=bank(2)[:, 0:256]), reads=[('ps', 2)], writes=['Rb'])
            V(lambda e: e.tensor_copy(out=Rf[:], in_=bank(2)[:, 0:256]), reads=[('ps', 2)], writes=['Rf'])
            for n in range(16, 32):
                own = True
                p = n % 2
                ksl = slice(n * 128, (n + 1) * 128)
                T(lambda e, ksl=ksl: e.transpose(out=bank_bf(4)[:, 0:128], in_=kT[:, ksl], identity=identb[:]),
                  reads=[('kT', n // 4)], writes=[('ps', 4)])
                V(lambda e, p=p, h=h: e.tensor_scalar(out=Kz[p][:], in0=bank_bf(4)[:, 0:128],
                                                      scalar1=rvec[:, h:h + 1], scalar2=None, op0=ALU.mult),
                  reads=[('ps', 4), 'rvec'], writes=[('Kz', p)])
                if own:
                    j = n - 16
                    qsl = slice(j * 128, (j + 1) * 128)
                    T(lambda e, ksl=ksl, qsl=qsl: e.matmul(bank(5)[:, 0:128], lhsT=kT[:, ksl], rhs=qT[:, qsl],
                                                           start=True, stop=True),
                      reads=[('kT', n // 4), ('qT', j // 4)], writes=[('ps', 5)])
                    V(lambda e, p=p, h=h: e.tensor_tensor(out=PT[p][:], in0=bank(5)[:, 0:128], in1=Dm[:, h, :],
                                                          op=ALU.mult),
                      reads=[('ps', 5), 'Dm'], writes=[('PT', p)])
                    bI = 6 + p
                    T(lambda e, p=p, n=n, bI=bI: e.matmul(bank(bI)[:, 0:256], lhsT=PT[p][:], rhs=Vt[:, n, :],
                                                          start=True, stop=True),
                      reads=[('PT', p), ('V', n)], writes=[('ps', bI)])
                    bC = 0 + p
                    T(lambda e, qsl=qsl, bC=bC: e.matmul(bank(bC)[:, 0:256], lhsT=qT[:, qsl], rhs=Rb[:],
                                                         start=True, stop=True),
                      reads=[('qT', j // 4), 'Rb'], writes=[('ps', bC)])
                    A(lambda e, p=p, bI=bI: e.activation(out=yI[p][:], in_=bank(bI)[:, 0:256], func=AF.Copy),
                      reads=[('ps', bI)], writes=[('yI', p)])
                    V(lambda e, p=p, bC=bC, h=h: e.scalar_tensor_tensor(
                        out=yy[p][:], in0=bank(bC)[:, 0:256], scalar=rvec[:, 4 + h:5 + h], in1=yI[p][:],
                        op0=ALU.mult, op1=ALU.add),
                      reads=[('ps', bC), ('yI', p), 'rvec'], writes=[('yy', p)])
                    V(lambda e, p=p: e.bn_stats(out=st6[p][:], in_=yy[p][:]), reads=[('yy', p)], writes=[('st6', p)])
                    V(lambda e, p=p: e.bn_aggr(out=mv[p][:], in_=st6[p][:]), reads=[('st6', p)], writes=[('mv', p)])
                    V(lambda e, p=p: e.tensor_scalar(out=rstd[p][:], in0=mv[p][:, 1:2], scalar1=GN_EPS, scalar2=None,
                                                     op0=ALU.add),
                      reads=[('mv', p)], writes=[('rstd', p)])
                    A(lambda e, p=p: e.activation(out=rstd[p][:], in_=rstd[p][:], func=AF.Sqrt),
                      reads=[('rstd', p)], writes=[('rstd', p)])
                    V(lambda e, p=p: e.reciprocal(out=rstd[p][:], in_=rstd[p][:]),
                      reads=[('rstd', p)], writes=[('rstd', p)])
                    V(lambda e, p=p: e.scalar_tensor_tensor(out=nmr[p][:], in0=mv[p][:, 0:1], scalar=-1.0,
                                                            in1=rstd[p][:], op0=ALU.mult, op1=ALU.mult),
                      reads=[('mv', p), ('rstd', p)], writes=[('nmr', p)])
                    A(lambda e, p=p: e.activation(out=yn[p][:], in_=yy[p][:], func=AF.Identity,
                                                  bias=nmr[p][:, 0:1], scale=rstd[p][:, 0:1]),
                      reads=[('yy', p), ('rstd', p), ('nmr', p)], writes=[('yn', p)])
                    G(lambda e, p=p, j=j: e.tensor_tensor(out=yg[p][:], in0=yn[p][:], in1=SG[:, j, :], op=ALU.mult),
                      reads=[('yn', p), ('SG', j)], writes=[('yg', p)])

                    def trY(e, p=p):
                        ins = None
                        for dc in range(2):
                            ins = e.transpose(out=bank_bf(3)[:, dc * 128:(dc + 1) * 128],
                                              in_=yg[p][:, dc * 128:(dc + 1) * 128], identity=identb[:])
                        return ins
                    T(trY, reads=[('yg', p)], writes=[('ps', 3)])
                    for dc in range(2):
                        A(lambda e, h=h, dc=dc, qsl=qsl: e.activation(
                            out=RetT[:, h * 2 + dc, qsl], in_=bank_bf(3)[:, dc * 128:(dc + 1) * 128], func=AF.Copy),
                          reads=[('ps', 3)], writes=[('RetT', j)])
                if n < 31:
                    T(lambda e, p=p, n=n: e.matmul(bank(2)[:, 0:256], lhsT=Kz[p][:], rhs=Vt[:, n, :],
                                                   start=True, stop=True),
                      reads=[('Kz', p), ('V', n)], writes=[('ps', 2)])
                    V(lambda e, h=h: e.scalar_tensor_tensor(out=Rf[:], in0=Rf[:], scalar=rvec[:, 8 + h:9 + h],
                                                            in1=bank(2)[:, 0:256], op0=ALU.mult, op1=ALU.add),
                      reads=['Rf', ('ps', 2), 'rvec'], writes=['Rf'])
                    A(lambda e: e.activation(out=Rb[:], in_=Rf[:], func=AF.Copy), reads=['Rf'], writes=['Rb'])
        S.barrier()

    ropesc.close()

    if dbg_cols:
        for kc in range(8):
            dma('gpsimd', dbg_d[:, 8192 + kc * HALF:8192 + (kc + 1) * HALF], RetT[:, kc, :], semname="outg")
    if stage == 2:
        S.emit(final_waits=[k for k in (('D', 'out'), ('D', 'outg')) if k in S.cnt])
        mix.close()
        root.close()
        return nc

    AttT = sb(mix, "AttT", [128, 4, HALF], BF16)
    with contextlib.ExitStack() as asc:
        EB = sb(asc, "EB", [128, 12, 256], BF16)
        EBm = sb(asc, "EBm", [128, 12, 256], BF16)
        with contextlib.ExitStack() as ebs:
            relb = sb(ebs, "relb", [32, 12], F32)
            ohf = sb(ebs, "ohf", [32, 3, 384], F32)
            jf = sb(ebs, "jf", [128, 128], F32)
            Fsb = sb(ebs, "Fsb", [4, 3, 384], F32)
            Gh = [sb(ebs, "Gh%d" % i, [128, 256], F32) for i in range(2)]
            dma('sync', relb[:], relb_d, writes=['relb'])
            dma('sync', ohf[:], ohf_d, writes=['ohf'])
            dma('sync', jf[:], jf_d, writes=['jf'])
            A(lambda e: e.activation(out=relb[:], in_=relb[:], func=AF.Exp), reads=['relb'], writes=['relb'])
            for g in range(3):
                T(lambda e, g=g: e.matmul(bank(0)[0:4, 0:384], lhsT=relb[:, g * 4:(g + 1) * 4], rhs=ohf[:, g, :],
                                          start=True, stop=True),
                  reads=['relb', 'ohf'], writes=[('ps', 0)])
                V(lambda e, g=g: e.tensor_copy(out=Fsb[:, g, :], in_=bank(0)[0:4, 0:384]),
                  reads=[('ps', 0)], writes=['Fsb'])
            dma('sync', fd_d.rearrange("(g h) m -> h g m", g=3), Fsb[:], reads=['Fsb'], writes=['fd'])
            for idx in range(12):
                hk = bass.AP(tensor=fd_t, offset=idx * 384, ap=[[1, 128], [1, 256]])
                dma('sync', Gh[idx % 2][:], hk, reads=['fd'], writes=[('Gh', idx % 2)], semname="Gh%d" % (idx % 2))
                bk = 1 + idx % 2
                T(lambda e, idx=idx, bk=bk: e.matmul(bank(bk)[:, 0:256], lhsT=jf[:], rhs=Gh[idx % 2][:],
                                                     start=True, stop=True),
                  reads=['jf', ('Gh', idx % 2)], writes=[('ps', bk)])
                V(lambda e, idx=idx, bk=bk: e.tensor_copy(out=EB[:, idx, :], in_=bank(bk)[:, 0:256]),
                  reads=[('ps', bk)], writes=[('EBa', idx)])
                V(lambda e, idx=idx, bk=bk: e.tensor_copy(out=EBm[:, idx, 0:128], in_=bank(bk)[:, 0:128]),
                  reads=[('ps', bk)], writes=[('EBb', idx)])
                V(lambda e, idx=idx, bk=bk: e.tensor_scalar(out=EBm[:, idx, 128:256], in0=bank(bk)[:, 128:256],
                                                            scalar1=flag[:, 0:1], scalar2=None, op0=ALU.mult),
                  reads=[('ps', bk)], writes=[('EBv', idx)])
            S.barrier()

        if dbg_cols:
            for idx in range(12):
                dma('gpsimd', dbg_d[:, 24576 + idx * 256:24576 + (idx + 1) * 256], EB[:, idx, :], semname="outg")
        if stage == 25:
            S.emit(final_waits=[k for k in (('D', 'out'), ('D', 'outg')) if k in S.cnt])
            asc.close()
            mix.close()
            root.close()
            return nc

        qa = [sb(asc, "qa%d" % i, [128, HALF], BF16) for i in range(2)]
        ka = [sb(asc, "ka%d" % i, [128, SEQ], BF16) for i in range(2)]
        VA = sb(asc, "VA", [128, 32, 128], BF16)
        ND = sb(asc, "ND", [128, 2, HALF], F32)
        Et = [sb(asc, "Et%d" % i, [128, 256], F32) for i in range(2)]
        Pt = [sb(asc, "Pt%d" % i, [128, 256], BF16) for i in range(2)]
        blkc = [0]
        items = [(hg, g) for hg in range(4) for g in range(3)]

        def qk_ops(ii):
            hg, g = items[ii]
            par = ii % 2
            qT_, kT_ = qa[par], ka[par]
            ops = []
            st = {}

            def q_op(tg):
                def f():
                    if 'sq' not in st:
                        st['sq'] = ring_get(1)
                    s_q = st['sq']
                    bk = next_pbank()
                    T(fm_mm(bk, s_q, HALF + tg * 512), reads=[('WB', s_q)], writes=[('ps', bk)])
                    A(lambda e: e.activation(out=qT_[:, tg * 512:(tg + 1) * 512], in_=bank(bk)[:, 0:512],
                                             func=AF.Copy, scale=float(128 ** -0.5)),
                      reads=[('ps', bk)], writes=[('qa', par)])
                return f

            def k_op(tg):
                def f():
                    if 'sk' not in st:
                        st['sk'] = ring_get(1)
                    s_k = st['sk']
                    bk = next_pbank()
                    T(fm_mm(bk, s_k, tg * 512), reads=[('WB', s_k)], writes=[('ps', bk)])
                    V(lambda e: e.tensor_copy(out=kT_[:, tg * 512:(tg + 1) * 512], in_=bank(bk)[:, 0:512]),
                      reads=[('ps', bk)], writes=[('ka', par)])
                return f
            for tg in range(4):
                ops.append(q_op(tg))
            for tg in (list(range(8)) if g == 2 else list(range(3, 8))):
                ops.append(k_op(tg))
            return ops

        def v_proj(ii):
            hg, g = items[ii]
            w, r = ATT_PATTERNS[g]
            nbb = 16 // r
            s_v = ring_get(1)
            vblocks = []
            for bb in range(nbb):
                for c in range(r):
                    vblocks.append((bb * r + c, HALF + 128 * r * bb + c))
            for c in range(r):
                vblocks.append((16 + c, 128 * r * (nbb - 1) + c))
            for vi, (idx, st0) in enumerate(vblocks):
                bk = next_pbank()
                tsl = slice(st0, st0 + 127 * r + 1, r)
                T(tm_mm(bk, s_v, tsl, 128), reads=[('WB', s_v)], writes=[('ps', bk)])
                if vi % 2 == 0 or (g == 0 and ii > 0):
                    A(lambda e, bk=bk, idx=idx: e.activation(out=VA[:, idx, :], in_=bank(bk)[:, 0:128],
                                                             func=AF.Copy),
                      reads=[('ps', bk)], writes=[('VAa', idx)])
                else:
                    V(lambda e, bk=bk, idx=idx: e.tensor_copy(out=VA[:, idx, :], in_=bank(bk)[:, 0:128]),
                      reads=[('ps', bk)], writes=[('VAv', idx)])

        def block_ops(ii):
            hg, g = items[ii]
            w, r = ATT_PATTERNS[g]
            nbb = 16 // r
            par = ii % 2
            qT_, kT_ = qa[par], ka[par]
            ebi = g * 4 + hg
            ops = []
            for bb in range(nbb):
                for c in range(r):
                    def f(bb=bb, c=c):
                        bp = blkc[0] % 2
                        blkc[0] += 1
                        q0 = 128 * r * bb + c
                        qsl = slice(q0, q0 + 127 * r + 1, r)
                        o0 = HALF + q0
                        osl = slice(o0, o0 + 127 * r + 1, r)
                        if bb > 0:
                            p0 = o0 - 128 * r
                            pidx = (bb - 1) * r + c
                        else:
                            p0 = 128 * r * (nbb - 1) + c
                            pidx = 16 + c
                        psl = slice(p0, p0 + 127 * r + 1, r)
                        oidx = bb * r + c
                        bS = 4 + bp
                        bN = 6 + bp

                        def mmS(e):
                            e.matmul(bank(bS)[:, 0:128], lhsT=kT_[:, osl], rhs=qT_[:, qsl], start=True, stop=True)
                            return e.matmul(bank(bS)[:, 128:256], lhsT=kT_[:, psl], rhs=qT_[:, qsl],
                                            start=True, stop=True)
                        T(mmS, reads=[('ka', par), ('qa', par)], writes=[('ps', bS)])
                        A(lambda e: e.activation(out=Et[bp][:], in_=bank(bS)[:, 0:256], func=AF.Exp),
                          reads=[('ps', bS)], writes=[('Et', bp)])
                        EBt = EBm if bb == 0 else EB
                        V(lambda e: e.tensor_tensor(out=Pt[bp][:], in0=Et[bp][:], in1=EBt[:, ebi, :], op=ALU.mult),
                          reads=[('Et', bp)], writes=[('Pt', bp)])

                        def mmN(e):
                            e.matmul(bank(bN)[:, 0:128], lhsT=VA[:, oidx, :], rhs=Pt[bp][:, 0:128],
                                     start=True, stop=False)
                            e.matmul(bank(bN)[:, 0:128], lhsT=VA[:, pidx, :], rhs=Pt[bp][:, 128:256],
                                     start=False, stop=True)
                            e.matmul(bank(bN)[:, 128:256], lhsT=onesb[:], rhs=Pt[bp][:, 0:128],
                                     start=True, stop=False)
                            return e.matmul(bank(bN)[:, 128:256], lhsT=onesb[:], rhs=Pt[bp][:, 128:256],
                                            start=False, stop=True)
                        T(mmN, reads=[('Pt', bp), ('VAa', oidx), ('VAv', oidx), ('VAa', pidx), ('VAv', pidx)],
                          writes=[('ps', bN)])
                        src3 = bank(bN)[:, 0:256].rearrange("p (a q) -> p a q", a=2)
                        if g == 0:
                            V(lambda e: e.tensor_copy(out=ND[:, :, qsl], in_=src3),
                              reads=[('ps', bN)], writes=['ND'])
                        else:
                            V(lambda e: e.tensor_tensor(out=ND[:, :, qsl], in0=src3, in1=ND[:, :, qsl], op=ALU.add),
                              reads=[('ps', bN), 'ND'], writes=['ND'])
                    ops.append(f)
            return ops

        for f in qk_ops(0):
            f()
        v_proj(0)
        for ii in range(len(items)):
            hg, g = items[ii]
            blks = block_ops(ii)
            nxt = qk_ops(ii + 1) if ii + 1 < len(items) else []
            ni = 0
            for bi, f in enumerate(blks):
                f()
                want = (len(nxt) * (bi + 1)) // len(blks)
                while ni < want:
                    nxt[ni]()
                    ni += 1
            while ni < len(nxt):
                nxt[ni]()
                ni += 1
            if g == 2:
                V(lambda e: e.reciprocal(out=ND[:, 1, :], in_=ND[:, 1, :]), reads=['ND'], writes=['ND'])
                V(lambda e, hg=hg: e.tensor_tensor(out=AttT[:, hg, :], in0=ND[:, 0, :], in1=ND[:, 1, :], op=ALU.mult),
                  reads=['ND'], writes=['AttT'])
            if ii + 1 < len(items):
                v_proj(ii + 1)
        S.barrier()

    if dbg_cols:
        for kc in range(4):
            dma('gpsimd', dbg_d[:, 27648 + kc * HALF:27648 + (kc + 1) * HALF], AttT[:, kc, :], semname="outg")
    if stage == 3:
        S.emit(final_waits=[k for k in (('D', 'out'), ('D', 'outg')) if k in S.cnt])
        mix.close()
        root.close()
        return nc

    mrg = contextlib.ExitStack()
    mrg.__enter__()
    MergedT = sb(mrg, "MergedT", [128, 8, HALF], BF16, side="right")
    with contextlib.ExitStack() as msc:
        sgA = [sb(msc, "sgA%d" % i, [128, 512], F32) for i in range(2)]
        sgB = [sb(msc, "sgB%d" % i, [128, 512], F32) for i in range(2)]
        m1 = [sb(msc, "m1_%d" % i, [128, 512], F32) for i in range(2)]
        m2 = [sb(msc, "m2_%d" % i, [128, 512], F32) for i in range(2)]
        WBm = WB + [sb(msc, "WBm%d" % i, [128, 8, 256], BF16) for i in range(4)]
        mloads = loads[N_MAIN_LOADS:]
        assert len(mloads) == 32 and ring['issued'] == ring['consumed'] == N_MAIN_LOADS
        mr = {'issued': 0, 'consumed': 0}

        def mring_get(hold):
            while mr['issued'] < min(len(mloads), mr['consumed'] + 8 - hold + 1):
                n = mr['issued']
                s = n % 8
                src_, kind = mloads[n]
                dst = WBm[s][:, :, 0:128] if kind == 1 else WBm[s][:, 0:4, 0:128]
                dma('gpsimd', dst, src_, writes=[('WBm', s)], semname="WBm%d" % s)
                mr['issued'] += 1
            s = mr['consumed'] % 8
            mr['consumed'] += 1
            return s
        WB_save = WB

        def fm_mm_m(bk, s, col0, nk=8, src=None):
            src = HT if src is None else src

            def f(e):
                ins = None
                for kc in range(nk):
                    ins = e.matmul(bank(bk)[:, 0:512], lhsT=WBm[s][:, kc, 0:128],
                                   rhs=src[:, kc, col0:col0 + 512], start=(kc == 0), stop=(kc == nk - 1))
                return ins
            return f
        it = 0
        for m in range(8):
            s_ga = mring_get(1)
            s_gb = mring_get(2)
            s_wr = mring_get(3)
            s_wa = mring_get(4)
            for tg in range(4):
                p = it % 2
                it += 1
                bs = 0 if p == 0 else 4
                T(fm_mm_m(bs + 0, s_ga, HALF + tg * 512), reads=[('WBm', s_ga)], writes=[('ps', bs + 0)])
                T(fm_mm_m(bs + 1, s_gb, HALF + tg * 512), reads=[('WBm', s_gb)], writes=[('ps', bs + 1)])
                T(fm_mm_m(bs + 2, s_wr, tg * 512, src=RetT), reads=[('WBm', s_wr)], writes=[('ps', bs + 2)])
                T(fm_mm_m(bs + 3, s_wa, tg * 512, nk=4, src=AttT), reads=[('WBm', s_wa)], writes=[('ps', bs + 3)])
                A(lambda e, p=p, bs=bs: e.activation(out=sgA[p][:], in_=bank(bs)[:, 0:512], func=AF.Sigmoid),
                  reads=[('ps', bs)], writes=[('sgA', p)])
                A(lambda e, p=p, bs=bs: e.activation(out=sgB[p][:], in_=bank(bs + 1)[:, 0:512], func=AF.Sigmoid),
                  reads=[('ps', bs + 1)], writes=[('sgB', p)])
                V(lambda e, p=p, bs=bs: e.tensor_tensor(out=m1[p][:], in0=bank(bs + 2)[:, 0:512], in1=sgA[p][:],
                                                        op=ALU.mult),
                  reads=[('ps', bs + 2), ('sgA', p)], writes=[('m1', p)])
                V(lambda e, p=p, bs=bs: e.tensor_tensor(out=m2[p][:], in0=bank(bs + 3)[:, 0:512], in1=sgB[p][:],
                                                        op=ALU.mult),
                  reads=[('ps', bs + 3), ('sgB', p)], writes=[('m2', p)])
                G(lambda e, p=p, m=m, tg=tg: e.tensor_tensor(out=MergedT[:, m, tg * 512:(tg + 1) * 512],
                                                             in0=m1[p][:], in1=m2[p][:], op=ALU.add),
                  reads=[('m1', p), ('m2', p)], writes=[('MergedT', m, tg)])
        S.barrier()
    mix.close()

    X = sb(root, "X", [128, NT, D], F32)
    with contextlib.ExitStack() as p3:
        WO = sb(p3, "WO", [128, 8, D], BF16)
        wst = [sb(p3, "wst%d" % i, [128, D], F32) for i in range(4)]
        for kc in range(8):
            dma('sync', wst[kc % 4][:], wo_d[kc * 128:(kc + 1) * 128, :], writes=[('wst', kc % 4)],
                semname="wst%d" % (kc % 4))
            if kc % 2 == 0:
                G(lambda e, kc=kc: e.tensor_tensor(out=WO[:, kc, :], in0=wst[kc % 4][:], in1=gtA_bc[:], op=ALU.mult),
                  reads=[('wst', kc % 4)], writes=[('WOg', kc)])
            else:
                V(lambda e, kc=kc: e.tensor_tensor(out=WO[:, kc, :], in0=wst[kc % 4][:], in1=gtA_bc[:], op=ALU.mult),
                  reads=[('wst', kc % 4)], writes=[('WOv', kc)])
        for t in range(NT):
            dma('sync', X[:, t, :], x_own[t * 128:(t + 1) * 128, :], writes=[('X', t, 0), ('X', t, 1)])
        for t in range(NT):
            for nh in range(2):
                bk = next_pbank()

                def mmo(e, bk=bk, t=t, nh=nh):
                    ins = None
                    for kc in range(8):
                        ins = e.matmul(bank(bk)[:, 0:512], lhsT=MergedT[:, kc, t * 128:(t + 1) * 128],
                                       rhs=WO[:, kc, nh * 512:(nh + 1) * 512], start=(kc == 0), stop=(kc == 7))
                    return ins
                T(mmo, reads=[('WOg', 0), ('WOv', 1), ('WOg', 2), ('WOv', 3), ('WOg', 4), ('WOv', 5), ('WOg', 6), ('WOv', 7)],
                  writes=[('ps', bk)])
                V(lambda e, bk=bk, t=t, nh=nh: e.tensor_tensor(out=X[:, t, nh * 512:(nh + 1) * 512],
                                                               in0=bank(bk)[:, 0:512],
                                                               in1=X[:, t, nh * 512:(nh + 1) * 512], op=ALU.add),
                  reads=[('ps', bk), ('X', t, nh)], writes=[('X', t, nh)])
        S.barrier()

    mrg.close()

    if dbg_cols:
        for t in range(NT):
            dma('sync', dbg_d[:, 35840 + t * D:35840 + (t + 1) * D], X[:, t, :], semname="out")
    if stage == 4:
        S.emit(final_waits=[k for k in (('D', 'out'), ('D', 'outg')) if k in S.cnt])
        root.close()
        return nc

    H2T = sb(root, "H2T", [128, 8, HALF], BF16)
    Gt = sb(root, "Gt", [128, NT, N_EXP], F32)
    with contextlib.ExitStack() as p4:
        xnf = [sb(p4, "xnf%d" % i, [128, D], F32) for i in range(3)]
        h2f = [sb(p4, "h2f%d" % i, [128, 8, 128], F32) for i in range(3)]
        junk4 = sb(p4, "junk4", [128, D], BF16)
        WR = sb(p4, "WR", [128, 8, N_EXP], F32)
        brbc = sb(p4, "brbc", [128, N_EXP], F32)
        Lg = [sb(p4, "Lg%d" % i, [128, N_EXP], F32) for i in range(2)]
        m8 = [sb(p4, "m8_%d" % i, [128, 8], F32) for i in range(2)]
        ngm = [sb(p4, "ngm%d" % i, [128, 1], F32) for i in range(2)]
        msk = [sb(p4, "msk%d" % i, [128, N_EXP], F32) for i in range(2)]
        Ex = [sb(p4, "Ex%d" % i, [128, N_EXP], F32) for i in range(2)]
        sm = [sb(p4, "sm%d" % i, [128, 1], F32) for i in range(2)]
        dma('sync', WR[:], wr_d.rearrange("(kc p) n -> p kc n", p=128), writes=['WR'])
        dma('sync', brbc[:], brbc_d, writes=['brbc'])
        NP4 = 3

        def s4_sq(t):
            i = 32 + t
            A(lambda e: e.activation(out=junk4[:], in_=X[:, t, :], func=AF.Square, accum_out=ss[:, i:i + 1]),
              reads=[('X', t, 0), ('X', t, 1)], writes=['junk4', ('ss', i)])

        def s4_ts(t):
            i = 32 + t
            V(lambda e: e.tensor_scalar(out=rs[:, i:i + 1], in0=ss[:, i:i + 1], scalar1=1.0 / D, scalar2=EPS,
                                        op0=ALU.mult, op1=ALU.add),
              reads=[('ss', i)], writes=[('rs', i)])

        def s4_sqrt(t):
            i = 32 + t
            A(lambda e: e.activation(out=rs[:, i:i + 1], in_=rs[:, i:i + 1], func=AF.Sqrt),
              reads=[('rs', i)], writes=[('rs', i)])

        def s4_xn(t):
            i = 32 + t
            p = t % NP4
            V(lambda e: e.reciprocal(out=rs[:, i:i + 1], in_=rs[:, i:i + 1]),
              reads=[('rs', i)], writes=[('rs', i)])
            V(lambda e: e.tensor_scalar(out=xnf[p][:], in0=X[:, t, :], scalar1=rs[:, i:i + 1],
                                        scalar2=None, op0=ALU.mult),
              reads=[('X', t, 0), ('X', t, 1), ('rs', i)], writes=[('xnf', p)])

        def s4_b0(t):
            return (0, 2, 6)[t % NP4]

        def s4_tr(t):
            p = t % NP4
            b0 = s4_b0(t)

            def tr2(e):
                ins = None
                for kc in range(8):
                    ins = e.transpose(out=PS[:, b0 + kc // 4, (kc % 4) * 128:(kc % 4 + 1) * 128],
                                      in_=xnf[p][:, kc * 128:(kc + 1) * 128], identity=identf[:])
                return ins
            T(tr2, reads=[('xnf', p)], writes=[('ps', b0), ('ps', b0 + 1)])

        def s4_ev(t):
            p = t % NP4
            b0 = s4_b0(t)
            for kc in range(8):
                srcp = PS[:, b0 + kc // 4, (kc % 4) * 128:(kc % 4 + 1) * 128]
                if t % 2 == 0:
                    A(lambda e, srcp=srcp, kc=kc: e.activation(out=h2f[p][:, kc, :], in_=srcp, func=AF.Identity,
                                                               bias=shF[:, kc:kc + 1], scale=scF[:, kc:kc + 1]),
                      reads=[('ps', b0), ('ps', b0 + 1)], writes=[('h2fa', p)])
                else:
                    V(lambda e, srcp=srcp, kc=kc: e.tensor_scalar(out=h2f[p][:, kc, :], in0=srcp,
                                                                  scalar1=scF[:, kc:kc + 1],
                                                                  scalar2=shF[:, kc:kc + 1],
                                                                  op0=ALU.mult, op1=ALU.add),
                      reads=[('ps', b0), ('ps', b0 + 1)], writes=[('h2fv', p)])

        def s4_rt(t):
            p = t % NP4
            G(lambda e: e.tensor_copy(out=H2T[:, :, t * 128:(t + 1) * 128], in_=h2f[p][:]),
              reads=[('h2fa', p), ('h2fv', p)], writes=[('H2T', t)])
            bl = 4 + t % 2

            def mmr(e):
                ins = None
                for kc in range(8):
                    ins = e.matmul(bank(bl)[:, 0:N_EXP], lhsT=h2f[p][:, kc, :], rhs=WR[:, kc, :],
                                   start=(kc == 0), stop=(kc == 7))
                return ins
            T(mmr, reads=[('h2fa', p), ('h2fv', p), 'WR'], writes=[('ps', bl)])

        def s4_gate(t):
            q = t % 2
            bl = 4 + t % 2
            V(lambda e: e.tensor_tensor(out=Lg[q][:], in0=bank(bl)[:, 0:N_EXP], in1=brbc[:], op=ALU.add),
              reads=[('ps', bl), 'brbc'], writes=[('Lg', q)])
            V(lambda e: e.max(out=m8[q][:], in_=Lg[q][:]), reads=[('Lg', q)], writes=[('m8', q)])
            V(lambda e: e.tensor_scalar(out=ngm[q][:], in0=m8[q][:, 0:1], scalar1=-1.0, scalar2=None, op0=ALU.mult),
              reads=[('m8', q)], writes=[('ngm', q)])
            V(lambda e: e.tensor_scalar(out=msk[q][:], in0=Lg[q][:], scalar1=m8[q][:, 3:4], scalar2=None,
                                        op0=ALU.is_ge),
              reads=[('Lg', q), ('m8', q)], writes=[('msk', q)])
            A(lambda e: e.activation(out=Ex[q][:], in_=Lg[q][:], func=AF.Exp, bias=ngm[q][:, 0:1], scale=1.0),
              reads=[('Lg', q), ('ngm', q)], writes=[('Ex', q)])

        def s4_gate2(t):
            q = t % 2
            V(lambda e: e.tensor_tensor(out=Ex[q][:], in0=Ex[q][:], in1=msk[q][:], op=ALU.mult),
              reads=[('Ex', q), ('msk', q)], writes=[('Ex', q)])
            V(lambda e: e.tensor_reduce(out=sm[q][:], in_=Ex[q][:], axis=AX.X, op=ALU.add),
              reads=[('Ex', q)], writes=[('sm', q)])
            V(lambda e: e.reciprocal(out=sm[q][:], in_=sm[q][:]), reads=[('sm', q)], writes=[('sm', q)])
            V(lambda e: e.tensor_scalar(out=Gt[:, t, :], in0=Ex[q][:], scalar1=sm[q][:, 0:1], scalar2=None,
                                        op0=ALU.mult),
              reads=[('Ex', q), ('sm', q)], writes=[('Gt', t)])
        stages4 = [(s4_sq, 0), (s4_ts, 1), (s4_sqrt, 2), (s4_xn, 3), (s4_tr, 4), (s4_ev, 5), (s4_rt, 6),
                   (s4_gate, 7), (s4_gate2, 8)]
        for j in range(NT + 8):
            for fn, lag in stages4:
                t = j - lag
                if 0 <= t < NT:
                    fn(t)
        S.barrier()

    if dbg_cols:
        dma('sync', dbg_d[:, 52224:52224 + NT * N_EXP], Gt[:].rearrange("p t e -> p (t e)"), semname="out")
        dma('gpsimd', dbg_d[:, 52736:52736 + HALF], H2T[:, 0, :], semname="outg")
    if stage == 5:
        S.emit(final_waits=[k for k in (('D', 'out'), ('D', 'outg')) if k in S.cnt])
        root.close()
        return nc

    with contextlib.ExitStack() as p5:
        aT = sb(p5, "aT", [128, 8, HALF], BF16)
        W2 = sb(p5, "W2", [128, 8, D], BF16)
        W1 = [sb(p5, "W1_%d" % i, [128, 8, 256], BF16) for i in range(3)]
        w2st = [sb(p5, "w2st%d" % i, [128, D], F32) for i in range(2)]
        b1 = sb(p5, "b1", [128, N_EXP, 8, 2], F32)
        tA = [sb(p5, "tA%d" % i, [128, 512], F32) for i in range(2)]
        tB = [sb(p5, "tB%d" % i, [128, 512], F32) for i in range(2)]
        tC = [sb(p5, "tC%d" % i, [128, 512], F32) for i in range(2)]
        tD = [sb(p5, "tD%d" % i, [128, 512], F32) for i in range(2)]
        dma('sync', b1[:], b1r_d, writes=['b1'])
        n_exp_run = N_EXP
        w1n = [0]
        total_w1 = n_exp_run * 8

        def w1_issue():
            n = w1n[0]
            e_, fc_ = n // 8, n % 8
            s = n % 3
            dma('gpsimd', W1[s][:], w1r_d[e_, fc_], writes=[('W1', s)], semname="W1_%d" % s)
            w1n[0] += 1
        w1c = [0]

        def w1_get():
            while w1n[0] < min(total_w1, w1c[0] + 3):
                w1_issue()
            s = w1c[0] % 3
            w1c[0] += 1
            return s
        ac = [0]
        sc_ = [0]
        for ex in range(n_exp_run):
            def w2_dma(fc2, ex=ex):
                q = (ex * 8 + fc2) % 2
                dma('sync', w2st[q][:], w2_d[ex, fc2 * 128:(fc2 + 1) * 128, :], writes=[('w2st', q)],
                    semname="w2st%d" % q)

            def w2_chunk(fc2, ex=ex):
                q = (ex * 8 + fc2) % 2
                if fc2 + 1 < 8:
                    w2_dma(fc2 + 1)
                V(lambda e: e.tensor_tensor(out=W2[:, fc2, :], in0=w2st[q][:], in1=gtF_bc[:], op=ALU.mult),
                  reads=[('w2st', q)], writes=['W2'])
            w2_dma(0)
            w2_chunk(0)
            for fc in range(8):
                s = w1_get()
                for tg in range(4):
                    p = ac[0] % 2
                    ac[0] += 1
                    bg, bl_ = (0, 1) if p == 0 else (2, 3)

                    def mm1(e, s=s, tg=tg, bg=bg, bl_=bl_):
                        ins = None
                        for kc in range(8):
                            e.matmul(bank(bg)[:, 0:512], lhsT=W1[s][:, kc, 0:128],
                                     rhs=H2T[:, kc, tg * 512:(tg + 1) * 512], start=(kc == 0), stop=(kc == 7))
                        for kc in range(8):
                            ins = e.matmul(bank(bl_)[:, 0:512], lhsT=W1[s][:, kc, 128:256],
                                           rhs=H2T[:, kc, tg * 512:(tg + 1) * 512], start=(kc == 0), stop=(kc == 7))
                        return ins
                    T(mm1, reads=[('W1', s)], writes=[('ps', bg), ('ps', bl_)])
                    V(lambda e, p=p, bg=bg, ex=ex, fc=fc: e.tensor_scalar(
                        out=tA[p][:], in0=bank(bg)[:, 0:512], scalar1=b1[:, ex, fc, 0:1], scalar2=7.0,
                        op0=ALU.add, op1=ALU.min),
                      reads=[('ps', bg), 'b1'], writes=[('tA', p)])
                    A(lambda e, p=p: e.activation(out=tB[p][:], in_=tA[p][:], func=AF.Sigmoid, scale=1.702),
                      reads=[('tA', p)], writes=[('tB', p)])
                    V(lambda e, p=p, bl_=bl_, ex=ex, fc=fc: e.tensor_scalar(
                        out=tC[p][:], in0=bank(bl_)[:, 0:512], scalar1=b1[:, ex, fc, 1:2], scalar2=7.0,
                        op0=ALU.add, op1=ALU.min),
                      reads=[('ps', bl_), 'b1'], writes=[('tC', p)])
                    V(lambda e, p=p: e.tensor_scalar(out=tC[p][:], in0=tC[p][:], scalar1=-7.0, scalar2=1.0,
                                                     op0=ALU.max, op1=ALU.add),
                      reads=[('tC', p)], writes=[('tC', p)])
                    G(lambda e, p=p: e.tensor_tensor(out=tD[p][:], in0=tA[p][:], in1=tB[p][:], op=ALU.mult),
                      reads=[('tA', p), ('tB', p)], writes=[('tD', p)])
                    G(lambda e, p=p, fc=fc, tg=tg: e.tensor_tensor(out=aT[:, fc, tg * 512:(tg + 1) * 512],
                                                                   in0=tD[p][:], in1=tC[p][:], op=ALU.mult),
                      reads=[('tD', p), ('tC', p)], writes=[('aT', tg)])
                if fc + 1 < 8:
                    w2_chunk(fc + 1)
            for t in range(NT):
                for nh in range(2):
                    bk = 4 + sc_[0] % 2
                    sc_[0] += 1

                    def mm2(e, bk=bk, t=t, nh=nh):
                        ins = None
                        for fc in range(8):
                            ins = e.matmul(bank(bk)[:, 0:512], lhsT=aT[:, fc, t * 128:(t + 1) * 128],
                                           rhs=W2[:, fc, nh * 512:(nh + 1) * 512], start=(fc == 0), stop=(fc == 7))
                        return ins
                    T(mm2, reads=[('aT', t // 4), 'W2'], writes=[('ps', bk)])
                    V(lambda e, bk=bk, t=t, nh=nh, ex=ex: e.scalar_tensor_tensor(
                        out=X[:, t, nh * 512:(nh + 1) * 512], in0=bank(bk)[:, 0:512], scalar=Gt[:, t, ex:ex + 1],
                        in1=X[:, t, nh * 512:(nh + 1) * 512], op0=ALU.mult, op1=ALU.add),
                      reads=[('ps', bk), ('X', t, nh)], writes=[('X', t, nh)])
        S.barrier()

    with contextlib.ExitStack() as p5b:
        B2s = sb(p5b, "B2s", [N_EXP, D], F32)
        GT = sb(p5b, "GT", [N_EXP, NT, 128], F32)
        dma('sync', B2s[:], b2_d, writes=['B2s'])
        V(lambda e: e.tensor_tensor(out=B2s[:], in0=B2s[:], in1=gtF_bc[0:N_EXP, :], op=ALU.mult),
          reads=['B2s'], writes=['B2s'])
        for t in range(NT):
            bk = 6 + t % 2
            T(lambda e, t=t, bk=bk: e.transpose(out=bank(bk)[0:N_EXP, 0:128], in_=Gt[:, t, :], identity=identf[:]),
              writes=[('ps', bk)])
            V(lambda e, t=t, bk=bk: e.tensor_copy(out=GT[:, t, :], in_=bank(bk)[0:N_EXP, 0:128]),
              reads=[('ps', bk)], writes=[('GT', t)])
        for t in range(NT):
            for nh in range(2):
                bk = next_pbank()
                T(lambda e, t=t, nh=nh, bk=bk: e.matmul(bank(bk)[:, 0:512], lhsT=GT[:, t, :],
                                                        rhs=B2s[:, nh * 512:(nh + 1) * 512], start=True, stop=True),
                  reads=[('GT', t), 'B2s'], writes=[('ps', bk)])
                V(lambda e, t=t, nh=nh, bk=bk: e.tensor_tensor(out=X[:, t, nh * 512:(nh + 1) * 512],
                                                               in0=bank(bk)[:, 0:512],
                                                               in1=X[:, t, nh * 512:(nh + 1) * 512], op=ALU.add),
                  reads=[('ps', bk), ('X', t, nh)], writes=[('X', t, nh)])
        S.barrier()

    with contextlib.ExitStack() as p6:
        gfin = sb(p6, "gfin", [128, D], F32)
        ot = [sb(p6, "ot%d" % i, [128, D], F32) for i in range(3)]
        junk6 = sb(p6, "junk6", [128, D], BF16)
        dma('sync', gfin[:], gfin_d, writes=['gfin'])
        NO6 = 3

        def s6_sq(t):
            i = 48 + t
            A(lambda e: e.activation(out=junk6[:], in_=X[:, t, :], func=AF.Square, accum_out=ss[:, i:i + 1]),
              reads=[('X', t, 0), ('X', t, 1)], writes=['junk6', ('ss', i)])

        def s6_ts(t):
            i = 48 + t
            V(lambda e: e.tensor_scalar(out=rs[:, i:i + 1], in0=ss[:, i:i + 1], scalar1=1.0 / D, scalar2=EPS,
                                        op0=ALU.mult, op1=ALU.add),
              reads=[('ss', i)], writes=[('rs', i)])

        def s6_sqrt(t):
            i = 48 + t
            A(lambda e: e.activation(out=rs[:, i:i + 1], in_=rs[:, i:i + 1], func=AF.Sqrt),
              reads=[('rs', i)], writes=[('rs', i)])

        def s6_out(t):
            i = 48 + t
            p = t % NO6
            V(lambda e: e.reciprocal(out=rs[:, i:i + 1], in_=rs[:, i:i + 1]),
              reads=[('rs', i)], writes=[('rs', i)])
            V(lambda e: e.scalar_tensor_tensor(out=ot[p][:], in0=X[:, t, :], scalar=rs[:, i:i + 1],
                                               in1=gfin[:], op0=ALU.mult, op1=ALU.mult),
              reads=[('X', t, 0), ('X', t, 1), ('rs', i), 'gfin'], writes=[('ot', p)])
            dma('sync', out_d[t * 128:(t + 1) * 128, :], ot[p][:], reads=[('ot', p)], semname="out%d" % p)
        stages6 = [(s6_sq, 0), (s6_ts, 1), (s6_sqrt, 2), (s6_out, 3)]
        for j in range(NT + 3):
            for fn, lag in stages6:
                t = j - lag
                if 0 <= t < NT:
                    fn(t)
    S.emit(final_waits=[k for k in (('D', 'out'), ('D', 'outg'), ('D', 'out0'), ('D', 'out1'), ('D', 'out2')) if k in S.cnt])
    root.close()
    return nc


def _t5_bucket(dist):
    dist = np.asarray(dist, dtype=np.int64)
    small = dist < 16
    ratio = np.log(np.maximum(dist, 1).astype(np.float32) / np.float32(16)) / np.float32(math.log(2048 / 16))
    large = 16 + (ratio.astype(np.float32) * np.float32(16)).astype(np.int32)
    large = np.minimum(large, 31)
    return np.where(small, dist, large)


def _constants():
    cst = {}
    half = 64
    inv = (10000.0 ** (-np.arange(half, dtype=np.float32) / half)).astype(np.float32)
    pos = np.arange(SEQ, dtype=np.float32)
    ang = pos[None, :] * inv[:, None]
    cos = np.cos(ang).astype(np.float32)
    sin = np.sin(ang).astype(np.float32)
    cst['cos_full'] = np.concatenate([cos, cos], axis=0)
    cst['sin_full'] = np.concatenate([-sin, sin], axis=0)
    Hh = 4
    log_g = np.log1p(-np.exp2(-5.0 - np.arange(Hh, dtype=np.float64)))
    i = np.arange(128, dtype=np.float64)
    scale = 128 ** -0.5
    dm = np.zeros((128, 4, 128), np.float32)
    for h in range(Hh):
        diff = i[None, :] - i[:, None]
        dm[:, h, :] = np.where(diff >= 0, np.exp(np.maximum(diff, 0) * log_g[h]) * scale, 0.0)
    rvec = np.zeros((128, 16), np.float32)
    for h in range(Hh):
        rvec[:, h] = np.exp((127.0 - i) * log_g[h]) * scale
        rvec[:, 4 + h] = np.exp((i + 1.0) * log_g[h])
        rvec[:, 8 + h] = np.exp(128.0 * log_g[h])
    cst['dmask'] = dm
    cst['rvec'] = rvec
    z2 = np.zeros((128, 4, 16), np.float32)
    for h in range(Hh):
        for n in range(16):
            z2[:, h, n] = np.exp((127.0 - i) * log_g[h] + 128.0 * (15 - n) * log_g[h]) * scale
    cst['zeta2'] = z2
    ohf = np.zeros((32, 3, 384), np.float32)
    for g, (w, r) in enumerate(ATT_PATTERNS):
        j = np.arange(129)
        bk = _t5_bucket(r * j)
        for jj in range(129):
            ohf[bk[jj], g, 127 + jj] = 1.0
    cst['ohf'] = ohf
    cst['identf'] = np.eye(128, dtype=np.float32)
    cst['jf'] = np.ascontiguousarray(np.eye(128, dtype=np.float32)[::-1])
    return cst


def _prep_shared(inp):
    sh = {}
    w_in = inp['w_in'][0]

    def chunk(cols):
        return w_in[:, cols].reshape(8, 128, len(cols)).transpose(1, 0, 2)

    def rng(a, n=128):
        return np.arange(a, a + n)
    r1 = []
    r2 = []
    for h in range(4):
        q0 = C_RQ + h * 128
        k0 = C_RK + h * 128
        r1.append(chunk(rng(q0)))
        r1.append(chunk(np.concatenate([rng(q0 + 64, 64), rng(q0, 64)])))
        r1.append(chunk(rng(k0)))
        r1.append(chunk(np.concatenate([rng(k0 + 64, 64), rng(k0, 64)])))
        r2.append(chunk(rng(C_RV + h * 256, 256)))
        r2.append(chunk(rng(C_RG + h * 256, 256)))
    for hg in range(4):
        for g in range(3):
            hd = g * 4 + hg
            r1.append(chunk(rng(C_AQ + hd * 128)))
            r1.append(chunk(rng(C_AK + hd * 128)))
            r1.append(chunk(rng(C_AV + hd * 128)))
    for m in range(8):
        r1.append(chunk(rng(C_GA + m * 128)))
    for m in range(8):
        r1.append(chunk(rng(C_GB + m * 128)))
    sh['w_in_r1'] = np.ascontiguousarray(np.stack(r1, axis=0))
    sh['w_in_r2'] = np.ascontiguousarray(np.stack(r2, axis=0))
    b_ada = inp['b_ada'][0]
    secs = [0, 1, 3, 4]
    bc = np.zeros((128, 32), np.float32)
    for si, s in enumerate(secs):
        bc[:, si * 8:(si + 1) * 8] = b_ada[s * 1024:(s + 1) * 1024].reshape(8, 128).T
    sh['bada_col'] = bc
    bb = np.stack([b_ada[2048:3072], b_ada[5120:6144]], axis=0)
    sh['bada_bc'] = np.ascontiguousarray(np.broadcast_to(bb[None], (128, 2, 1024)))
    sh['gmix_col'] = np.ascontiguousarray(inp['g_mix'][0].reshape(8, 128).T)
    sh['gffn_col'] = np.ascontiguousarray(inp['g_ffn'][0].reshape(8, 128).T)
    sh['gfin_bc'] = np.ascontiguousarray(np.broadcast_to(inp['g_final'][None, :], (128, 1024)))
    sh['w_ada'] = np.ascontiguousarray(inp['w_ada'][0])
    sh['rel_bias'] = np.ascontiguousarray(inp['rel_bias'])
    sh['w_ret_o'] = np.ascontiguousarray(inp['w_ret_o'][0])
    sh['w_att_o'] = np.ascontiguousarray(inp['w_att_o'][0])
    sh['w_o'] = np.ascontiguousarray(inp['w_o'][0])
    sh['w_router'] = np.ascontiguousarray(inp['w_router'][0])
    sh['brouter_bc'] = np.ascontiguousarray(np.broadcast_to(inp['b_router'][0][None, :], (128, 32)))
    w1 = inp['w_mlp1'][0].reshape(32, 8, 128, 8, 128, 2)
    sh['w1r'] = np.ascontiguousarray(w1.transpose(0, 3, 2, 1, 5, 4)).reshape(32, 8, 128, 8, 256)
    sh['b1r'] = np.ascontiguousarray(inp['b_mlp1'][0].reshape(32, 8, 128, 2).transpose(2, 0, 1, 3))
    sh['w_mlp2'] = np.ascontiguousarray(inp['w_mlp2'][0])
    sh['b_mlp2'] = np.ascontiguousarray(inp['b_mlp2'][0])
    return sh


def make_in_maps(inp):
    inp = {k: np.asarray(v, dtype=np.float32) for k, v in inp.items()}
    cst = _constants()
    sh = _prep_shared(inp)
    x = inp['x']
    c = inp['c']
    zeros_prev = np.zeros((HALF, D), np.float32)
    in_maps = []
    for core in range(8):
        b, hf = core // 2, core % 2
        m = dict(sh)
        m['x_own'] = np.ascontiguousarray(x[b, hf * HALF:(hf + 1) * HALF])
        m['x_prev'] = np.ascontiguousarray(x[b, 0:HALF]) if hf == 1 else zeros_prev
        m['flag'] = np.full((128, 1), float(hf), np.float32)
        m['c_col'] = np.ascontiguousarray(c[b].reshape(8, 128).T)
        if hf == 1:
            m['rope_cos'] = cst['cos_full']
            m['rope_sin'] = cst['sin_full']
        else:
            m['rope_cos'] = np.ascontiguousarray(np.concatenate([cst['cos_full'][:, :HALF]] * 2, axis=1))
            m['rope_sin'] = np.ascontiguousarray(np.concatenate([cst['sin_full'][:, :HALF]] * 2, axis=1))
        for k in ('dmask', 'rvec', 'zeta2', 'ohf', 'identf', 'jf'):
            m[k] = cst[k]
        in_maps.append(m)
    return in_maps


def kernel(**inputs):
    in_maps = make_in_maps(inputs)
    nc = build_program()
    res = run_bass_kernel_spmd(nc, in_maps, core_ids=list(range(8)))
    out = np.zeros((4, SEQ, D), np.float32)
    for core in range(8):
        b, hf = core // 2, core % 2
        out[b, hf * HALF:(hf + 1) * HALF] = res.results[core]["out"]
    return out
```

```python
import contextlib
import math
import numpy as np
import concourse.bass as bass
import concourse.mybir as mybir
from concourse.bass_utils import run_bass_kernel_spmd

F32 = mybir.dt.float32
BF16 = mybir.dt.bfloat16
AF = mybir.ActivationFunctionType
ALU = mybir.AluOpType
AX = mybir.AxisListType

D = 1024
SEQ = 4096
HALF = 2048
NT = 16
EPS = 1e-6
GN_EPS = 1e-5
N_EXP = 32
ATT_PATTERNS = ((128, 1), (512, 4), (2048, 16))
NSLOT = 4

C_RQ, C_RK, C_RV, C_RG = 0, 512, 1024, 2048
C_AQ, C_AK, C_AV = 3072, 4608, 6144
C_GA, C_GB = 7680, 8704


class Sched:
    ENG = ['sync', 'scalar', 'vector', 'gpsimd', 'tensor']

    def __init__(self, nc):
        self.nc = nc
        self.ops = {e: [] for e in self.ENG}
        self.cnt = {}
        self.waited = {e: {} for e in self.ENG}
        self.lastw = {}
        self.readers = {}

    def op(self, eng, fn, reads=(), writes=(), sem=None, inc=1):
        deps = {}
        own = ('E', eng)

        def add(d, raw):
            if d is None:
                return
            k, v = d
            if k == own and not raw:
                return
            if deps.get(k, 0) < v:
                deps[k] = v
        for b in reads:
            add(self.lastw.get(b), True)
        waw_own = eng in ('vector', 'gpsimd', 'scalar')
        for b in writes:
            add(self.lastw.get(b), waw_own)
            for r in self.readers.get(b, ()):
                add(r, False)
        waits = []
        for k, v in deps.items():
            if self.waited[eng].get(k, 0) < v:
                self.waited[eng][k] = v
                waits.append((k, v))
        semkey = sem if sem is not None else own
        self.cnt[semkey] = self.cnt.get(semkey, 0) + inc
        val = self.cnt[semkey]
        self.ops[eng].append((waits, fn, semkey, inc))
        for b in reads:
            self.readers.setdefault(b, []).append((semkey, val))
        for b in writes:
            self.lastw[b] = (semkey, val)
            self.readers[b] = []
        return val

    def barrier(self):
        for e in self.ENG:
            waits = []
            for k, v in self.cnt.items():
                if self.waited[e].get(k, 0) < v:
                    self.waited[e][k] = v
                    waits.append((k, v))
            if waits:
                self.ops[e].append((waits, None, None, 0))
        self.lastw = {}
        self.readers = {}

    def emit(self, final_waits=()):
        nc = self.nc
        keys = list(self.cnt.keys())
        with contextlib.ExitStack() as st:
            sems = {}
            for i, k in enumerate(keys):
                sems[k] = st.enter_context(nc.semaphore("s%d" % i))
            block = st.enter_context(nc.Block())

            def body(engname):
                def f(e):
                    for waits, fn, semkey, inc in self.ops[engname]:
                        for k, v in waits:
                            e.wait_ge(sems[k], v)
                        if fn is None:
                            continue
                        ins = fn(e)
                        ins.then_inc(sems[semkey], inc)
                    if engname == 'sync':
                        for k in final_waits:
                            e.wait_ge(sems[k], self.cnt[k])
                return f
            block.sync(body('sync'))
            block.scalar(body('scalar'))
            block.vector(body('vector'))
            block.gpsimd(body('gpsimd'))
            block.tensor(body('tensor'))


def build_program(stage=99, dbg_cols=0):
    nc = bass.Bass("TRN2", target_bir_lowering=False)
    S = Sched(nc)

    def din(name, shape, dt=F32):
        return nc.dram_tensor(name, list(shape), dt, kind="ExternalInput").ap()

    x_own = din("x_own", [HALF, D])
    x_prev = din("x_prev", [HALF, D])
    flag_d = din("flag", [128, 1])
    ccol_d = din("c_col", [128, 8])
    w_ada_d = din("w_ada", [D, 6 * D])
    badacol_d = din("bada_col", [128, 32])
    badabc_d = din("bada_bc", [128, 2, D])
    gmix_d = din("gmix_col", [128, 8])
    gffn_d = din("gffn_col", [128, 8])
    gfin_d = din("gfin_bc", [128, D])
    win1_d = din("w_in_r1", [68, 128, 8, 128])
    win2_d = din("w_in_r2", [8, 128, 8, 256])
    ropec_d = din("rope_cos", [128, SEQ])
    ropes_d = din("rope_sin", [128, SEQ])
    dm_d = din("dmask", [128, 4, 128])
    rvec_d = din("rvec", [128, 16])
    zeta2_d = din("zeta2", [128, 4, 16])
    relb_d = din("rel_bias", [32, 12])
    ohf_d = din("ohf", [32, 3, 384])
    identf_d = din("identf", [128, 128])
    jf_d = din("jf", [128, 128])
    wreto_d = din("w_ret_o", [D, D])
    watto_d = din("w_att_o", [512, D])
    wo_d = din("w_o", [D, D])
    wr_d = din("w_router", [D, N_EXP])
    brbc_d = din("brouter_bc", [128, N_EXP])
    if stage > 5:
        w1r_d = din("w1r", [N_EXP, 8, 128, 8, 256])
        b1r_d = din("b1r", [128, N_EXP, 8, 2])
        w2_d = din("w_mlp2", [N_EXP, D, D])
        b2_d = din("b_mlp2", [N_EXP, D])
    out_d = nc.dram_tensor("out", [HALF, D], F32, kind="ExternalOutput").ap()
    fd_t = nc.dram_tensor("fd_scratch", [12, 384], F32)
    fd_d = fd_t.ap()
    dbg_d = None
    if dbg_cols:
        dbg_d = nc.dram_tensor("dbg", [128, dbg_cols], F32, kind="ExternalOutput").ap()

    root = contextlib.ExitStack()
    root.__enter__()

    def sb(st, name, shape, dt, side=None):
        name = "sb_" + name
        if side is None:
            return st.enter_context(nc.sbuf_tensor(name, list(shape), dt))
        return st.enter_context(nc.sbuf_tensor(name, list(shape), dt, side=side))

    PS = root.enter_context(nc.psum_tensor("psall", [128, 8, 512], F32))

    def bank(i):
        return PS[:, i, :]

    def bank_bf(i):
        return PS[:, i, :].bitcast(BF16)

    dma_n = [0]

    def dma(eng, out, in_, reads=(), writes=(), semname=None):
        if semname is None:
            semname = "u%d" % dma_n[0]
            dma_n[0] += 1
        return S.op(eng, lambda e: e.dma_start(out=out, in_=in_), reads=reads, writes=writes,
                    sem=('D', semname), inc=16)

    def V(fn, reads=(), writes=()):
        return S.op('vector', fn, reads, writes)

    def A(fn, reads=(), writes=()):
        return S.op('scalar', fn, reads, writes)

    def G(fn, reads=(), writes=()):
        return S.op('gpsimd', fn, reads, writes)

    def T(fn, reads=(), writes=()):
        return S.op('tensor', fn, reads, writes)

    flag = sb(root, "flag", [128, 1], F32)
    identb = sb(root, "identb", [128, 128], BF16)
    identf = sb(root, "identf", [128, 128], F32)
    onesb = sb(root, "onesb", [128, 128], BF16)
    onesf = sb(root, "onesf", [128, 128], F32)
    modcol = sb(root, "modcol", [128, 32], F32)
    scA = sb(root, "scA", [128, 8], F32)
    scAp = sb(root, "scAp", [128, 8], F32)
    shAp = sb(root, "shAp", [128, 8], F32)
    scF = sb(root, "scF", [128, 8], F32)
    gtA_bc = sb(root, "gtA_bc", [128, D], F32)
    gtF_bc = sb(root, "gtF_bc", [128, D], F32)
    ss = sb(root, "ss", [128, 64], F32)
    rs = sb(root, "rs", [128, 64], F32)
    shA = modcol[:, 0:8]
    shF = modcol[:, 16:24]

    dma('sync', flag[:], flag_d, writes=['flag'])
    dma('sync', identf[:], identf_d, writes=['identf'])
    dma('gpsimd', identb[:], identf_d, writes=['identb'])
    V(lambda e: e.memset(onesb[:], 1.0), writes=['onesb'])
    V(lambda e: e.memset(onesf[:], 1.0), writes=['onesf'])

    with contextlib.ExitStack() as p0:
        ccol = sb(p0, "ccol", [128, 8], F32)
        cs2 = sb(p0, "cs2", [128, 8, 2], F32)
        csb = sb(p0, "csb", [128, 8, 128], F32)
        badacol = sb(p0, "badacol", [128, 32], F32)
        badabc = sb(p0, "badabc", [128, 2, D], F32)
        gmix = sb(p0, "gmix", [128, 8], F32)
        gffn = sb(p0, "gffn", [128, 8], F32)
        wast = [sb(p0, "wast%d" % i, [128, 8, 512], F32) for i in range(3)]
        dma('sync', ccol[:], ccol_d, writes=['ccol'])
        dma('sync', badacol[:], badacol_d, writes=['badacol'])
        dma('sync', badabc[:], badabc_d, writes=['badabc'])
        dma('sync', gmix[:], gmix_d, writes=['gmix'])
        dma('sync', gffn[:], gffn_d, writes=['gffn'])
        for j in range(2):
            A(lambda e, j=j: e.activation(out=cs2[:, :, j], in_=ccol[:], func=AF.Silu),
              reads=['ccol'], writes=[('cs2', j)])
        for kc in range(8):
            V(lambda e, kc=kc: e.tensor_scalar(out=csb[:, kc, :], in0=onesf[:], scalar1=cs2[:, kc, 0:1],
                                               scalar2=None, op0=ALU.mult),
              reads=['onesf', ('cs2', 0)], writes=['csb'])
        w_ada_v = w_ada_d.rearrange("(kc p) n -> p kc n", p=128)
        colsec = {0: 0, 1: 1, 3: 2, 4: 3}
        for j in range(12):
            sec, half = j // 2, j % 2
            wt = wast[j % 3]
            dma('sync', wt[:], w_ada_v[:, :, j * 512:(j + 1) * 512], writes=[('wast', j % 3)],
                semname="wast%d" % (j % 3))
            if sec in colsec:
                si = colsec[sec]

                def mmcol(e, wt=wt, si=si, half=half):
                    ins = None
                    for cc in range(4):
                        c = half * 4 + cc
                        o = (si * 8 + c) * 2
                        for kc in range(8):
                            ins = e.matmul(bank(0)[:, o:o + 2], lhsT=wt[:, kc, cc * 128:(cc + 1) * 128],
                                           rhs=cs2[:, kc, :], start=(kc == 0), stop=(kc == 7))
                    return ins
                T(mmcol, reads=[('wast', j % 3), ('cs2', 0), ('cs2', 1)], writes=[('ps', 0)])
            else:
                bk = 1 + half
                which = 0 if sec == 2 else 1
                dst = gtA_bc if sec == 2 else gtF_bc

                def mmrow(e, wt=wt, bk=bk):
                    ins = None
                    for kc in range(8):
                        ins = e.matmul(bank(bk)[:, 0:512], lhsT=csb[:, kc, :], rhs=wt[:, kc, :],
                                       start=(kc == 0), stop=(kc == 7))
                    return ins
                T(mmrow, reads=[('wast', j % 3), 'csb'], writes=[('ps', bk)])
                V(lambda e, bk=bk, dst=dst, which=which, half=half: e.tensor_tensor(
                    out=dst[:, half * 512:(half + 1) * 512], in0=bank(bk)[:, 0:512],
                    in1=badabc[:, which, half * 512:(half + 1) * 512], op=ALU.add),
                  reads=[('ps', bk), 'badabc'], writes=[('gt', sec, half)])
        V(lambda e: e.tensor_tensor(out=modcol[:], in0=bank(0)[:, 0:64:2], in1=badacol[:], op=ALU.add),
          reads=[('ps', 0), 'badacol'], writes=['modcol'])
        V(lambda e: e.scalar_tensor_tensor(out=scA[:], in0=modcol[:, 8:16], scalar=1.0, in1=gmix[:],
                                           op0=ALU.add, op1=ALU.mult),
          reads=['modcol', 'gmix'], writes=['scA'])
        V(lambda e: e.scalar_tensor_tensor(out=scF[:], in0=modcol[:, 24:32], scalar=1.0, in1=gffn[:],
                                           op0=ALU.add, op1=ALU.mult),
          reads=['modcol', 'gffn'], writes=['scF'])
        V(lambda e: e.tensor_scalar(out=scAp[:], in0=scA[:], scalar1=flag[:, 0:1], scalar2=None, op0=ALU.mult),
          reads=['scA', 'flag'], writes=['scAp'])
        V(lambda e: e.tensor_scalar(out=shAp[:], in0=modcol[:, 0:8], scalar1=flag[:, 0:1], scalar2=None,
                                    op0=ALU.mult),
          reads=['modcol', 'flag'], writes=['shAp'])
        S.barrier()

    if stage == 0:
        dma('sync', dbg_d[:, 0:32], modcol[:], semname="out")
        dma('sync', dbg_d[:, 32:40], scA[:], semname="out")
        dma('sync', dbg_d[:, 40:48], scAp[:], semname="out")
        dma('sync', dbg_d[:, 48:56], scF[:], semname="out")
        dma('sync', dbg_d[:, 64:64 + 1024], gtA_bc[:], semname="out")
        dma('sync', dbg_d[:, 1088:1088 + 1024], gtF_bc[:], semname="out")
        S.emit(final_waits=[k for k in (('D', 'out'), ('D', 'outg')) if k in S.cnt])
        root.close()
        return nc

    mix = contextlib.ExitStack()
    mix.__enter__()
    HT = sb(mix, "HT", [128, 8, SEQ], BF16)
    RetT = sb(mix, "RetT", [128, 8, HALF], BF16)
    WB = [sb(mix, "WB%d" % i, [128, 8, 256], BF16) for i in range(NSLOT)]

    ropesc = contextlib.ExitStack()
    ropesc.__enter__()
    ropeC = sb(ropesc, "ropeC", [128, SEQ], BF16, side="right")
    ropeS = sb(ropesc, "ropeS", [128, SEQ], BF16, side="right")
    dma('gpsimd', ropeC[:], ropec_d, writes=['ropeC'])
    dma('gpsimd', ropeS[:], ropes_d, writes=['ropeS'])

    with contextlib.ExitStack() as p1:
        NXI, NXN = 6, 3
        xin = [sb(p1, "xin%d" % i, [128, D], F32) for i in range(NXI)]
        xn = [sb(p1, "xn%d" % i, [128, D], BF16) for i in range(NXN)]
        junk = sb(p1, "junk", [128, D], BF16)

        def st_dma(i):
            src = x_prev if i < 16 else x_own
            t = i % 16
            dma('sync', xin[i % NXI][:], src[t * 128:(t + 1) * 128, :], writes=[('xin', i % NXI)],
                semname="xin%d" % (i % NXI))

        def st_sq(i):
            A(lambda e: e.activation(out=junk[:], in_=xin[i % NXI][:], func=AF.Square, accum_out=ss[:, i:i + 1]),
              reads=[('xin', i % NXI)], writes=['junk', ('ss', i)])

        def st_ts(i):
            V(lambda e: e.tensor_scalar(out=rs[:, i:i + 1], in0=ss[:, i:i + 1], scalar1=1.0 / D, scalar2=EPS,
                                        op0=ALU.mult, op1=ALU.add),
              reads=[('ss', i)], writes=[('rs', i)])

        def st_sqrt(i):
            A(lambda e: e.activation(out=rs[:, i:i + 1], in_=rs[:, i:i + 1], func=AF.Sqrt),
              reads=[('rs', i)], writes=[('rs', i)])

        def st_xn(i):
            V(lambda e: e.reciprocal(out=rs[:, i:i + 1], in_=rs[:, i:i + 1]),
              reads=[('rs', i)], writes=[('rs', i)])
            V(lambda e: e.tensor_scalar(out=xn[i % NXN][:], in0=xin[i % NXI][:], scalar1=rs[:, i:i + 1],
                                        scalar2=None, op0=ALU.mult),
              reads=[('xin', i % NXI), ('rs', i)], writes=[('xn', i % NXN)])

        def st_tr(i):
            pT = bank_bf(i % 4)

            def tr(e):
                ins = None
                for kc in range(8):
                    ins = e.transpose(out=pT[:, kc * 128:(kc + 1) * 128],
                                      in_=xn[i % NXN][:, kc * 128:(kc + 1) * 128], identity=identb[:])
                return ins
            T(tr, reads=[('xn', i % NXN)], writes=[('ps', i % 4)])

        def st_ev(i):
            pT = bank_bf(i % 4)
            sc_, sh_ = (scAp, shAp) if i < 16 else (scA, shA)
            for kc in range(8):
                dst = HT[:, kc, i * 128:(i + 1) * 128]
                srcp = pT[:, kc * 128:(kc + 1) * 128]
                if i % 2 == 0:
                    A(lambda e, dst=dst, srcp=srcp, kc=kc: e.activation(
                        out=dst, in_=srcp, func=AF.Identity, bias=sh_[:, kc:kc + 1], scale=sc_[:, kc:kc + 1]),
                      reads=[('ps', i % 4)], writes=[('HTa', i)])
                else:
                    V(lambda e, dst=dst, srcp=srcp, kc=kc: e.tensor_scalar(
                        out=dst, in0=srcp, scalar1=sc_[:, kc:kc + 1], scalar2=sh_[:, kc:kc + 1],
                        op0=ALU.mult, op1=ALU.add),
                      reads=[('ps', i % 4)], writes=[('HTv', i)])
        st_dma(0)
        st_dma(1)
        stages = [(st_sq, 0), (st_ts, 1), (st_sqrt, 2), (st_xn, 3), (st_tr, 4), (st_ev, 5)]
        for j in range(32 + 5):
            if j + 2 < 32:
                st_dma(j + 2)
            for fn, lag in stages:
                i = j - lag
                if 0 <= i < 32:
                    fn(i)
        S.barrier()

    if dbg_cols:
        dma('gpsimd', dbg_d[:, 0:4096], HT[:, 0, :], semname="outg")
        dma('gpsimd', dbg_d[:, 4096:8192], HT[:, 5, :], semname="outg")
    if stage == 1:
        S.emit(final_waits=[k for k in (('D', 'out'), ('D', 'outg')) if k in S.cnt])
        mix.close()
        root.close()
        return nc

    loads = []
    for h in range(4):
        for k in range(4):
            loads.append((win1_d[h * 4 + k], 1))
        loads.append((win2_d[h * 2 + 0], 2))
        loads.append((win2_d[h * 2 + 1], 2))
    for hg in range(4):
        for g in range(3):
            for k in range(3):
                loads.append((win1_d[16 + (hg * 3 + g) * 3 + k], 1))
    wreto_v = wreto_d.rearrange("(kc p) n -> p kc n", p=128)
    watto_v = watto_d.rearrange("(kc p) n -> p kc n", p=128)
    for m in range(8):
        loads.append((win1_d[52 + m], 1))
        loads.append((win1_d[60 + m], 1))
        loads.append((wreto_v[:, :, m * 128:(m + 1) * 128], 1))
        loads.append((watto_v[:, :, m * 128:(m + 1) * 128], 3))
    ring = {'issued': 0, 'consumed': 0}
    N_MAIN_LOADS = 60

    def ring_issue():
        n = ring['issued']
        s = n % NSLOT
        src, kind = loads[n]
        if kind == 1:
            dst = WB[s][:, :, 0:128]
        elif kind == 2:
            dst = WB[s][:, :, :]
        else:
            dst = WB[s][:, 0:4, 0:128]
        dma('gpsimd', dst, src, writes=[('WB', s)], semname="WB%d" % s)
        ring['issued'] += 1

    def ring_get(hold=1):
        while ring['issued'] < min(N_MAIN_LOADS, ring['consumed'] + NSLOT - hold + 1):
            ring_issue()
        s = ring['consumed'] % NSLOT
        ring['consumed'] += 1
        return s

    pbank = [0]

    def next_pbank():
        b = pbank[0]
        pbank[0] = (pbank[0] + 1) % 4
        return b

    def fm_mm(bk, s, col0, ncols=512, nk=8, src=None, wcols=(0, 128)):
        src = HT if src is None else src

        def f(e):
            ins = None
            for kc in range(nk):
                ins = e.matmul(bank(bk)[:, 0:ncols], lhsT=WB[s][:, kc, wcols[0]:wcols[1]],
                               rhs=src[:, kc, col0:col0 + ncols], start=(kc == 0), stop=(kc == nk - 1))
            return ins
        return f

    def tm_mm(bk, s, tok_sl, ncols):
        def f(e):
            ins = None
            for kc in range(8):
                ins = e.matmul(bank(bk)[:, 0:ncols], lhsT=HT[:, kc, tok_sl], rhs=WB[s][:, kc, 0:ncols],
                               start=(kc == 0), stop=(kc == 7))
            return ins
        return f

    with contextlib.ExitStack() as rsc:
        Dm = sb(rsc, "Dm", [128, 4, 128], F32)
        rvec = sb(rsc, "rvec", [128, 16], F32)
        zeta2 = sb(rsc, "zeta2", [128, 4, 16], F32)
        qT = sb(rsc, "qT", [128, HALF], BF16)
        kT = sb(rsc, "kT", [128, SEQ], BF16)
        Vt = sb(rsc, "Vt", [128, 32, 256], BF16)
        SG = sb(rsc, "SG", [128, 16, 256], BF16)
        t1 = [sb(rsc, "t1_%d" % i, [128, 512], F32) for i in range(2)]
        t2 = [sb(rsc, "t2_%d" % i, [128, 512], F32) for i in range(2)]
        Kz = [sb(rsc, "Kz%d" % i, [128, 128], BF16) for i in range(2)]
        PT = [sb(rsc, "PT%d" % i, [128, 128], BF16) for i in range(2)]
        yI = [sb(rsc, "yI%d" % i, [128, 256], F32) for i in range(2)]
        yy = [sb(rsc, "yy%d" % i, [128, 256], F32) for i in range(2)]
        yn = [sb(rsc, "yn%d" % i, [128, 256], BF16) for i in range(2)]
        yg = [sb(rsc, "yg%d" % i, [128, 256], BF16) for i in range(2)]
        st6 = [sb(rsc, "st6_%d" % i, [128, 6], F32) for i in range(2)]
        mv = [sb(rsc, "mv%d" % i, [128, 2], F32) for i in range(2)]
        rstd = [sb(rsc, "rstd%d" % i, [128, 1], F32) for i in range(2)]
        nmr = [sb(rsc, "nmr%d" % i, [128, 1], F32) for i in range(2)]
        Rf = sb(rsc, "Rf", [128, 256], F32)
        Rb = sb(rsc, "Rb", [128, 256], BF16)
        dma('sync', Dm[:], dm_d, writes=['Dm'])
        dma('sync', rvec[:], rvec_d, writes=['rvec'])
        dma('sync', zeta2[:], zeta2_d, writes=['zeta2'])
        ecnt = [0]

        for h in range(4):
            sq, sqs, sk, sks = None, None, None, None
            for which in ('q', 'k'):
                sa = ring_get(1)
                sbw = ring_get(2)
                ntg = 4 if which == 'q' else 8
                for tg in range(ntg):
                    col0 = (HALF + tg * 512) if which == 'q' else tg * 512
                    bA, bB = next_pbank(), next_pbank()
                    T(fm_mm(bA, sa, col0), reads=[('WB', sa)], writes=[('ps', bA)])
                    T(fm_mm(bB, sbw, col0), reads=[('WB', sbw)], writes=[('ps', bB)])
                    par = ecnt[0] % 2
                    ecnt[0] += 1
                    V(lambda e, bA=bA, col0=col0, par=par: e.tensor_tensor(
                        out=t1[par][:], in0=bank(bA)[:, 0:512], in1=ropeC[:, col0:col0 + 512], op=ALU.mult),
                      reads=[('ps', bA), 'ropeC'], writes=[('t1', par)])
                    V(lambda e, bB=bB, col0=col0, par=par: e.tensor_tensor(
                        out=t2[par][:], in0=bank(bB)[:, 0:512], in1=ropeS[:, col0:col0 + 512], op=ALU.mult),
                      reads=[('ps', bB), 'ropeS'], writes=[('t2', par)])
                    if which == 'q':
                        dst = qT[:, tg * 512:(tg + 1) * 512]
                        key = ('qT', tg)
                    else:
                        dst = kT[:, tg * 512:(tg + 1) * 512]
                        key = ('kT', tg)
                    G(lambda e, dst=dst, par=par: e.tensor_tensor(out=dst, in0=t1[par][:], in1=t2[par][:], op=ALU.add),
                      reads=[('t1', par), ('t2', par)], writes=[key])
            sv = ring_get()
            for t in range(32):
                bk = next_pbank()
                T(tm_mm(bk, sv, slice(t * 128, (t + 1) * 128), 256), reads=[('WB', sv)], writes=[('ps', bk)])
                A(lambda e, bk=bk, t=t: e.activation(out=Vt[:, t, :], in_=bank(bk)[:, 0:256], func=AF.Copy),
                  reads=[('ps', bk)], writes=[('V', t)])
            sg_ = ring_get()
            for t in range(16):
                bk = next_pbank()
                T(tm_mm(bk, sg_, slice(HALF + t * 128, HALF + (t + 1) * 128), 256), reads=[('WB', sg_)],
                  writes=[('ps', bk)])
                A(lambda e, bk=bk, t=t: e.activation(out=SG[:, t, :], in_=bank(bk)[:, 0:256], func=AF.Silu),
                  reads=[('ps', bk)], writes=[('SG', t)])
            for n in range(16):
                p = n % 2
                ksl = slice(n * 128, (n + 1) * 128)
                T(lambda e, ksl=ksl: e.transpose(out=bank_bf(4)[:, 0:128], in_=kT[:, ksl], identity=identb[:]),
                  reads=[('kT', n // 4)], writes=[('ps', 4)])
                V(lambda e, p=p, h=h, n=n: e.tensor_scalar(out=Kz[p][:], in0=bank_bf(4)[:, 0:128],
                                                           scalar1=zeta2[:, h, n:n + 1], scalar2=None, op0=ALU.mult),
                  reads=[('ps', 4), 'zeta2'], writes=[('Kz', p)])
                T(lambda e, p=p, n=n: e.matmul(bank(2)[:, 0:256], lhsT=Kz[p][:], rhs=Vt[:, n, :],
                                               start=(n == 0), stop=(n == 15)),
                  reads=[('Kz', p), ('V', n)], writes=[('ps', 2)])
            V(lambda e: e.tensor_copy(out=Rb[:], in_=bank(2)[:, 0:256]), reads=[('ps', 2)], writes=['Rb'])
            V(lambda e: e.tensor_copy(out=Rf[:], in_=bank(2)[:, 0:256]), reads=[('ps', 2)], writes=['Rf'])
            for n in range(16, 32):
                own = True
                p = n % 2
                ksl = slice(n * 128, (n + 1) * 128)
                T(lambda e, ksl=ksl: e.transpose(out=bank_bf(4)[:, 0:128], in_=kT[:, ksl], identity=identb[:]),
                  reads=[('kT', n // 4)], writes=[('ps', 4)])
                V(lambda e, p=p, h=h: e.tensor_scalar(out=Kz[p][:], in0=bank_bf(4)[:, 0:128],
                                                      scalar1=rvec[:, h:h + 1], scalar2=None, op0=ALU.mult),
                  reads=[('ps', 4), 'rvec'], writes=[('Kz', p)])
                if own:
                    j = n - 16
                    qsl = slice(j * 128, (j + 1) * 128)
                    T(lambda e, ksl=ksl, qsl=qsl: e.matmul(bank(5)[:, 0:128], lhsT=kT[:, ksl], rhs=qT[:, qsl],
                                                           start=True, stop=True),
                      reads=[('kT', n // 4), ('qT', j // 4)], writes=[('ps', 5)])
                    V(lambda e, p=p, h=h: e.tensor_tensor(out=PT[p][:], in0=bank(5)[:, 0:128], in1=Dm[:, h, :],
                                                          op=ALU.mult),
                      reads=[('ps', 5), 'Dm'], writes=[('PT', p)])
                    bI = 6 + p
                    T(lambda e, p=p, n=n, bI=bI: e.matmul(bank(bI)[:, 0:256], lhsT=PT[p][:], rhs=Vt[:, n, :],
                                                          start=True, stop=True),
                      reads=[('PT', p), ('V', n)], writes=[('ps', bI)])
                    bC = 0 + p
                    T(lambda e, qsl=qsl, bC=bC: e.matmul(bank(bC)[:, 0:256], lhsT=qT[:, qsl], rhs=Rb[:],
                                                         start=True, stop=True),
                      reads=[('qT', j // 4), 'Rb'], writes=[('ps', bC)])
                    A(lambda e, p=p, bI=bI: e.activation(out=yI[p][:], in_=bank(bI)[:, 0:256], func=AF.Copy),
                      reads=[('ps', bI)], writes=[('yI', p)])
                    V(lambda e, p=p, bC=bC, h=h: e.scalar_tensor_tensor(
                        out=yy[p][:], in0=bank(bC)[:, 0:256], scalar=rvec[:, 4 + h:5 + h], in1=yI[p][:],
                        op0=ALU.mult, op1=ALU.add),
                      reads=[('ps', bC), ('yI', p), 'rvec'], writes=[('yy', p)])
                    V(lambda e, p=p: e.bn_stats(out=st6[p][:], in_=yy[p][:]), reads=[('yy', p)], writes=[('st6', p)])
                    V(lambda e, p=p: e.bn_aggr(out=mv[p][:], in_=st6[p][:]), reads=[('st6', p)], writes=[('mv', p)])
                    V(lambda e, p=p: e.tensor_scalar(out=rstd[p][:], in0=mv[p][:, 1:2], scalar1=GN_EPS, scalar2=None,
                                                     op0=ALU.add),
                      reads=[('mv', p)], writes=[('rstd', p)])
                    A(lambda e, p=p: e.activation(out=rstd[p][:], in_=rstd[p][:], func=AF.Sqrt),
                      reads=[('rstd', p)], writes=[('rstd', p)])
                    V(lambda e, p=p: e.reciprocal(out=rstd[p][:], in_=rstd[p][:]),
                      reads=[('rstd', p)], writes=[('rstd', p)])
                    V(lambda e, p=p: e.scalar_tensor_tensor(out=nmr[p][:], in0=mv[p][:, 0:1], scalar=-1.0,
                                                            in1=rstd[p][:], op0=ALU.mult, op1=ALU.mult),
                      reads=[('mv', p), ('rstd', p)], writes=[('nmr', p)])
                    A(lambda e, p=p: e.activation(out=yn[p][:], in_=yy[p][:], func=AF.Identity,
                                                  bias=nmr[p][:, 0:1], scale=rstd[p][:, 0:1]),
                      reads=[('yy', p), ('rstd', p), ('nmr', p)], writes=[('yn', p)])
                    G(lambda e, p=p, j=j: e.tensor_tensor(out=yg[p][:], in0=yn[p][:], in1=SG[:, j, :], op=ALU.mult),
                      reads=[('yn', p), ('SG', j)], writes=[('yg', p)])

                    def trY(e, p=p):
                        ins = None
                        for dc in range(2):
                            ins = e.transpose(out=bank_bf(3)[:, dc * 128:(dc + 1) * 128],
                                              in_=yg[p][:, dc * 128:(dc + 1) * 128], identity=identb[:])
                        return ins
                    T(trY, reads=[('yg', p)], writes=[('ps', 3)])
                    for dc in range(2):
                        A(lambda e, h=h, dc=dc, qsl=qsl: e.activation(
                            out=RetT[:, h * 2 + dc, qsl], in_=bank_bf(3)[:, dc * 128:(dc + 1) * 128], func=AF.Copy),
                          reads=[('ps', 3)], writes=[('RetT', j)])
                if n < 31:
                    T(lambda e, p=p, n=n: e.matmul(bank(2)[:, 0:256], lhsT=Kz[p][:], rhs=Vt[:, n, :],
                                                   start=True, stop=True),
                      reads=[('Kz', p), ('V', n)], writes=[('ps', 2)])
                    V(lambda e, h=h: e.scalar_tensor_tensor(out=Rf[:], in0=Rf[:], scalar=rvec[:, 8 + h:9 + h],
                                                            in1=bank(2)[:, 0:256], op0=ALU.mult, op1=ALU.add),
                      reads=['Rf', ('ps', 2), 'rvec'], writes=['Rf'])
                    A(lambda e: e.activation(out=Rb[:], in_=Rf[:], func=AF.Copy), reads=['Rf'], writes=['Rb'])
        S.barrier()

    ropesc.close()

    if dbg_cols:
        for kc in range(8):
            dma('gpsimd', dbg_d[:, 8192 + kc * HALF:8192 + (kc + 1) * HALF], RetT[:, kc, :], semname="outg")
    if stage == 2:
        S.emit(final_waits=[k for k in (('D', 'out'), ('D', 'outg')) if k in S.cnt])
        mix.close()
        root.close()
        return nc

    AttT = sb(mix, "AttT", [128, 4, HALF], BF16)
    with contextlib.ExitStack() as asc:
        EB = sb(asc, "EB", [128, 12, 256], BF16)
        EBm = sb(asc, "EBm", [128, 12, 256], BF16)
        with contextlib.ExitStack() as ebs:
            relb = sb(ebs, "relb", [32, 12], F32)
            ohf = sb(ebs, "ohf", [32, 3, 384], F32)
            jf = sb(ebs, "jf", [128, 128], F32)
            Fsb = sb(ebs, "Fsb", [4, 3, 384], F32)
            Gh = [sb(ebs, "Gh%d" % i, [128, 256], F32) for i in range(2)]
            dma('sync', relb[:], relb_d, writes=['relb'])
            dma('sync', ohf[:], ohf_d, writes=['ohf'])
            dma('sync', jf[:], jf_d, writes=['jf'])
            A(lambda e: e.activation(out=relb[:], in_=relb[:], func=AF.Exp), reads=['relb'], writes=['relb'])
            for g in range(3):
                T(lambda e, g=g: e.matmul(bank(0)[0:4, 0:384], lhsT=relb[:, g * 4:(g + 1) * 4], rhs=ohf[:, g, :],
                                          start=True, stop=True),
                  reads=['relb', 'ohf'], writes=[('ps', 0)])
                V(lambda e, g=g: e.tensor_copy(out=Fsb[:, g, :], in_=bank(0)[0:4, 0:384]),
                  reads=[('ps', 0)], writes=['Fsb'])
            dma('sync', fd_d.rearrange("(g h) m -> h g m", g=3), Fsb[:], reads=['Fsb'], writes=['fd'])
            for idx in range(12):
                hk = bass.AP(tensor=fd_t, offset=idx * 384, ap=[[1, 128], [1, 256]])
                dma('sync', Gh[idx % 2][:], hk, reads=['fd'], writes=[('Gh', idx % 2)], semname="Gh%d" % (idx % 2))
                bk = 1 + idx % 2
                T(lambda e, idx=idx, bk=bk: e.matmul(bank(bk)[:, 0:256], lhsT=jf[:], rhs=Gh[idx % 2][:],
                                                     start=True, stop=True),
                  reads=['jf', ('Gh', idx % 2)], writes=[('ps', bk)])
                V(lambda e, idx=idx, bk=bk: e.tensor_copy(out=EB[:, idx, :], in_=bank(bk)[:, 0:256]),
                  reads=[('ps', bk)], writes=[('EBa', idx)])
                V(lambda e, idx=idx, bk=bk: e.tensor_copy(out=EBm[:, idx, 0:128], in_=bank(bk)[:, 0:128]),
                  reads=[('ps', bk)], writes=[('EBb', idx)])
                V(lambda e, idx=idx, bk=bk: e.tensor_scalar(out=EBm[:, idx, 128:256], in0=bank(bk)[:, 128:256],
                                                            scalar1=flag[:, 0:1], scalar2=None, op0=ALU.mult),
                  reads=[('ps', bk)], writes=[('EBv', idx)])
            S.barrier()

        if dbg_cols:
            for idx in range(12):
                dma('gpsimd', dbg_d[:, 24576 + idx * 256:24576 + (idx + 1) * 256], EB[:, idx, :], semname="outg")
        if stage == 25:
            S.emit(final_waits=[k for k in (('D', 'out'), ('D', 'outg')) if k in S.cnt])
            asc.close()
            mix.close()
            root.close()
            return nc

        qa = [sb(asc, "qa%d" % i, [128, HALF], BF16) for i in range(2)]
        ka = [sb(asc, "ka%d" % i, [128, SEQ], BF16) for i in range(2)]
        VA = sb(asc, "VA", [128, 32, 128], BF16)
        ND = sb(asc, "ND", [128, 2, HALF], F32)
        Et = [sb(asc, "Et%d" % i, [128, 256], F32) for i in range(2)]
        Pt = [sb(asc, "Pt%d" % i, [128, 256], BF16) for i in range(2)]
        blkc = [0]
        items = [(hg, g) for hg in range(4) for g in range(3)]

        def qk_ops(ii):
            hg, g = items[ii]
            par = ii % 2
            qT_, kT_ = qa[par], ka[par]
            ops = []
            st = {}

            def q_op(tg):
                def f():
                    if 'sq' not in st:
                        st['sq'] = ring_get(1)
                    s_q = st['sq']
                    bk = next_pbank()
                    T(fm_mm(bk, s_q, HALF + tg * 512), reads=[('WB', s_q)], writes=[('ps', bk)])
                    A(lambda e: e.activation(out=qT_[:, tg * 512:(tg + 1) * 512], in_=bank(bk)[:, 0:512],
                                             func=AF.Copy, scale=float(128 ** -0.5)),
                      reads=[('ps', bk)], writes=[('qa', par)])
                return f

            def k_op(tg):
                def f():
                    if 'sk' not in st:
                        st['sk'] = ring_get(1)
                    s_k = st['sk']
                    bk = next_pbank()
                    T(fm_mm(bk, s_k, tg * 512), reads=[('WB', s_k)], writes=[('ps', bk)])
                    V(lambda e: e.tensor_copy(out=kT_[:, tg * 512:(tg + 1) * 512], in_=bank(bk)[:, 0:512]),
                      reads=[('ps', bk)], writes=[('ka', par)])
                return f
            for tg in range(4):
                ops.append(q_op(tg))
            for tg in (list(range(8)) if g == 2 else list(range(3, 8))):
                ops.append(k_op(tg))
            return ops

        def v_proj(ii):
            hg, g = items[ii]
            w, r = ATT_PATTERNS[g]
            nbb = 16 // r
            s_v = ring_get(1)
            vblocks = []
            for bb in range(nbb):
                for c in range(r):
                    vblocks.append((bb * r + c, HALF + 128 * r * bb + c))
            for c in range(r):
                vblocks.append((16 + c, 128 * r * (nbb - 1) + c))
            for vi, (idx, st0) in enumerate(vblocks):
                bk = next_pbank()
                tsl = slice(st0, st0 + 127 * r + 1, r)
                T(tm_mm(bk, s_v, tsl, 128), reads=[('WB', s_v)], writes=[('ps', bk)])
                if True:
                    A(lambda e, bk=bk, idx=idx: e.activation(out=VA[:, idx, :], in_=bank(bk)[:, 0:128],
                                                             func=AF.Copy),
                      reads=[('ps', bk)], writes=[('VAa', idx)])
                else:
                    V(lambda e, bk=bk, idx=idx: e.tensor_copy(out=VA[:, idx, :], in_=bank(bk)[:, 0:128]),
                      reads=[('ps', bk)], writes=[('VAv', idx)])

        def block_ops(ii):
            hg, g = items[ii]
            w, r = ATT_PATTERNS[g]
            nbb = 16 // r
            par = ii % 2
            qT_, kT_ = qa[par], ka[par]
            ebi = g * 4 + hg
            ops = []
            for bb in range(nbb):
                for c in range(r):
                    def f(bb=bb, c=c):
                        bp = blkc[0] % 2
                        blkc[0] += 1
                        q0 = 128 * r * bb + c
                        qsl = slice(q0, q0 + 127 * r + 1, r)
                        o0 = HALF + q0
                        osl = slice(o0, o0 + 127 * r + 1, r)
                        if bb > 0:
                            p0 = o0 - 128 * r
                            pidx = (bb - 1) * r + c
                        else:
                            p0 = 128 * r * (nbb - 1) + c
                            pidx = 16 + c
                        psl = slice(p0, p0 + 127 * r + 1, r)
                        oidx = bb * r + c
                        bS = 4 + bp
                        bN = 6 + bp

                        def mmS(e):
                            e.matmul(bank(bS)[:, 0:128], lhsT=kT_[:, osl], rhs=qT_[:, qsl], start=True, stop=True)
                            return e.matmul(bank(bS)[:, 128:256], lhsT=kT_[:, psl], rhs=qT_[:, qsl],
                                            start=True, stop=True)
                        T(mmS, reads=[('ka', par), ('qa', par)], writes=[('ps', bS)])
                        A(lambda e: e.activation(out=Et[bp][:], in_=bank(bS)[:, 0:256], func=AF.Exp),
                          reads=[('ps', bS)], writes=[('Et', bp)])
                        EBt = EBm if bb == 0 else EB
                        V(lambda e: e.tensor_tensor(out=Pt[bp][:], in0=Et[bp][:], in1=EBt[:, ebi, :], op=ALU.mult),
                          reads=[('Et', bp)], writes=[('Pt', bp)])

                        def mmN(e):
                            e.matmul(bank(bN)[:, 0:128], lhsT=VA[:, oidx, :], rhs=Pt[bp][:, 0:128],
                                     start=True, stop=False)
                            e.matmul(bank(bN)[:, 0:128], lhsT=VA[:, pidx, :], rhs=Pt[bp][:, 128:256],
                                     start=False, stop=True)
                            e.matmul(bank(bN)[:, 128:256], lhsT=onesb[:], rhs=Pt[bp][:, 0:128],
                                     start=True, stop=False)
                            return e.matmul(bank(bN)[:, 128:256], lhsT=onesb[:], rhs=Pt[bp][:, 128:256],
                                            start=False, stop=True)
                        T(mmN, reads=[('Pt', bp), ('VAa', oidx), ('VAv', oidx), ('VAa', pidx), ('VAv', pidx)],
                          writes=[('ps', bN)])
                        src3 = bank(bN)[:, 0:256].rearrange("p (a q) -> p a q", a=2)
                        if g == 0:
                            V(lambda e: e.tensor_copy(out=ND[:, :, qsl], in_=src3),
                              reads=[('ps', bN)], writes=['ND'])
                        else:
                            V(lambda e: e.tensor_tensor(out=ND[:, :, qsl], in0=src3, in1=ND[:, :, qsl], op=ALU.add),
                              reads=[('ps', bN), 'ND'], writes=['ND'])
                    ops.append(f)
            return ops

        for f in qk_ops(0):
            f()
        v_proj(0)
        for ii in range(len(items)):
            hg, g = items[ii]
            blks = block_ops(ii)
            nxt = qk_ops(ii + 1) if ii + 1 < len(items) else []
            ni = 0
            for bi, f in enumerate(blks):
                f()
                want = (len(nxt) * (bi + 1)) // len(blks)
                while ni < want:
                    nxt[ni]()
                    ni += 1
            while ni < len(nxt):
                nxt[ni]()
                ni += 1
            if g == 2:
                V(lambda e: e.reciprocal(out=ND[:, 1, :], in_=ND[:, 1, :]), reads=['ND'], writes=['ND'])
                V(lambda e, hg=hg: e.tensor_tensor(out=AttT[:, hg, :], in0=ND[:, 0, :], in1=ND[:, 1, :], op=ALU.mult),
                  reads=['ND'], writes=['AttT'])
            if ii + 1 < len(items):
                v_proj(ii + 1)
        S.barrier()

    if dbg_cols:
        for kc in range(4):
            dma('gpsimd', dbg_d[:, 27648 + kc * HALF:27648 + (kc + 1) * HALF], AttT[:, kc, :], semname="outg")
    if stage == 3:
        S.emit(final_waits=[k for k in (('D', 'out'), ('D', 'outg')) if k in S.cnt])
        mix.close()
        root.close()
        return nc

    mrg = contextlib.ExitStack()
    mrg.__enter__()
    MergedT = sb(mrg, "MergedT", [128, 8, HALF], BF16, side="right")
    with contextlib.ExitStack() as msc:
        sgA = [sb(msc, "sgA%d" % i, [128, 512], F32) for i in range(2)]
        sgB = [sb(msc, "sgB%d" % i, [128, 512], F32) for i in range(2)]
        m1 = [sb(msc, "m1_%d" % i, [128, 512], F32) for i in range(2)]
        m2 = [sb(msc, "m2_%d" % i, [128, 512], F32) for i in range(2)]
        WBm = WB + [sb(msc, "WBm%d" % i, [128, 8, 256], BF16) for i in range(4)]
        mloads = loads[N_MAIN_LOADS:]
        assert len(mloads) == 32 and ring['issued'] == ring['consumed'] == N_MAIN_LOADS
        mr = {'issued': 0, 'consumed': 0}

        def mring_get(hold):
            while mr['issued'] < min(len(mloads), mr['consumed'] + 8 - hold + 1):
                n = mr['issued']
                s = n % 8
                src_, kind = mloads[n]
                dst = WBm[s][:, :, 0:128] if kind == 1 else WBm[s][:, 0:4, 0:128]
                dma('gpsimd', dst, src_, writes=[('WBm', s)], semname="WBm%d" % s)
                mr['issued'] += 1
            s = mr['consumed'] % 8
            mr['consumed'] += 1
            return s
        WB_save = WB

        def fm_mm_m(bk, s, col0, nk=8, src=None):
            src = HT if src is None else src

            def f(e):
                ins = None
                for kc in range(nk):
                    ins = e.matmul(bank(bk)[:, 0:512], lhsT=WBm[s][:, kc, 0:128],
                                   rhs=src[:, kc, col0:col0 + 512], start=(kc == 0), stop=(kc == nk - 1))
                return ins
            return f
        it = 0
        for m in range(8):
            s_ga = mring_get(1)
            s_gb = mring_get(2)
            s_wr = mring_get(3)
            s_wa = mring_get(4)
            for tg in range(4):
                p = it % 2
                it += 1
                bs = 0 if p == 0 else 4
                T(fm_mm_m(bs + 0, s_ga, HALF + tg * 512), reads=[('WBm', s_ga)], writes=[('ps', bs + 0)])
                T(fm_mm_m(bs + 1, s_gb, HALF + tg * 512), reads=[('WBm', s_gb)], writes=[('ps', bs + 1)])
                T(fm_mm_m(bs + 2, s_wr, tg * 512, src=RetT), reads=[('WBm', s_wr)], writes=[('ps', bs + 2)])
                T(fm_mm_m(bs + 3, s_wa, tg * 512, nk=4, src=AttT), reads=[('WBm', s_wa)], writes=[('ps', bs + 3)])
                A(lambda e, p=p, bs=bs: e.activation(out=sgA[p][:], in_=bank(bs)[:, 0:512], func=AF.Sigmoid),
                  reads=[('ps', bs)], writes=[('sgA', p)])
                A(lambda e, p=p, bs=bs: e.activation(out=sgB[p][:], in_=bank(bs + 1)[:, 0:512], func=AF.Sigmoid),
                  reads=[('ps', bs + 1)], writes=[('sgB', p)])
                V(lambda e, p=p, bs=bs: e.tensor_tensor(out=m1[p][:], in0=bank(bs + 2)[:, 0:512], in1=sgA[p][:],
                                                        op=ALU.mult),
                  reads=[('ps', bs + 2), ('sgA', p)], writes=[('m1', p)])
                V(lambda e, p=p, bs=bs: e.tensor_tensor(out=m2[p][:], in0=bank(bs + 3)[:, 0:512], in1=sgB[p][:],
                                                        op=ALU.mult),
                  reads=[('ps', bs + 3), ('sgB', p)], writes=[('m2', p)])
                G(lambda e, p=p, m=m, tg=tg: e.tensor_tensor(out=MergedT[:, m, tg * 512:(tg + 1) * 512],
                                                             in0=m1[p][:], in1=m2[p][:], op=ALU.add),
                  reads=[('m1', p), ('m2', p)], writes=[('MergedT', m, tg)])
        S.barrier()
    mix.close()

    X = sb(root, "X", [128, NT, D], F32)
    with contextlib.ExitStack() as p3:
        WO = sb(p3, "WO", [128, 8, D], BF16)
        wst = [sb(p3, "wst%d" % i, [128, D], F32) for i in range(4)]
        for kc in range(8):
            dma('sync', wst[kc % 4][:], wo_d[kc * 128:(kc + 1) * 128, :], writes=[('wst', kc % 4)],
                semname="wst%d" % (kc % 4))
            if kc % 2 == 0:
                G(lambda e, kc=kc: e.tensor_tensor(out=WO[:, kc, :], in0=wst[kc % 4][:], in1=gtA_bc[:], op=ALU.mult),
                  reads=[('wst', kc % 4)], writes=[('WOg', kc)])
            else:
                V(lambda e, kc=kc: e.tensor_tensor(out=WO[:, kc, :], in0=wst[kc % 4][:], in1=gtA_bc[:], op=ALU.mult),
                  reads=[('wst', kc % 4)], writes=[('WOv', kc)])
        for t in range(NT):
            dma('sync', X[:, t, :], x_own[t * 128:(t + 1) * 128, :], writes=[('X', t, 0), ('X', t, 1)])
        for t in range(NT):
            for nh in range(2):
                bk = next_pbank()

                def mmo(e, bk=bk, t=t, nh=nh):
                    ins = None
                    for kc in range(8):
                        ins = e.matmul(bank(bk)[:, 0:512], lhsT=MergedT[:, kc, t * 128:(t + 1) * 128],
                                       rhs=WO[:, kc, nh * 512:(nh + 1) * 512], start=(kc == 0), stop=(kc == 7))
                    return ins
                T(mmo, reads=[('WOg', 0), ('WOv', 1), ('WOg', 2), ('WOv', 3), ('WOg', 4), ('WOv', 5), ('WOg', 6), ('WOv', 7)],
                  writes=[('ps', bk)])
                V(lambda e, bk=bk, t=t, nh=nh: e.tensor_tensor(out=X[:, t, nh * 512:(nh + 1) * 512],
                                                               in0=bank(bk)[:, 0:512],
                                                               in1=X[:, t, nh * 512:(nh + 1) * 512], op=ALU.add),
                  reads=[('ps', bk), ('X', t, nh)], writes=[('X', t, nh)])
        S.barrier()

    mrg.close()

    if dbg_cols:
        for t in range(NT):
            dma('sync', dbg_d[:, 35840 + t * D:35840 + (t + 1) * D], X[:, t, :], semname="out")
    if stage == 4:
        S.emit(final_waits=[k for k in (('D', 'out'), ('D', 'outg')) if k in S.cnt])
        root.close()
        return nc

    H2T = sb(root, "H2T", [128, 8, HALF], BF16)
    Gt = sb(root, "Gt", [128, NT, N_EXP], F32)
    with contextlib.ExitStack() as p4:
        xnf = [sb(p4, "xnf%d" % i, [128, D], F32) for i in range(3)]
        h2f = [sb(p4, "h2f%d" % i, [128, 8, 128], F32) for i in range(3)]
        junk4 = sb(p4, "junk4", [128, D], BF16)
        WR = sb(p4, "WR", [128, 8, N_EXP], F32)
        brbc = sb(p4, "brbc", [128, N_EXP], F32)
        Lg = [sb(p4, "Lg%d" % i, [128, N_EXP], F32) for i in range(2)]
        m8 = [sb(p4, "m8_%d" % i, [128, 8], F32) for i in range(2)]
        ngm = [sb(p4, "ngm%d" % i, [128, 1], F32) for i in range(2)]
        msk = [sb(p4, "msk%d" % i, [128, N_EXP], F32) for i in range(2)]
        Ex = [sb(p4, "Ex%d" % i, [128, N_EXP], F32) for i in range(2)]
        sm = [sb(p4, "sm%d" % i, [128, 1], F32) for i in range(2)]
        dma('sync', WR[:], wr_d.rearrange("(kc p) n -> p kc n", p=128), writes=['WR'])
        dma('sync', brbc[:], brbc_d, writes=['brbc'])
        NP4 = 3

        def s4_sq(t):
            i = 32 + t
            A(lambda e: e.activation(out=junk4[:], in_=X[:, t, :], func=AF.Square, accum_out=ss[:, i:i + 1]),
              reads=[('X', t, 0), ('X', t, 1)], writes=['junk4', ('ss', i)])

        def s4_ts(t):
            i = 32 + t
            V(lambda e: e.tensor_scalar(out=rs[:, i:i + 1], in0=ss[:, i:i + 1], scalar1=1.0 / D, scalar2=EPS,
                                        op0=ALU.mult, op1=ALU.add),
              reads=[('ss', i)], writes=[('rs', i)])

        def s4_sqrt(t):
            i = 32 + t
            A(lambda e: e.activation(out=rs[:, i:i + 1], in_=rs[:, i:i + 1], func=AF.Sqrt),
              reads=[('rs', i)], writes=[('rs', i)])

        def s4_xn(t):
            i = 32 + t
            p = t % NP4
            V(lambda e: e.reciprocal(out=rs[:, i:i + 1], in_=rs[:, i:i + 1]),
              reads=[('rs', i)], writes=[('rs', i)])
            V(lambda e: e.tensor_scalar(out=xnf[p][:], in0=X[:, t, :], scalar1=rs[:, i:i + 1],
                                        scalar2=None, op0=ALU.mult),
              reads=[('X', t, 0), ('X', t, 1), ('rs', i)], writes=[('xnf', p)])

        def s4_b0(t):
            return (0, 2, 6)[t % NP4]

        def s4_tr(t):
            p = t % NP4
            b0 = s4_b0(t)

            def tr2(e):
                ins = None
                for kc in range(8):
                    ins = e.transpose(out=PS[:, b0 + kc // 4, (kc % 4) * 128:(kc % 4 + 1) * 128],
                                      in_=xnf[p][:, kc * 128:(kc + 1) * 128], identity=identf[:])
                return ins
            T(tr2, reads=[('xnf', p)], writes=[('ps', b0), ('ps', b0 + 1)])

        def s4_ev(t):
            p = t % NP4
            b0 = s4_b0(t)
            for kc in range(8):
                srcp = PS[:, b0 + kc // 4, (kc % 4) * 128:(kc % 4 + 1) * 128]
                if t % 2 == 0:
                    A(lambda e, srcp=srcp, kc=kc: e.activation(out=h2f[p][:, kc, :], in_=srcp, func=AF.Identity,
                                                               bias=shF[:, kc:kc + 1], scale=scF[:, kc:kc + 1]),
                      reads=[('ps', b0), ('ps', b0 + 1)], writes=[('h2fa', p)])
                else:
                    V(lambda e, srcp=srcp, kc=kc: e.tensor_scalar(out=h2f[p][:, kc, :], in0=srcp,
                                                                  scalar1=scF[:, kc:kc + 1],
                                                                  scalar2=shF[:, kc:kc + 1],
                                                                  op0=ALU.mult, op1=ALU.add),
                      reads=[('ps', b0), ('ps', b0 + 1)], writes=[('h2fv', p)])

        def s4_rt(t):
            p = t % NP4
            G(lambda e: e.tensor_copy(out=H2T[:, :, t * 128:(t + 1) * 128], in_=h2f[p][:]),
              reads=[('h2fa', p), ('h2fv', p)], writes=[('H2T', t)])
            bl = 4 + t % 2

            def mmr(e):
                ins = None
                for kc in range(8):
                    ins = e.matmul(bank(bl)[:, 0:N_EXP], lhsT=h2f[p][:, kc, :], rhs=WR[:, kc, :],
                                   start=(kc == 0), stop=(kc == 7))
                return ins
            T(mmr, reads=[('h2fa', p), ('h2fv', p), 'WR'], writes=[('ps', bl)])

        def s4_gate(t):
            q = t % 2
            bl = 4 + t % 2
            V(lambda e: e.tensor_tensor(out=Lg[q][:], in0=bank(bl)[:, 0:N_EXP], in1=brbc[:], op=ALU.add),
              reads=[('ps', bl), 'brbc'], writes=[('Lg', q)])
            V(lambda e: e.max(out=m8[q][:], in_=Lg[q][:]), reads=[('Lg', q)], writes=[('m8', q)])
            V(lambda e: e.tensor_scalar(out=ngm[q][:], in0=m8[q][:, 0:1], scalar1=-1.0, scalar2=None, op0=ALU.mult),
              reads=[('m8', q)], writes=[('ngm', q)])
            V(lambda e: e.tensor_scalar(out=msk[q][:], in0=Lg[q][:], scalar1=m8[q][:, 3:4], scalar2=None,
                                        op0=ALU.is_ge),
              reads=[('Lg', q), ('m8', q)], writes=[('msk', q)])
            A(lambda e: e.activation(out=Ex[q][:], in_=Lg[q][:], func=AF.Exp, bias=ngm[q][:, 0:1], scale=1.0),
              reads=[('Lg', q), ('ngm', q)], writes=[('Ex', q)])

        def s4_gate2(t):
            q = t % 2
            V(lambda e: e.tensor_tensor(out=Ex[q][:], in0=Ex[q][:], in1=msk[q][:], op=ALU.mult),
              reads=[('Ex', q), ('msk', q)], writes=[('Ex', q)])
            V(lambda e: e.tensor_reduce(out=sm[q][:], in_=Ex[q][:], axis=AX.X, op=ALU.add),
              reads=[('Ex', q)], writes=[('sm', q)])
            V(lambda e: e.reciprocal(out=sm[q][:], in_=sm[q][:]), reads=[('sm', q)], writes=[('sm', q)])
            V(lambda e: e.tensor_scalar(out=Gt[:, t, :], in0=Ex[q][:], scalar1=sm[q][:, 0:1], scalar2=None,
                                        op0=ALU.mult),
              reads=[('Ex', q), ('sm', q)], writes=[('Gt', t)])
        stages4 = [(s4_sq, 0), (s4_ts, 1), (s4_sqrt, 2), (s4_xn, 3), (s4_tr, 4), (s4_ev, 5), (s4_rt, 6),
                   (s4_gate, 7), (s4_gate2, 8)]
        for j in range(NT + 8):
            for fn, lag in stages4:
                t = j - lag
                if 0 <= t < NT:
                    fn(t)
        S.barrier()

    if dbg_cols:
        dma('sync', dbg_d[:, 52224:52224 + NT * N_EXP], Gt[:].rearrange("p t e -> p (t e)"), semname="out")
        dma('gpsimd', dbg_d[:, 52736:52736 + HALF], H2T[:, 0, :], semname="outg")
    if stage == 5:
        S.emit(final_waits=[k for k in (('D', 'out'), ('D', 'outg')) if k in S.cnt])
        root.close()
        return nc

    with contextlib.ExitStack() as p5:
        aT = sb(p5, "aT", [128, 8, HALF], BF16)
        W2 = sb(p5, "W2", [128, 8, D], BF16)
        W1 = [sb(p5, "W1_%d" % i, [128, 8, 256], BF16) for i in range(3)]
        w2st = [sb(p5, "w2st%d" % i, [128, D], F32) for i in range(2)]
        b1 = sb(p5, "b1", [128, N_EXP, 8, 2], F32)
        tA = [sb(p5, "tA%d" % i, [128, 512], F32) for i in range(2)]
        tB = [sb(p5, "tB%d" % i, [128, 512], F32) for i in range(2)]
        tC = [sb(p5, "tC%d" % i, [128, 512], F32) for i in range(2)]
        tD = [sb(p5, "tD%d" % i, [128, 512], F32) for i in range(2)]
        dma('sync', b1[:], b1r_d, writes=['b1'])
        n_exp_run = N_EXP
        w1n = [0]
        total_w1 = n_exp_run * 8

        def w1_issue():
            n = w1n[0]
            e_, fc_ = n // 8, n % 8
            s = n % 3
            dma('gpsimd', W1[s][:], w1r_d[e_, fc_], writes=[('W1', s)], semname="W1_%d" % s)
            w1n[0] += 1
        w1c = [0]

        def w1_get():
            while w1n[0] < min(total_w1, w1c[0] + 3):
                w1_issue()
            s = w1c[0] % 3
            w1c[0] += 1
            return s
        ac = [0]
        sc_ = [0]
        for ex in range(n_exp_run):
            def w2_dma(fc2, ex=ex):
                q = (ex * 8 + fc2) % 2
                dma('sync', w2st[q][:], w2_d[ex, fc2 * 128:(fc2 + 1) * 128, :], writes=[('w2st', q)],
                    semname="w2st%d" % q)

            def w2_chunk(fc2, ex=ex):
                q = (ex * 8 + fc2) % 2
                if fc2 + 1 < 8:
                    w2_dma(fc2 + 1)
                V(lambda e: e.tensor_tensor(out=W2[:, fc2, :], in0=w2st[q][:], in1=gtF_bc[:], op=ALU.mult),
                  reads=[('w2st', q)], writes=['W2'])
            w2_dma(0)
            w2_chunk(0)
            for fc in range(8):
                s = w1_get()
                for tg in range(4):
                    p = ac[0] % 2
                    ac[0] += 1
                    bg, bl_ = (0, 1) if p == 0 else (2, 3)

                    def mm1(e, s=s, tg=tg, bg=bg, bl_=bl_):
                        ins = None
                        for kc in range(8):
                            e.matmul(bank(bg)[:, 0:512], lhsT=W1[s][:, kc, 0:128],
                                     rhs=H2T[:, kc, tg * 512:(tg + 1) * 512], start=(kc == 0), stop=(kc == 7))
                        for kc in range(8):
                            ins = e.matmul(bank(bl_)[:, 0:512], lhsT=W1[s][:, kc, 128:256],
                                           rhs=H2T[:, kc, tg * 512:(tg + 1) * 512], start=(kc == 0), stop=(kc == 7))
                        return ins
                    T(mm1, reads=[('W1', s)], writes=[('ps', bg), ('ps', bl_)])
                    V(lambda e, p=p, bg=bg, ex=ex, fc=fc: e.tensor_scalar(
                        out=tA[p][:], in0=bank(bg)[:, 0:512], scalar1=b1[:, ex, fc, 0:1], scalar2=7.0,
                        op0=ALU.add, op1=ALU.min),
                      reads=[('ps', bg), 'b1'], writes=[('tA', p)])
                    A(lambda e, p=p: e.activation(out=tB[p][:], in_=tA[p][:], func=AF.Sigmoid, scale=1.702),
                      reads=[('tA', p)], writes=[('tB', p)])
                    V(lambda e, p=p, bl_=bl_, ex=ex, fc=fc: e.tensor_scalar(
                        out=tC[p][:], in0=bank(bl_)[:, 0:512], scalar1=b1[:, ex, fc, 1:2], scalar2=7.0,
                        op0=ALU.add, op1=ALU.min),
                      reads=[('ps', bl_), 'b1'], writes=[('tC', p)])
                    V(lambda e, p=p: e.tensor_scalar(out=tC[p][:], in0=tC[p][:], scalar1=-7.0, scalar2=1.0,
                                                     op0=ALU.max, op1=ALU.add),
                      reads=[('tC', p)], writes=[('tC', p)])
                    G(lambda e, p=p: e.tensor_tensor(out=tD[p][:], in0=tA[p][:], in1=tB[p][:], op=ALU.mult),
                      reads=[('tA', p), ('tB', p)], writes=[('tD', p)])
                    G(lambda e, p=p, fc=fc, tg=tg: e.tensor_tensor(out=aT[:, fc, tg * 512:(tg + 1) * 512],
                                                                   in0=tD[p][:], in1=tC[p][:], op=ALU.mult),
                      reads=[('tD', p), ('tC', p)], writes=[('aT', tg)])
                if fc + 1 < 8:
                    w2_chunk(fc + 1)
            for t in range(NT):
                for nh in range(2):
                    bk = 4 + sc_[0] % 2
                    sc_[0] += 1

                    def mm2(e, bk=bk, t=t, nh=nh):
                        ins = None
                        for fc in range(8):
                            ins = e.matmul(bank(bk)[:, 0:512], lhsT=aT[:, fc, t * 128:(t + 1) * 128],
                                           rhs=W2[:, fc, nh * 512:(nh + 1) * 512], start=(fc == 0), stop=(fc == 7))
                        return ins
                    T(mm2, reads=[('aT', t // 4), 'W2'], writes=[('ps', bk)])
                    V(lambda e, bk=bk, t=t, nh=nh, ex=ex: e.scalar_tensor_tensor(
                        out=X[:, t, nh * 512:(nh + 1) * 512], in0=bank(bk)[:, 0:512], scalar=Gt[:, t, ex:ex + 1],
                        in1=X[:, t, nh * 512:(nh + 1) * 512], op0=ALU.mult, op1=ALU.add),
                      reads=[('ps', bk), ('X', t, nh)], writes=[('X', t, nh)])
        S.barrier()

    with contextlib.ExitStack() as p5b:
        B2s = sb(p5b, "B2s", [N_EXP, D], F32)
        GT = sb(p5b, "GT", [N_EXP, NT, 128], F32)
        dma('sync', B2s[:], b2_d, writes=['B2s'])
        V(lambda e: e.tensor_tensor(out=B2s[:], in0=B2s[:], in1=gtF_bc[0:N_EXP, :], op=ALU.mult),
          reads=['B2s'], writes=['B2s'])
        for t in range(NT):
            bk = 6 + t % 2
            T(lambda e, t=t, bk=bk: e.transpose(out=bank(bk)[0:N_EXP, 0:128], in_=Gt[:, t, :], identity=identf[:]),
              writes=[('ps', bk)])
            V(lambda e, t=t, bk=bk: e.tensor_copy(out=GT[:, t, :], in_=bank(bk)[0:N_EXP, 0:128]),
              reads=[('ps', bk)], writes=[('GT', t)])
        for t in range(NT):
            for nh in range(2):
                bk = next_pbank()
                T(lambda e, t=t, nh=nh, bk=bk: e.matmul(bank(bk)[:, 0:512], lhsT=GT[:, t, :],
                                                        rhs=B2s[:, nh * 512:(nh + 1) * 512], start=True, stop=True),
                  reads=[('GT', t), 'B2s'], writes=[('ps', bk)])
                V(lambda e, t=t, nh=nh, bk=bk: e.tensor_tensor(out=X[:, t, nh * 512:(nh + 1) * 512],
                                                               in0=bank(bk)[:, 0:512],
                                                               in1=X[:, t, nh * 512:(nh + 1) * 512], op=ALU.add),
                  reads=[('ps', bk), ('X', t, nh)], writes=[('X', t, nh)])
        S.barrier()

    with contextlib.ExitStack() as p6:
        gfin = sb(p6, "gfin", [128, D], F32)
        ot = [sb(p6, "ot%d" % i, [128, D], F32) for i in range(3)]
        junk6 = sb(p6, "junk6", [128, D], BF16)
        dma('sync', gfin[:], gfin_d, writes=['gfin'])
        NO6 = 3

        def s6_sq(t):
            i = 48 + t
            A(lambda e: e.activation(out=junk6[:], in_=X[:, t, :], func=AF.Square, accum_out=ss[:, i:i + 1]),
              reads=[('X', t, 0), ('X', t, 1)], writes=['junk6', ('ss', i)])

        def s6_ts(t):
            i = 48 + t
            V(lambda e: e.tensor_scalar(out=rs[:, i:i + 1], in0=ss[:, i:i + 1], scalar1=1.0 / D, scalar2=EPS,
                                        op0=ALU.mult, op1=ALU.add),
              reads=[('ss', i)], writes=[('rs', i)])

        def s6_sqrt(t):
            i = 48 + t
            A(lambda e: e.activation(out=rs[:, i:i + 1], in_=rs[:, i:i + 1], func=AF.Sqrt),
              reads=[('rs', i)], writes=[('rs', i)])

        def s6_out(t):
            i = 48 + t
            p = t % NO6
            V(lambda e: e.reciprocal(out=rs[:, i:i + 1], in_=rs[:, i:i + 1]),
              reads=[('rs', i)], writes=[('rs', i)])
            V(lambda e: e.scalar_tensor_tensor(out=ot[p][:], in0=X[:, t, :], scalar=rs[:, i:i + 1],
                                               in1=gfin[:], op0=ALU.mult, op1=ALU.mult),
              reads=[('X', t, 0), ('X', t, 1), ('rs', i), 'gfin'], writes=[('ot', p)])
            dma('sync', out_d[t * 128:(t + 1) * 128, :], ot[p][:], reads=[('ot', p)], semname="out%d" % p)
        stages6 = [(s6_sq, 0), (s6_ts, 1), (s6_sqrt, 2), (s6_out, 3)]
        for j in range(NT + 3):
            for fn, lag in stages6:
                t = j - lag
                if 0 <= t < NT:
                    fn(t)
    S.emit(final_waits=[k for k in (('D', 'out'), ('D', 'outg'), ('D', 'out0'), ('D', 'out1'), ('D', 'out2')) if k in S.cnt])
    root.close()
    return nc


def _t5_bucket(dist):
    dist = np.asarray(dist, dtype=np.int64)
    small = dist < 16
    ratio = np.log(np.maximum(dist, 1).astype(np.float32) / np.float32(16)) / np.float32(math.log(2048 / 16))
    large = 16 + (ratio.astype(np.float32) * np.float32(16)).astype(np.int32)
    large = np.minimum(large, 31)
    return np.where(small, dist, large)


def _constants():
    cst = {}
    half = 64
    inv = (10000.0 ** (-np.arange(half, dtype=np.float32) / half)).astype(np.float32)
    pos = np.arange(SEQ, dtype=np.float32)
    ang = pos[None, :] * inv[:, None]
    cos = np.cos(ang).astype(np.float32)
    sin = np.sin(ang).astype(np.float32)
    cst['cos_full'] = np.concatenate([cos, cos], axis=0)
    cst['sin_full'] = np.concatenate([-sin, sin], axis=0)
    Hh = 4
    log_g = np.log1p(-np.exp2(-5.0 - np.arange(Hh, dtype=np.float64)))
    i = np.arange(128, dtype=np.float64)
    scale = 128 ** -0.5
    dm = np.zeros((128, 4, 128), np.float32)
    for h in range(Hh):
        diff = i[None, :] - i[:, None]
        dm[:, h, :] = np.where(diff >= 0, np.exp(np.maximum(diff, 0) * log_g[h]) * scale, 0.0)
    rvec = np.zeros((128, 16), np.float32)
    for h in range(Hh):
        rvec[:, h] = np.exp((127.0 - i) * log_g[h]) * scale
        rvec[:, 4 + h] = np.exp((i + 1.0) * log_g[h])
        rvec[:, 8 + h] = np.exp(128.0 * log_g[h])
    cst['dmask'] = dm
    cst['rvec'] = rvec
    z2 = np.zeros((128, 4, 16), np.float32)
    for h in range(Hh):
        for n in range(16):
            z2[:, h, n] = np.exp((127.0 - i) * log_g[h] + 128.0 * (15 - n) * log_g[h]) * scale
    cst['zeta2'] = z2
    ohf = np.zeros((32, 3, 384), np.float32)
    for g, (w, r) in enumerate(ATT_PATTERNS):
        j = np.arange(129)
        bk = _t5_bucket(r * j)
        for jj in range(129):
            ohf[bk[jj], g, 127 + jj] = 1.0
    cst['ohf'] = ohf
    cst['identf'] = np.eye(128, dtype=np.float32)
    cst['jf'] = np.ascontiguousarray(np.eye(128, dtype=np.float32)[::-1])
    return cst


def _prep_shared(inp):
    sh = {}
    w_in = inp['w_in'][0]

    def chunk(cols):
        return w_in[:, cols].reshape(8, 128, len(cols)).transpose(1, 0, 2)

    def rng(a, n=128):
        return np.arange(a, a + n)
    r1 = []
    r2 = []
    for h in range(4):
        q0 = C_RQ + h * 128
        k0 = C_RK + h * 128
        r1.append(chunk(rng(q0)))
        r1.append(chunk(np.concatenate([rng(q0 + 64, 64), rng(q0, 64)])))
        r1.append(chunk(rng(k0)))
        r1.append(chunk(np.concatenate([rng(k0 + 64, 64), rng(k0, 64)])))
        r2.append(chunk(rng(C_RV + h * 256, 256)))
        r2.append(chunk(rng(C_RG + h * 256, 256)))
    for hg in range(4):
        for g in range(3):
            hd = g * 4 + hg
            r1.append(chunk(rng(C_AQ + hd * 128)))
            r1.append(chunk(rng(C_AK + hd * 128)))
            r1.append(chunk(rng(C_AV + hd * 128)))
    for m in range(8):
        r1.append(chunk(rng(C_GA + m * 128)))
    for m in range(8):
        r1.append(chunk(rng(C_GB + m * 128)))
    sh['w_in_r1'] = np.ascontiguousarray(np.stack(r1, axis=0))
    sh['w_in_r2'] = np.ascontiguousarray(np.stack(r2, axis=0))
    b_ada = inp['b_ada'][0]
    secs = [0, 1, 3, 4]
    bc = np.zeros((128, 32), np.float32)
    for si, s in enumerate(secs):
        bc[:, si * 8:(si + 1) * 8] = b_ada[s * 1024:(s + 1) * 1024].reshape(8, 128).T
    sh['bada_col'] = bc
    bb = np.stack([b_ada[2048:3072], b_ada[5120:6144]], axis=0)
    sh['bada_bc'] = np.ascontiguousarray(np.broadcast_to(bb[None], (128, 2, 1024)))
    sh['gmix_col'] = np.ascontiguousarray(inp['g_mix'][0].reshape(8, 128).T)
    sh['gffn_col'] = np.ascontiguousarray(inp['g_ffn'][0].reshape(8, 128).T)
    sh['gfin_bc'] = np.ascontiguousarray(np.broadcast_to(inp['g_final'][None, :], (128, 1024)))
    sh['w_ada'] = np.ascontiguousarray(inp['w_ada'][0])
    sh['rel_bias'] = np.ascontiguousarray(inp['rel_bias'])
    sh['w_ret_o'] = np.ascontiguousarray(inp['w_ret_o'][0])
    sh['w_att_o'] = np.ascontiguousarray(inp['w_att_o'][0])
    sh['w_o'] = np.ascontiguousarray(inp['w_o'][0])
    sh['w_router'] = np.ascontiguousarray(inp['w_router'][0])
    sh['brouter_bc'] = np.ascontiguousarray(np.broadcast_to(inp['b_router'][0][None, :], (128, 32)))
    w1 = inp['w_mlp1'][0].reshape(32, 8, 128, 8, 128, 2)
    sh['w1r'] = np.ascontiguousarray(w1.transpose(0, 3, 2, 1, 5, 4)).reshape(32, 8, 128, 8, 256)
    sh['b1r'] = np.ascontiguousarray(inp['b_mlp1'][0].reshape(32, 8, 128, 2).transpose(2, 0, 1, 3))
    sh['w_mlp2'] = np.ascontiguousarray(inp['w_mlp2'][0])
    sh['b_mlp2'] = np.ascontiguousarray(inp['b_mlp2'][0])
    return sh


def make_in_maps(inp):
    inp = {k: np.asarray(v, dtype=np.float32) for k, v in inp.items()}
    cst = _constants()
    sh = _prep_shared(inp)
    x = inp['x']
    c = inp['c']
    zeros_prev = np.zeros((HALF, D), np.float32)
    in_maps = []
    for core in range(8):
        b, hf = core // 2, core % 2
        m = dict(sh)
        m['x_own'] = np.ascontiguousarray(x[b, hf * HALF:(hf + 1) * HALF])
        m['x_prev'] = np.ascontiguousarray(x[b, 0:HALF]) if hf == 1 else zeros_prev
        m['flag'] = np.full((128, 1), float(hf), np.float32)
        m['c_col'] = np.ascontiguousarray(c[b].reshape(8, 128).T)
        if hf == 1:
            m['rope_cos'] = cst['cos_full']
            m['rope_sin'] = cst['sin_full']
        else:
            m['rope_cos'] = np.ascontiguousarray(np.concatenate([cst['cos_full'][:, :HALF]] * 2, axis=1))
            m['rope_sin'] = np.ascontiguousarray(np.concatenate([cst['sin_full'][:, :HALF]] * 2, axis=1))
        for k in ('dmask', 'rvec', 'zeta2', 'ohf', 'identf', 'jf'):
            m[k] = cst[k]
        in_maps.append(m)
    return in_maps


def kernel(**inputs):
    in_maps = make_in_maps(inputs)
    nc = build_program()
    res = run_bass_kernel_spmd(nc, in_maps, core_ids=list(range(8)))
    out = np.zeros((4, SEQ, D), np.float32)
    for core in range(8):
        b, hf = core // 2, core % 2
        out[b, hf * HALF:(hf + 1) * HALF] = res.results[core]["out"]
    return out
```
